# Optimizing a Trainium2 kernel written in Bass

```python
import math
import jax, jax.numpy as jnp
from jax import lax
import numpy as np

D_MODEL = 1024
BATCH = 4
SEQ = 8192
DEPTH = 2

CTX_LEN = 256
GRID_W = 64
RMS_EPS = 1e-6
POOL_WIDTH = 1024
POOL_WINDOWS = (2, 4, 8, 16)
POOL_GROUP = POOL_WIDTH // len(POOL_WINDOWS)
LRU_WIDTH = 1024
LRU_BLOCKS = 8
LRU_BLOCK_W = LRU_WIDTH // LRU_BLOCKS
CONV_W = 4
LRU_C = 8.0
MLA_HEADS = 8
Q_LORA = 384
KV_LORA = 256
QK_NOPE = 128
QK_ROPE = 64
V_DIM = 128
MLA_WIDTH = MLA_HEADS * V_DIM
MLA_SCALE = (QK_NOPE + QK_ROPE) ** -0.5
ROPE_FREQS = QK_ROPE // 4
ROPE_THETA = 10000.0
Q_BLOCK = 128
N_BRANCH = 3
IN_SPLITS = (POOL_WIDTH, LRU_WIDTH, LRU_WIDTH, Q_LORA, KV_LORA, QK_ROPE, N_BRANCH * D_MODEL)
IN_COLS = sum(IN_SPLITS)
D_FF = 2816
N_EXPERTS = 8
TOP_K = 2
EXPERT_FF = 3584
MOE_BLOCK = 256
N_DENSE = (DEPTH + 1) // 2
N_MOE = DEPTH // 2

kernel_name = "hybrid_pool_rglru_mla_moe_diffusion_trunk"


def rms_norm(x, g):
    xf = x.astype(jnp.float32)
    y = xf * lax.rsqrt(jnp.mean(xf * xf, axis=-1, keepdims=True) + RMS_EPS)
    return (y * g.astype(jnp.float32)).astype(x.dtype)


def modulate(h, shift, scale):
    return h * (1 + scale) + shift


def split_cols(z):
    offs = [int(o) for o in np.cumsum(IN_SPLITS)[:-1]]
    return jnp.split(z, offs, axis=-1)


def swiglu(h, w1, w3, w2):
    return (jax.nn.silu(h @ w1) * (h @ w3)) @ w2


def multiscale_pool(u):
    L = u.shape[1]
    cs = jnp.pad(jnp.cumsum(u.astype(jnp.float32), axis=1), ((0, 0), (1, 0), (0, 0)))
    t = jnp.arange(L)
    outs = []
    for g, w in enumerate(POOL_WINDOWS):
        lo = jnp.clip(t - w // 2, 0, L)
        hi = jnp.clip(t + w - w // 2, 0, L)
        cg = cs[..., g * POOL_GROUP:(g + 1) * POOL_GROUP]
        s = jnp.take(cg, hi, axis=1) - jnp.take(cg, lo, axis=1)
        outs.append(s / (hi - lo).astype(jnp.float32)[None, :, None])
    return jnp.concatenate(outs, axis=-1).astype(u.dtype) - u


def pool_branch(u, pool_w, pool_scale, pool_proj):
    B, L, _ = u.shape
    m = multiscale_pool(u).reshape(B, L, len(POOL_WINDOWS), POOL_GROUP)
    m = jnp.einsum('blgi,gij->blgj', m, pool_w).reshape(B, L, POOL_WIDTH) * pool_scale
    return m @ pool_proj


def short_conv(u, w, b):
    L = u.shape[1]
    left = CONV_W // 2
    up = jnp.pad(u, ((0, 0), (left, CONV_W - 1 - left), (0, 0)))
    out = b
    for k in range(CONV_W):
        out = out + w[k] * up[:, k:k + L]
    return out


def block_diag(u, w, b):
    B, L, _ = u.shape
    y = jnp.einsum('blnj,njk->blnk', u.reshape(B, L, LRU_BLOCKS, LRU_BLOCK_W), w)
    return y.reshape(B, L, LRU_WIDTH) + b


def rglru_coeffs(u, wa, ba, wx, bx, lam):
    r = jax.nn.sigmoid(block_diag(u, wa, ba).astype(jnp.float32))
    i = jax.nn.sigmoid(block_diag(u, wx, bx).astype(jnp.float32))
    log_a = -LRU_C * r * jax.nn.softplus(-lam.astype(jnp.float32))
    a = jnp.exp(log_a)
    b = jnp.sqrt(-jnp.expm1(2.0 * log_a)) * (i * u.astype(jnp.float32))
    return a, b


def linear_scan(a, b, h0, reverse):
    def combine(l, r):
        return l[0] * r[0], r[0] * l[1] + r[1]
    a_cum, b_cum = lax.associative_scan(combine, (a, b), reverse=reverse, axis=1)
    return a_cum * h0[:, None, :] + b_cum


def axial_rope_tables(n_tokens):
    rows = n_tokens // GRID_W
    row = jnp.repeat(jnp.arange(rows, dtype=jnp.float32), GRID_W)
    col = (jnp.arange(rows * GRID_W) % GRID_W).astype(jnp.float32)
    inv = ROPE_THETA ** (-jnp.arange(ROPE_FREQS, dtype=jnp.float32) / ROPE_FREQS)
    ang = jnp.stack([row[:, None] * inv, col[:, None] * inv], axis=1)
    return jnp.cos(ang), jnp.sin(ang)


def apply_axial_rope(x, cos, sin):
    xs = x.reshape(x.shape[:-1] + (2, 2, ROPE_FREQS))
    x1, x2 = xs[..., 0, :], xs[..., 1, :]
    out = jnp.stack([x1 * cos - x2 * sin, x2 * cos + x1 * sin], axis=-2)
    return out.reshape(x.shape).astype(x.dtype)


def mla_query(cq, q_norm_g, w_uq):
    B, L, _ = cq.shape
    return (rms_norm(cq, q_norm_g) @ w_uq).reshape(B, L, MLA_HEADS, QK_NOPE + QK_ROPE)


def mla_keys_values(ckv, k_rope, kv_norm_g, w_ukv):
    B, L, _ = ckv.shape
    kv = (rms_norm(ckv, kv_norm_g) @ w_ukv).reshape(B, L, MLA_HEADS, QK_NOPE + V_DIM)
    k_r = jnp.broadcast_to(k_rope[:, :, None, :], (B, L, MLA_HEADS, QK_ROPE))
    return jnp.concatenate([kv[..., :QK_NOPE], k_r], axis=-1), kv[..., QK_NOPE:]


def attend(q, k, v):
    s = jnp.einsum('bqhd,bkhd->bhqk', q, k).astype(jnp.float32) * MLA_SCALE
    p = jax.nn.softmax(s, axis=-1).astype(v.dtype)
    return jnp.einsum('bhqk,bkhd->bqhd', p, v)


def attend_blocks(q, k, v):
    B, L, H, Dk = q.shape
    nb = L // Q_BLOCK
    qb = q.reshape(B, nb, Q_BLOCK, H, Dk).transpose(1, 0, 2, 3, 4)
    ob = lax.map(lambda qi: attend(qi, k, v), qb)
    return ob.transpose(1, 0, 2, 3, 4).reshape(B, L, H, V_DIM)


def merge_branches(gt, ys, w_out):
    g = jnp.split(jax.nn.sigmoid(gt.astype(jnp.float32)).astype(gt.dtype), N_BRANCH, axis=-1)
    return (g[0] * ys[0] + g[1] * ys[1] + g[2] * ys[2]) @ w_out


def token_mixer(h_ctx, h_lat, cos, sin, w_in, pool_w, pool_scale, pool_proj, conv_w, conv_b,
                gate_a_w, gate_a_b, gate_x_w, gate_x_b, lru_lambda, lru_proj,
                q_norm_g, w_uq, kv_norm_g, w_ukv, mla_proj, w_out, need_ctx):
    B, L, _ = h_lat.shape
    pool_c, lx_c, lg_c, cq_c, ckv_c, kr_c, gt_c = split_cols(h_ctx @ w_in)
    pool_l, lx_l, lg_l, cq_l, ckv_l, kr_l, gt_l = split_cols(h_lat @ w_in)

    u_c = short_conv(lx_c, conv_w, conv_b)
    u_l = short_conv(lx_l, conv_w, conv_b)
    ctx_dirs, lat_dirs = [], []
    for d in range(2):
        rev = d == 1
        a, b = rglru_coeffs(u_c, gate_a_w[d], gate_a_b[d], gate_x_w[d], gate_x_b[d], lru_lambda[d])
        hc = linear_scan(a, b, jnp.zeros_like(b[:, 0]), rev)
        ctx_dirs.append(hc)
        h0 = hc[:, 0] if rev else hc[:, -1]
        a, b = rglru_coeffs(u_l, gate_a_w[d], gate_a_b[d], gate_x_w[d], gate_x_b[d], lru_lambda[d])
        lat_dirs.append(linear_scan(a, b, h0, rev))
    y_lru_l = ((lat_dirs[0] + lat_dirs[1]).astype(lx_l.dtype) * jax.nn.gelu(lg_l)) @ lru_proj

    k_c, v_c = mla_keys_values(ckv_c, kr_c, kv_norm_g, w_ukv)
    k_l, v_l = mla_keys_values(ckv_l, apply_axial_rope(kr_l, cos, sin), kv_norm_g, w_ukv)
    q_l = mla_query(cq_l, q_norm_g, w_uq)
    q_l = jnp.concatenate([q_l[..., :QK_NOPE],
                           apply_axial_rope(q_l[..., QK_NOPE:], cos[:, None], sin[:, None])], axis=-1)
    k_all = jnp.concatenate([k_c, k_l], axis=1)
    v_all = jnp.concatenate([v_c, v_l], axis=1)
    y_mla_l = attend_blocks(q_l, k_all, v_all).reshape(B, L, MLA_WIDTH) @ mla_proj

    y_pool_l = pool_branch(pool_l, pool_w, pool_scale, pool_proj)
    y_lat = merge_branches(gt_l, (y_pool_l, y_lru_l, y_mla_l), w_out)
    if not need_ctx:
        return None, y_lat

    Bc, Lc, _ = h_ctx.shape
    y_pool_c = pool_branch(pool_c, pool_w, pool_scale, pool_proj)
    y_lru_c = ((ctx_dirs[0] + ctx_dirs[1]).astype(lx_c.dtype) * jax.nn.gelu(lg_c)) @ lru_proj
    q_c = mla_query(cq_c, q_norm_g, w_uq)
    y_mla_c = attend(q_c, k_c, v_c).reshape(Bc, Lc, MLA_WIDTH) @ mla_proj
    y_ctx = merge_branches(gt_c, (y_pool_c, y_lru_c, y_mla_c), w_out)
    return y_ctx, y_lat


def moe_swiglu(h, router_w, w1, w3, w2):
    n_tok = h.shape[0]
    n_assign = n_tok * TOP_K
    logits = (h @ router_w).astype(jnp.float32)
    top_v, top_i = lax.top_k(logits, TOP_K)
    gates = jax.nn.softmax(top_v, axis=-1)
    e_flat = top_i.reshape(-1)
    tok_flat = jnp.repeat(jnp.arange(n_tok), TOP_K)
    order = jnp.argsort(e_flat)
    e_s, tok_s, g_s = e_flat[order], tok_flat[order], gates.reshape(-1)[order]
    counts = jnp.bincount(e_flat, length=N_EXPERTS)
    starts = jnp.cumsum(counts) - counts
    padded = (counts + MOE_BLOCK - 1) // MOE_BLOCK * MOE_BLOCK
    pad_end = jnp.cumsum(padded)
    pad_start = pad_end - padded
    dest = pad_start[e_s] + jnp.arange(n_assign) - starts[e_s]
    n_rows = -(-n_assign // MOE_BLOCK) * MOE_BLOCK + N_EXPERTS * MOE_BLOCK
    n_blocks = n_rows // MOE_BLOCK
    buf = jnp.zeros((n_rows, h.shape[1]), h.dtype).at[dest].set(h[tok_s])
    blk_e = jnp.minimum(jnp.searchsorted(pad_end, jnp.arange(n_blocks) * MOE_BLOCK, side='right'),
                        N_EXPERTS - 1)

    def expert_block(args):
        xb, e = args
        return swiglu(xb, w1[e], w3[e], w2[e])

    y_blk = lax.map(expert_block, (buf.reshape(n_blocks, MOE_BLOCK, h.shape[1]), blk_e))
    y_rows = y_blk.reshape(n_rows, h.shape[1])[dest]
    return jnp.zeros_like(h).at[tok_s].add(g_s[:, None].astype(h.dtype) * y_rows)


def setup_inputs(seed: int = 0) -> dict:
    key = jax.random.key(seed)
    ks = iter(jax.random.split(key, 48))

    def nrm(shape, scale):
        return scale * jax.random.normal(next(ks), shape, jnp.float32)

    D = D_MODEL
    u = jax.random.uniform(next(ks), (DEPTH, 2, LRU_WIDTH), jnp.float32, 0.9, 0.999)
    return {
        "x": nrm((BATCH, SEQ, D), 1.0),
        "c": nrm((BATCH, D), 1.0),
        "ctx": nrm((BATCH, CTX_LEN, D), 1.0),
        "c_ctx": nrm((D,), 1.0),
        "mod_w": nrm((DEPTH, D, 6 * D), 0.5 * D ** -0.5),
        "mod_b": nrm((DEPTH, 6 * D), 0.02),
        "pre_mix_g": 1.0 + nrm((DEPTH, D), 0.05),
        "post_mix_g": 1.0 + nrm((DEPTH, D), 0.05),
        "pre_ffn_g": 1.0 + nrm((DEPTH, D), 0.05),
        "post_ffn_g": 1.0 + nrm((DEPTH, D), 0.05),
        "w_in": nrm((DEPTH, D, IN_COLS), D ** -0.5),
        "pool_w": nrm((DEPTH, len(POOL_WINDOWS), POOL_GROUP, POOL_GROUP), POOL_GROUP ** -0.5),
        "pool_scale": 1.0 + nrm((DEPTH, POOL_WIDTH), 0.1),
        "pool_proj": nrm((DEPTH, POOL_WIDTH, D), POOL_WIDTH ** -0.5),
        "conv_w": nrm((DEPTH, CONV_W, LRU_WIDTH), CONV_W ** -0.5),
        "conv_b": nrm((DEPTH, LRU_WIDTH), 0.01),
        "gate_a_w": nrm((DEPTH, 2, LRU_BLOCKS, LRU_BLOCK_W, LRU_BLOCK_W), LRU_BLOCK_W ** -0.5),
        "gate_a_b": nrm((DEPTH, 2, LRU_WIDTH), 0.1),
        "gate_x_w": nrm((DEPTH, 2, LRU_BLOCKS, LRU_BLOCK_W, LRU_BLOCK_W), LRU_BLOCK_W ** -0.5),
        "gate_x_b": nrm((DEPTH, 2, LRU_WIDTH), 0.1),
        "lru_lambda": jnp.log(u) - jnp.log1p(-u),
        "lru_proj": nrm((DEPTH, LRU_WIDTH, D), LRU_WIDTH ** -0.5),
        "q_norm_g": 1.0 + nrm((DEPTH, Q_LORA), 0.05),
        "w_uq": nrm((DEPTH, Q_LORA, MLA_HEADS * (QK_NOPE + QK_ROPE)), Q_LORA ** -0.5),
        "kv_norm_g": 1.0 + nrm((DEPTH, KV_LORA), 0.05),
        "w_ukv": nrm((DEPTH, KV_LORA, MLA_HEADS * (QK_NOPE + V_DIM)), KV_LORA ** -0.5),
        "mla_proj": nrm((DEPTH, MLA_WIDTH, D), MLA_WIDTH ** -0.5),
        "w_out": nrm((DEPTH, D, D), D ** -0.5),
        "ffn_w1": nrm((N_DENSE, D, D_FF), D ** -0.5),
        "ffn_w3": nrm((N_DENSE, D, D_FF), D ** -0.5),
        "ffn_w2": nrm((N_DENSE, D_FF, D), D_FF ** -0.5),
        "router_w": nrm((N_MOE, D, N_EXPERTS), D ** -0.5),
        "moe_w1": nrm((N_MOE, N_EXPERTS, D, EXPERT_FF), D ** -0.5),
        "moe_w3": nrm((N_MOE, N_EXPERTS, D, EXPERT_FF), D ** -0.5),
        "moe_w2": nrm((N_MOE, N_EXPERTS, EXPERT_FF, D), EXPERT_FF ** -0.5),
    }


def reference(x, c, ctx, c_ctx, mod_w, mod_b, pre_mix_g, post_mix_g, pre_ffn_g, post_ffn_g, w_in,
              pool_w, pool_scale, pool_proj, conv_w, conv_b, gate_a_w, gate_a_b, gate_x_w, gate_x_b,
              lru_lambda, lru_proj, q_norm_g, w_uq, kv_norm_g, w_ukv, mla_proj, w_out,
              ffn_w1, ffn_w3, ffn_w2, router_w, moe_w1, moe_w3, moe_w2):
    cos, sin = axial_rope_tables(x.shape[1])
    x_lat, x_ctx = x, ctx
    for l in range(DEPTH):
        last = l == DEPTH - 1
        m_lat = jnp.split((jax.nn.silu(c) @ mod_w[l] + mod_b[l])[:, None, :], 6, axis=-1)
        m_ctx = jnp.split(jax.nn.silu(c_ctx) @ mod_w[l] + mod_b[l], 6, axis=-1)

        h_lat = modulate(rms_norm(x_lat, pre_mix_g[l]), m_lat[0], m_lat[1])
        h_ctx = modulate(rms_norm(x_ctx, pre_mix_g[l]), m_ctx[0], m_ctx[1])
        y_ctx, y_lat = token_mixer(h_ctx, h_lat, cos, sin, w_in[l], pool_w[l], pool_scale[l], pool_proj[l],
                                   conv_w[l], conv_b[l], gate_a_w[l], gate_a_b[l], gate_x_w[l], gate_x_b[l],
                                   lru_lambda[l], lru_proj[l], q_norm_g[l], w_uq[l], kv_norm_g[l], w_ukv[l],
                                   mla_proj[l], w_out[l], not last)
        x_lat = x_lat + m_lat[2] * rms_norm(y_lat, post_mix_g[l])

        h2_lat = modulate(rms_norm(x_lat, pre_ffn_g[l]), m_lat[3], m_lat[4])
        n_lat = h2_lat.shape[0] * h2_lat.shape[1]
        if last:
            tokens = h2_lat.reshape(n_lat, D_MODEL)
        else:
            x_ctx = x_ctx + m_ctx[2] * rms_norm(y_ctx, post_mix_g[l])
            h2_ctx = modulate(rms_norm(x_ctx, pre_ffn_g[l]), m_ctx[3], m_ctx[4])
            tokens = jnp.concatenate([h2_lat.reshape(n_lat, D_MODEL), h2_ctx.reshape(-1, D_MODEL)], axis=0)
        if l % 2 == 0:
            f = swiglu(tokens, ffn_w1[l // 2], ffn_w3[l // 2], ffn_w2[l // 2])
        else:
            f = moe_swiglu(tokens, router_w[l // 2], moe_w1[l // 2], moe_w3[l // 2], moe_w2[l // 2])
        x_lat = x_lat + m_lat[5] * rms_norm(f[:n_lat].reshape(x_lat.shape), post_ffn_g[l])
        if not last:
            x_ctx = x_ctx + m_ctx[5] * rms_norm(f[n_lat:].reshape(x_ctx.shape), post_ffn_g[l])
    return x_lat
```

```python
import numpy as np
import concourse.bass as bass
import concourse.mybir as mybir
from concourse.bass_utils import run_bass_kernel_spmd

F32 = mybir.dt.float32
BF16 = mybir.dt.bfloat16
AF = mybir.ActivationFunctionType
ALU = mybir.AluOpType
AX = mybir.AxisListType

D = 1024
SEQ = 8192
CTX = 256
LT = SEQ + CTX
NCH = 54
ZW = NCH * 128
EPS = 1e-6
DFF = 2816
EFF = 3584
NEXP = 8
MLA_SCALE = 192 ** -0.5
VNAMES = ["pre_mix_g", "pre_ffn_g", "pool_scale", "conv_b", "conv_w0", "conv_w1", "conv_w2", "conv_w3",
          "gate_a_b0", "gate_a_b1", "gate_x_b0", "gate_x_b1", "lru_lambda0", "lru_lambda1"]
NV = len(VNAMES)
VI = {n: i for i, n in enumerate(VNAMES)}
TILES = [(0, 256)] + [(256 + 512 * i, 512) for i in range(16)]
HALF = SEQ // 2
LTILES = [(512 * i, 512) for i in range(8)]


class Res:
    __slots__ = ("name", "w", "r", "dsem", "dcnt", "dq")

    def __init__(self, name):
        self.name = name
        self.w = None
        self.r = {}
        self.dsem = None
        self.dcnt = 0
        self.dq = None


class KB:
    def __init__(self, nc):
        self.nc = nc
        self.eng = {"pe": nc.tensor, "dve": nc.vector, "act": nc.scalar, "pool": nc.gpsimd, "sp": nc.sync}
        self.esem = {n: nc.alloc_semaphore("es_" + n) for n in self.eng}
        self.ecnt = {n: 0 for n in self.eng}
        self.seen = {n: {} for n in self.eng}
        self.nres = 0
        self.ndsem = 0
        self.local = []
        self.persist = []
        self.free_dsems = {"pool": [], "sp": []}

    def res(self, name="r", persist=False):
        self.nres += 1
        r = Res(name + str(self.nres))
        (self.persist if persist else self.local).append(r)
        return r

    def end_phase(self):
        deps = [(self.esem[e], self.ecnt[e]) for e in self.eng if self.ecnt[e] > 0]
        for r in self.local + self.persist:
            if r.dsem is not None:
                deps.append((r.dsem, r.dcnt))
        for en in self.eng:
            self._wait(en, deps)
        for r in self.local:
            if r.dsem is not None:
                self.free_dsems[r.dq].append((r.dsem, r.dcnt))
                r.dsem = None
                r.dq = None
        self.local = []
        for r in self.persist:
            r.w = None
            r.r = {}

    def _deps(self, reads, writes, dma=False):
        deps = []
        for r in reads:
            if r.w is not None:
                deps.append(r.w)
        for w in writes:
            if w.w is not None and not (dma and w.dsem is not None and w.w[0] is w.dsem):
                deps.append(w.w)
            for s, v in w.r.values():
                deps.append((s, v))
        return deps

    def _wait(self, en, deps):
        own = self.esem[en]
        seen = self.seen[en]
        for sem, val in deps:
            if sem is own and en == "pe":
                continue
            key = id(sem)
            if seen.get(key, 0) >= val:
                continue
            self.eng[en].wait_ge(sem, val)
            seen[key] = val

    def _record(self, tok, reads, writes):
        key = id(tok[0])
        for r in reads:
            old = r.r.get(key)
            if old is None or old[1] < tok[1]:
                r.r[key] = tok
        for w in writes:
            w.w = tok
            w.r = {}

    def op(self, en, fn, reads=(), writes=(), inc=True):
        self._wait(en, self._deps(reads, writes))
        inst = fn(self.eng[en])
        if inc:
            self.ecnt[en] += 1
            inst.then_inc(self.esem[en], 1)
            tok = (self.esem[en], self.ecnt[en])
        else:
            tok = (self.esem[en], self.ecnt[en] + 1)
        self._record(tok, reads, writes)
        return inst

    def dma(self, en, out, in_, reads=(), writes=()):
        self._wait(en, self._deps(reads, writes, dma=True))
        inst = self.eng[en].dma_start(out=out, in_=in_)
        w = writes[0]
        if w.dsem is None:
            w.dq = en
            if self.free_dsems[en]:
                w.dsem, w.dcnt = self.free_dsems[en].pop()
            else:
                w.dsem = self.nc.alloc_semaphore("ds_" + w.name)
                self.ndsem += 1
        assert w.dq == en, (w.name, w.dq, en)
        w.dcnt += 16
        inst.then_inc(w.dsem, 16)
        tok = (w.dsem, w.dcnt)
        self._record(tok, reads, writes)
        return inst

    def wait_all(self, en, ress):
        deps = []
        for r in ress:
            if r.w is not None:
                deps.append(r.w)
        self._wait(en, deps)


class Ctx:
    pass


def g_dbg_layer(dbg):
    return 1 if "L1" in dbg else 0


def g_att_heads(dbg):
    for d in dbg:
        if d.startswith("heads"):
            return int(d[5:])
    return 8


_uid = [0]


def nm(s):
    _uid[0] += 1
    return "t%d_%s" % (_uid[0], s)


def build(nc, n_layers=2, dbg=(), stop_after=None):
    kb = KB(nc)
    g = Ctx()
    g.nc, g.kb = nc, kb
    g.dbg = {}

    def din(name, shape, dt=F32):
        return nc.dram_tensor(name, list(shape), dt, kind="ExternalInput").ap()

    def dscr(name, shape, dt):
        t = nc.dram_tensor(name, list(shape), dt, kind="Internal").ap()
        return t

    I = {}
    I["xall"] = din("xall", [LT, D])
    I["ccol"] = din("ccol", [128, 8, 2])
    I["ident"] = din("ident", [128, 128])
    I["cosT"] = din("cosT", [64, SEQ])
    I["sinT"] = din("sinT", [64, SEQ])
    I["cosQ"] = din("cosQ", [64, HALF])
    I["sinQ"] = din("sinQ", [64, HALF])
    I["hm"] = din("hm", [128, 2])
    I["mod_w"] = din("mod_w", [2, D, 6 * D])
    I["mod_b"] = din("mod_b", [2, 6 * D])
    I["mod_b_col"] = din("mod_b_col", [2, 128, 48])
    I["vcol"] = din("vcol", [2, 128, NV, 8])
    I["qg_col"] = din("qg_col", [2, 128, 3])
    I["kvg_col"] = din("kvg_col", [2, 128, 2])
    for n in ("pre_mix_g", "post_mix_g", "pre_ffn_g", "post_ffn_g", "pool_scale", "conv_b"):
        I[n] = din(n, [2, D])
    I["w_in"] = din("w_in", [2, D, ZW])
    I["pool_w"] = din("pool_w", [2, 4, 256, 256])
    I["pool_proj"] = din("pool_proj", [2, D, D])
    I["conv_w"] = din("conv_w", [2, 4, D])
    I["gate_a_w"] = din("gate_a_w", [2, 2, 8, 128, 128])
    I["gate_x_w"] = din("gate_x_w", [2, 2, 8, 128, 128])
    I["gate_a_b"] = din("gate_a_b", [2, 2, D])
    I["gate_x_b"] = din("gate_x_b", [2, 2, D])
    I["lru_lambda"] = din("lru_lambda", [2, 2, D])
    I["lru_proj"] = din("lru_proj", [2, D, D])
    I["q_norm_g"] = din("q_norm_g", [2, 384])
    I["w_uq"] = din("w_uq", [2, 384, 2048])
    I["kv_norm_g"] = din("kv_norm_g", [2, 256])
    I["w_ukv"] = din("w_ukv", [2, 256, 2048])
    I["mla_proj"] = din("mla_proj", [2, D, D])
    I["w_out"] = din("w_out", [2, D, D])
    I["ffn_w1"] = din("ffn_w1", [1, D, DFF])
    I["ffn_w3"] = din("ffn_w3", [1, D, DFF])
    I["ffn_w2"] = din("ffn_w2", [1, DFF, D])
    I["router_wT"] = din("router_wT", [1, NEXP, D])
    I["moe_w1"] = din("moe_w1", [1, NEXP, D, EFF])
    I["moe_w3"] = din("moe_w3", [1, NEXP, D, EFF])
    I["moe_w2"] = din("moe_w2", [1, NEXP, EFF, D])
    g.I = I
    out = nc.dram_tensor("out", [HALF, D], F32, kind="ExternalOutput").ap()
    g.out = out
    g.r_out = kb.res("out", persist=True)

    S = {}
    S["Z"] = dscr("sZ", [ZW, LT], BF16)
    for n in ("MP", "YL", "AT"):
        S[n] = dscr("s" + n, [D, LT], BF16)
    S["KN"] = dscr("sKN", [8, 128, LT], BF16)
    S["KRD"] = dscr("sKRD", [64, LT], BF16)
    S["V"] = dscr("sV", [LT, D], BF16)
    S["QN"] = dscr("sQN", [8, 128, LT], BF16)
    S["QR"] = dscr("sQR", [8, 64, LT], BF16)
    S["X1"] = dscr("sX1", [LT, D], F32)
    S["X2"] = dscr("sX2", [LT, D], F32)
    S["FA"] = dscr("sFA", [EFF, LT], BF16)
    S["H2T"] = dscr("sH2T", [D, LT], BF16)
    S["GT"] = dscr("sGT", [LT, NEXP], F32)
    S["YA0"] = dscr("sYA0", [LT, D], F32)
    S["YA1"] = dscr("sYA1", [LT, D], F32)
    S["CQl"] = dscr("sCQl", [384, HALF], BF16)
    S["Gl"] = dscr("sGl", [3072, HALF], BF16)
    S["MPl"] = dscr("sMPl", [D, HALF], BF16)
    S["YLl"] = dscr("sYLl", [D, HALF], BF16)
    S["ATl"] = dscr("sATl", [D, HALF], BF16)
    S["Xl"] = dscr("sXl", [HALF, D], F32)
    S["X1l"] = dscr("sX1l", [HALF, D], F32)
    S["QNl"] = dscr("sQNl", [8, 128, HALF], BF16)
    S["QRl"] = dscr("sQRl", [8, 64, HALF], BF16)
    g.S = S
    g.RS = {k: kb.res("s" + k, persist=True) for k in S}
    g.r_in = kb.res("inputs", persist=True)

    def dbg_out(name, src_ap, src_res, shape, dt):
        o = nc.dram_tensor("dbg_" + name, list(shape), dt, kind="ExternalOutput").ap()
        r = kb.res("dbg" + name, persist=True)
        kb.dma("sp", o, src_ap, reads=[src_res], writes=[r])
        g.dbg[name] = r

    def sb(name, shape, dt):
        return nc.alloc_sbuf_tensor(nm(name), list(shape), dt)

    g.ident = sb("ident", [128, 128], BF16)
    g.r_ident = kb.res("ident", persist=True)
    kb.dma("pool", g.ident[:], I["ident"], reads=[g.r_in], writes=[g.r_ident])
    g.ones = sb("ones", [128, 128], BF16)
    g.r_ones = kb.res("ones", persist=True)
    kb.op("dve", lambda e: e.memset(g.ones[:], 1.0), writes=[g.r_ones])
    g.mv = sb("mv", [128, 48, 2], F32)
    g.G1 = sb("G1", [128, 8, 2], F32)
    g.G2 = sb("G2", [128, 8, 2], F32)
    g.GR = sb("GR", [128, 4, D], F32)
    g.MR = sb("MR", [128, 2, D], F32)
    g.r_mod = kb.res("mod", persist=True)
    g.psum = [nc.alloc_psum_tensor("ps%d" % i, [128, 512], F32) for i in range(8)]
    g.rps = [kb.res("ps", persist=True) for i in range(8)]

    def dbgS(name, dt=BF16):
        if name in dbg:
            dbg_out(name, S[name], g.RS[name], list(S[name].shape), dt)

    for l in range(n_layers):
        dl = (l == g_dbg_layer(dbg))
        Xsrc, r_X = (I["xall"], g.r_in) if l == 0 else (S["X2"], g.RS["X2"])
        phase_mod(g, l)
        kb.end_phase()
        phase_A(g, l, Xsrc, r_X)
        kb.end_phase()
        if dl:
            dbgS("Z")
        if stop_after == "A":
            break
        phase_pool(g, l)
        kb.end_phase()
        if dl:
            dbgS("MP")
        if stop_after == "pool":
            break
        phase_lru(g, l)
        kb.end_phase()
        if dl:
            dbgS("YL")
        if stop_after == "lru":
            break
        phase_kv(g, l)
        kb.end_phase()
        loc = (l == n_layers - 1) and n_layers == 2
        if loc:
            phase_sel(g)
            kb.end_phase()
        phase_q(g, l, loc)
        kb.end_phase()
        if dl:
            dbgS("KN"); dbgS("KRD"); dbgS("V"); dbgS("QN"); dbgS("QR")
        if stop_after == "kvq":
            break
        phase_att(g, l, loc, att_heads=g_att_heads(dbg))
        kb.end_phase()
        if dl:
            dbgS("AT")
        if stop_after == "att":
            break
        phase_D1(g, l, Xsrc, r_X, loc)
        kb.end_phase()
        if dl:
            dbgS("X1", F32)
        if stop_after == "D1":
            break
        if l == 0:
            phase_ffn_up(g, I["ffn_w1"][0], I["ffn_w3"][0], DFF // 128, S["X1"], g.RS["X1"], None, TILES, l)
            kb.end_phase()
            phase_ffn_down(g, I["ffn_w2"][0], DFF // 128, TILES, l, first=True, last=True, e=0, dst=S["X2"], dst_res=g.RS["X2"], dst_off=0,
                           x1=S["X1"], r_x1=g.RS["X1"])
            kb.end_phase()
            if dl:
                dbgS("X2", F32)
        else:
            phase_router(g, l, S["X1l"], g.RS["X1l"])
            kb.end_phase()
            if dl:
                dbgS("GT", F32)
            for e in range(NEXP):
                phase_ffn_up(g, I["moe_w1"][0, e], I["moe_w3"][0, e], EFF // 128, None, None, S["H2T"], LTILES, l)
                kb.end_phase()
                phase_ffn_down(g, I["moe_w2"][0, e], EFF // 128, LTILES, l, first=(e == 0), last=(e == NEXP - 1), e=e,
                               dst=g.out, dst_res=g.r_out, dst_off=0, x1=S["X1l"], r_x1=g.RS["X1l"])
                kb.end_phase()

    fin = [g.r_out] + list(g.dbg.values())
    kb.wait_all("sp", fin)
    return g


def load_w(g, dst, src, r_dst, stg, r_stg, cnt):
    kb = g.kb
    n = dst.shape[-1]
    cw = stg.shape[-1]
    for c0 in range(0, n, cw):
        c1 = min(n, c0 + cw)
        b = cnt[0] % 2
        cnt[0] += 1
        kb.dma("sp", stg[:, b, :c1 - c0], src[:, c0:c1], reads=[g.r_in], writes=[r_stg[b]])
        if b == 0:
            kb.op("dve", lambda e, b=b, c0=c0, c1=c1: e.tensor_copy(out=dst[:, c0:c1], in_=stg[:, b, :c1 - c0]), reads=[r_stg[b]], writes=[r_dst])
        else:
            kb.op("act", lambda e, b=b, c0=c0, c1=c1: e.activation(out=dst[:, c0:c1], in_=stg[:, b, :c1 - c0], func=AF.Copy), reads=[r_stg[b]], writes=[r_dst])


def phase_mod(g, l):
    nc, kb, I = g.nc, g.kb, g.I
    with nc.sbuf_tensor(nm("MW"), [128, 8, 6 * D], BF16) as MW, \
            nc.sbuf_tensor(nm("cc"), [128, 8, 2], F32) as cc, \
            nc.sbuf_tensor(nm("sc"), [128, 8, 2], BF16) as sc, \
            nc.sbuf_tensor(nm("scb"), [128, 8, 2, 128], BF16) as scb, \
            nc.sbuf_tensor(nm("modb_col"), [128, 48], F32) as modb_col, \
            nc.sbuf_tensor(nm("gcol"), [128, 2, 8], F32) as gcol, \
            nc.sbuf_tensor(nm("modb_bc"), [128, 2, D], F32) as modb_bc, \
            nc.sbuf_tensor(nm("postg_bc"), [128, 2, D], F32) as postg_bc, \
            nc.sbuf_tensor(nm("modb_bc2"), [128, 2, D], F32) as modb_bc2, \
            nc.sbuf_tensor(nm("preg_bc"), [128, D], F32) as preg_bc, \
            nc.sbuf_tensor(nm("stg"), [128, 2, 2048], F32) as stg, \
            nc.sbuf_tensor(nm("mtmp"), [128, 512], F32) as mtmp:
        r_stg = [kb.res("stg") for _ in range(2)]
        cnt = [0]
        r_MW = [kb.res("MW") for _ in range(8)]
        r_c, r_sc, r_scb, r_mb, r_gc, r_bc, r_tmp, r_bc2 = (kb.res("m") for _ in range(8))
        mw = I["mod_w"][l].rearrange("(k p) n -> p k n", p=128)
        for k in range(8):
            load_w(g, MW[:, k, :], mw[:, k, :], r_MW[k], stg, r_stg, cnt)
        kb.dma("sp", cc[:], I["ccol"], reads=[g.r_in], writes=[r_c])
        kb.dma("sp", modb_col[:], I["mod_b_col"][l], reads=[g.r_in], writes=[r_mb])
        kb.dma("sp", gcol[:], I["vcol"][l, :, 0:2, :], reads=[g.r_in], writes=[r_gc])
        kb.dma("sp", modb_bc[:, 0, :], I["mod_b"][l, 2 * D:3 * D].partition_broadcast(128), reads=[g.r_in], writes=[r_bc])
        kb.dma("sp", modb_bc[:, 1, :], I["mod_b"][l, 5 * D:6 * D].partition_broadcast(128), reads=[g.r_in], writes=[r_bc])
        kb.dma("sp", postg_bc[:, 0, :], I["post_mix_g"][l].partition_broadcast(128), reads=[g.r_in], writes=[r_bc])
        kb.dma("sp", postg_bc[:, 1, :], I["post_ffn_g"][l].partition_broadcast(128), reads=[g.r_in], writes=[r_bc])
        kb.op("act", lambda e: e.activation(out=sc[:], in_=cc[:], func=AF.Silu), reads=[r_c], writes=[r_sc])
        for k in range(8):
            for s in range(2):
                kb.op("dve", lambda e, k=k, s=s: e.tensor_copy(out=scb[:, k, s, :], in_=sc[:, k, s:s + 1].to_broadcast([128, 128])),
                      reads=[r_sc], writes=[r_scb])
        for j in range(48):
            bank = 0 if j < 24 else 3
            ps = g.psum[bank]
            c0 = (j % 24) * 16
            for k in range(8):
                kb.op("pe", lambda e, j=j, k=k, ps=ps, c0=c0: e.matmul(ps[:, c0:c0 + 2], lhsT=MW[:, k, j * 128:(j + 1) * 128], rhs=sc[:, k, :],
                                                                      start=(k == 0), stop=(k == 7)),
                      reads=[r_MW[k], r_sc], writes=[g.rps[bank]], inc=(j % 24 == 23 and k == 7))
        for hb, bank in enumerate((0, 3)):
            ps = g.psum[bank]
            kb.op("dve", lambda e, ps=ps, hb=hb: e.tensor_tensor(
                out=g.mv[:, hb * 24:(hb + 1) * 24, :], in0=ps[:, 0:384].rearrange("p (j s) -> p j s", s=16)[:, :, 0:2],
                in1=modb_col[:, hb * 24:(hb + 1) * 24].unsqueeze(2).to_broadcast([128, 24, 2]), op=ALU.add),
                reads=[g.rps[bank], r_mb], writes=[g.r_mod])
        kb.op("dve", lambda e: e.scalar_tensor_tensor(out=g.G1[:], in0=g.mv[:, 8:16, :], scalar=1.0,
                                                      in1=gcol[:, 0, :].unsqueeze(2).to_broadcast([128, 8, 2]), op0=ALU.add, op1=ALU.mult),
              reads=[g.r_mod, r_gc], writes=[g.r_mod])
        kb.op("dve", lambda e: e.scalar_tensor_tensor(out=g.G2[:], in0=g.mv[:, 32:40, :], scalar=1.0,
                                                      in1=gcol[:, 1, :].unsqueeze(2).to_broadcast([128, 8, 2]), op0=ALU.add, op1=ALU.mult),
              reads=[g.r_mod, r_gc], writes=[g.r_mod])
        if l == 1:
            kb.dma("sp", modb_bc2[:, 0, :], I["mod_b"][l, 3 * D:4 * D].partition_broadcast(128), reads=[g.r_in], writes=[r_bc2])
            kb.dma("sp", modb_bc2[:, 1, :], I["mod_b"][l, 4 * D:5 * D].partition_broadcast(128), reads=[g.r_in], writes=[r_bc2])
            kb.dma("sp", preg_bc[:], I["pre_ffn_g"][l].partition_broadcast(128), reads=[g.r_in], writes=[r_bc2])
            for part in range(2):
                col0 = (3 + part) * D
                for cb in range(2):
                    bank = 1 + cb
                    pb = g.psum[bank]
                    for k in range(8):
                        kb.op("pe", lambda e, k=k, pb=pb, c0=col0 + cb * 512: e.matmul(
                            pb[:], lhsT=scb[:, k, 0, :], rhs=MW[:, k, c0:c0 + 512], start=(k == 0), stop=(k == 7)),
                            reads=[r_MW[k], r_scb], writes=[g.rps[bank]], inc=(k == 7))
                    cs = slice(cb * 512, (cb + 1) * 512)
                    if part == 0:
                        kb.op("dve", lambda e, pb=pb, cs=cs: e.tensor_tensor(out=g.MR[:, 1, cs], in0=pb[:], in1=modb_bc2[:, 0, cs], op=ALU.add),
                              reads=[g.rps[bank], r_bc2], writes=[g.r_mod])
                    else:
                        kb.op("dve", lambda e, pb=pb, cs=cs: e.tensor_tensor(out=mtmp[:], in0=pb[:], in1=modb_bc2[:, 1, cs], op=ALU.add),
                              reads=[g.rps[bank], r_bc2], writes=[r_tmp])
                        kb.op("dve", lambda e, cs=cs: e.scalar_tensor_tensor(out=g.MR[:, 0, cs], in0=mtmp[:], scalar=1.0, in1=preg_bc[:, cs],
                                                                            op0=ALU.add, op1=ALU.mult), reads=[r_tmp, r_bc2], writes=[g.r_mod])
        n = 0
        for part in range(2):
            col0 = (2 if part == 0 else 5) * D
            for s in range(2):
                for cb in range(2):
                    bank = 1 + (n % 2)
                    n += 1
                    pb = g.psum[bank]
                    for k in range(8):
                        kb.op("pe", lambda e, k=k, s=s, pb=pb, c0=col0 + cb * 512: e.matmul(
                            pb[:], lhsT=scb[:, k, s, :], rhs=MW[:, k, c0:c0 + 512], start=(k == 0), stop=(k == 7)),
                            reads=[r_MW[k], r_scb], writes=[g.rps[bank]], inc=(k == 7))
                    kb.op("dve", lambda e, pb=pb, part=part, cb=cb: e.tensor_tensor(
                        out=mtmp[:], in0=pb[:], in1=modb_bc[:, part, cb * 512:(cb + 1) * 512], op=ALU.add),
                        reads=[g.rps[bank], r_bc], writes=[r_tmp])
                    kb.op("dve", lambda e, part=part, s=s, cb=cb: e.tensor_tensor(
                        out=g.GR[:, part * 2 + s, cb * 512:(cb + 1) * 512], in0=mtmp[:],
                        in1=postg_bc[:, part, cb * 512:(cb + 1) * 512], op=ALU.mult),
                        reads=[r_tmp, r_bc], writes=[g.r_mod])


def rms_rows(g, xt, r_x, nsub, ss, rstd, r_ss, junk, width=D):
    kb = g.kb
    for s in range(nsub):
        kb.op("act", lambda e, s=s: e.activation(out=junk[:], in_=xt[:, s, :], func=AF.Square, accum_out=ss[:, s:s + 1]),
              reads=[r_x], writes=[r_ss])
    kb.op("act", lambda e: e.activation(out=rstd[:, :nsub], in_=ss[:, :nsub], func=AF.Sqrt, bias=EPS, scale=1.0 / width),
          reads=[r_ss], writes=[r_ss])
    kb.op("dve", lambda e: e.reciprocal(out=rstd[:, :nsub], in_=rstd[:, :nsub]), reads=[r_ss], writes=[r_ss])


def norm_transpose(g, xt, r_x, nsub, T, Gv, Sv, sel, hT, r_hT, xn, r_xn, ss, rstd, r_ss, junk, pt_banks):
    kb = g.kb
    rms_rows(g, xt, r_x, nsub, ss, rstd, r_ss, junk)
    for s in range(nsub):
        kb.op("dve", lambda e, s=s: e.tensor_scalar(out=xn[:, s, :], in0=xt[:, s, :], scalar1=rstd[:, s:s + 1], scalar2=None, op0=ALU.mult),
              reads=[r_x, r_ss], writes=[r_xn])
    for k in range(8):
        bank = pt_banks[k // 2]
        pv = g.psum[bank][:].bitcast(BF16)
        off = (k % 2) * 512
        for s in range(nsub):
            kb.op("pe", lambda e, k=k, s=s, pv=pv, off=off: e.transpose(pv[:, off + s * 128:off + (s + 1) * 128],
                                                                       xn[:, s, k * 128:(k + 1) * 128], g.ident[:]),
                  reads=[r_xn, g.r_ident], writes=[g.rps[bank]], inc=(s == nsub - 1))
        kb.op("act", lambda e, k=k, pv=pv, off=off: e.activation(out=hT[:, k, :T], in_=pv[:, off:off + T], func=AF.Identity,
                                                                 bias=Sv[:, k, sel:sel + 1], scale=Gv[:, k, sel:sel + 1]),
              reads=[g.rps[bank], g.r_mod], writes=[r_hT])


def phase_A(g, l, Xsrc, r_X):
    nc, kb, I, S = g.nc, g.kb, g.I, g.S
    with nc.sbuf_tensor(nm("WIN"), [128, 8, ZW], BF16) as WIN, \
            nc.sbuf_tensor(nm("XT"), [128, 1, 4, D], F32) as XT, \
            nc.sbuf_tensor(nm("xn"), [128, 4, D], BF16) as xn, \
            nc.sbuf_tensor(nm("junk"), [128, D], BF16) as junk, \
            nc.sbuf_tensor(nm("hT"), [128, 2, 8, 512], BF16) as hT, \
            nc.sbuf_tensor(nm("ss"), [128, 2, 4], F32) as ss, \
            nc.sbuf_tensor(nm("rstd"), [128, 2, 4], F32) as rstd, \
            nc.sbuf_tensor(nm("stg"), [128, 2, 1152], F32) as stg, \
            nc.sbuf_tensor(nm("zo"), [128, 12, 512], BF16) as zo:
        r_stg = [kb.res("stg") for _ in range(2)]
        cnt = [0]
        r_W = [kb.res("WIN") for _ in range(8)]
        r_XT = [kb.res("XT") for _ in range(1)]
        r_xn = kb.res("xn")
        r_hT = [kb.res("hT") for _ in range(2)]
        r_ss = [kb.res("ss") for _ in range(2)]
        r_zo = [kb.res("zo") for _ in range(12)]
        wv = I["w_in"][l].rearrange("(k p) n -> p k n", p=128)
        for k in range(8):
            load_w(g, WIN[:, k, :], wv[:, k, :], r_W[k], stg, r_stg, cnt)
        nz = 0
        for ti, (t0, T) in enumerate(TILES):
            sel = 1 if ti == 0 else 0
            nsub = T // 128
            b = ti % 2
            kb.dma("sp", XT[:, 0, :nsub, :], Xsrc[t0:t0 + T, :].rearrange("(s p) d -> p s d", p=128), reads=[r_X], writes=[r_XT[0]])
            norm_transpose(g, XT[:, 0], r_XT[0], nsub, T, g.G1, g.mv[:, 0:8, :], sel, hT[:, b], r_hT[b], xn, r_xn,
                           ss[:, b], rstd[:, b], r_ss[b], junk, [0, 1, 2, 3])
            for j in range(NCH):
                bank = 4 + (j % 4)
                pb = g.psum[bank]
                for k in range(8):
                    kb.op("pe", lambda e, j=j, k=k, pb=pb: e.matmul(pb[:, :T], lhsT=WIN[:, k, j * 128:(j + 1) * 128], rhs=hT[:, b, k, :T],
                                                                    start=(k == 0), stop=(k == 7)),
                          reads=[r_W[k], r_hT[b]], writes=[g.rps[bank]], inc=(k == 7))
                zi = nz % 12
                nz += 1
                if j >= 30:
                    kb.op("act", lambda e, pb=pb, zi=zi: e.activation(out=zo[:, zi, :T], in_=pb[:, :T], func=AF.Sigmoid),
                          reads=[g.rps[bank]], writes=[r_zo[zi]])
                else:
                    kb.op("dve", lambda e, pb=pb, zi=zi: e.tensor_copy(out=zo[:, zi, :T], in_=pb[:, :T]),
                          reads=[g.rps[bank]], writes=[r_zo[zi]])
                kb.dma("pool", S["Z"][j * 128:(j + 1) * 128, t0:t0 + T], zo[:, zi, :T], reads=[r_zo[zi]], writes=[g.RS["Z"]])


def phase_pool(g, l):
    nc, kb, I, S = g.nc, g.kb, g.I, g.S
    LPM = SEQ + 32
    with nc.sbuf_tensor(nm("ub"), [128, 2, LPM], BF16) as ub, \
            nc.sbuf_tensor(nm("T1"), [128, LPM], F32) as T1, \
            nc.sbuf_tensor(nm("T2"), [128, LPM], F32) as T2, \
            nc.sbuf_tensor(nm("M"), [128, 2, SEQ], BF16) as M, \
            nc.sbuf_tensor(nm("RC"), [128, SEQ], F32) as RC, \
            nc.sbuf_tensor(nm("PW"), [128, 4, 2, 256], BF16) as PW, \
            nc.sbuf_tensor(nm("pscol"), [128, 8], F32) as pscol, \
            nc.sbuf_tensor(nm("po"), [128, 8, 512], BF16) as po:
        r_ub = [kb.res("ub") for _ in range(2)]
        r_T1, r_T2, r_RC, r_PW, r_ps = (kb.res("p") for _ in range(5))
        r_M = [kb.res("M") for _ in range(2)]
        r_po = [kb.res("po") for _ in range(8)]
        kb.dma("pool", PW[:], I["pool_w"][l].rearrange("g (ic p) j -> p g ic j", p=128), reads=[g.r_in], writes=[r_PW])
        kb.dma("sp", pscol[:], I["vcol"][l, :, VI["pool_scale"], :], reads=[g.r_in], writes=[r_ps])
        npo = 0
        nb = 0
        for gi in range(4):
            w = 2 << gi
            hw = w // 2
            for (off, L) in ((0, CTX), (CTX, SEQ)):
                LP = L + 32
                kb.op("dve", lambda e: e.memset(RC[:, :L], 1.0 / w), writes=[r_RC])
                for t in range(hw):
                    kb.op("dve", lambda e, t=t: e.memset(RC[:, t:t + 1], 1.0 / (t + hw)), writes=[r_RC])
                for t in range(L - hw + 1, L):
                    kb.op("dve", lambda e, t=t: e.memset(RC[:, t:t + 1], 1.0 / (L - t + hw)), writes=[r_RC])
                for ch in range(2):
                    c = 2 * gi + ch
                    u = ub[:, ch, :]
                    kb.op("dve", lambda e, u=u: e.memset(u[:, 0:16], 0.0), writes=[r_ub[ch]])
                    kb.op("dve", lambda e, u=u: e.memset(u[:, 16 + L:32 + L], 0.0), writes=[r_ub[ch]])
                    kb.dma("sp", u[:, 16:16 + L], S["Z"][c * 128:(c + 1) * 128, off:off + L], reads=[g.RS["Z"]], writes=[r_ub[ch]])
                    kb.op("dve", lambda e, u=u: e.tensor_tensor(out=T1[:, 1:LP], in0=u[:, 0:LP - 1], in1=u[:, 1:LP], op=ALU.add),
                          reads=[r_ub[ch]], writes=[r_T1])
                    Sb, rS, Ob, rO = T1, r_T1, T2, r_T2
                    if w >= 4:
                        kb.op("dve", lambda e: e.tensor_tensor(out=T2[:, 2:LP - 1], in0=T1[:, 1:LP - 2], in1=T1[:, 3:LP], op=ALU.add),
                              reads=[r_T1], writes=[r_T2])
                        Sb, rS, Ob, rO = T2, r_T2, T1, r_T1
                    if w >= 8:
                        kb.op("dve", lambda e: e.tensor_tensor(out=T1[:, 4:LP - 3], in0=T2[:, 2:LP - 5], in1=T2[:, 6:LP - 1], op=ALU.add),
                              reads=[r_T2], writes=[r_T1])
                        Sb, rS, Ob, rO = T1, r_T1, T2, r_T2
                    if w >= 16:
                        kb.op("dve", lambda e: e.tensor_tensor(out=T2[:, 8:LP - 7], in0=T1[:, 4:LP - 11], in1=T1[:, 12:LP - 3], op=ALU.add),
                              reads=[r_T1], writes=[r_T2])
                        Sb, rS, Ob, rO = T2, r_T2, T1, r_T1
                    kb.op("dve", lambda e, Sb=Sb, Ob=Ob: e.tensor_tensor(out=Ob[:, 16:16 + L], in0=Sb[:, 16:16 + L], in1=RC[:, :L], op=ALU.mult),
                          reads=[rS, r_RC], writes=[rO])
                    kb.op("dve", lambda e, Ob=Ob, u=u, ch=ch: e.tensor_tensor(out=M[:, ch, :L], in0=Ob[:, 16:16 + L], in1=u[:, 16:16 + L], op=ALU.subtract),
                          reads=[rO, r_ub[ch]], writes=[r_M[ch]])
                for t0 in range(0, L, 512):
                    T = min(512, L - t0)
                    for jc in range(2):
                        bank = nb % 8
                        nb += 1
                        pb = g.psum[bank]
                        for ic in range(2):
                            kb.op("pe", lambda e, ic=ic, jc=jc, pb=pb, t0=t0, T=T: e.matmul(
                                pb[:, :T], lhsT=PW[:, gi, ic, jc * 128:(jc + 1) * 128], rhs=M[:, ic, t0:t0 + T], start=(ic == 0), stop=(ic == 1)),
                                reads=[r_PW, r_M[ic]], writes=[g.rps[bank]], inc=(ic == 1))
                        pi = npo % 8
                        npo += 1
                        c = 2 * gi + jc
                        kb.op("act", lambda e, pb=pb, pi=pi, T=T, c=c: e.activation(out=po[:, pi, :T], in_=pb[:, :T], func=AF.Identity,
                                                                                   scale=pscol[:, c:c + 1]),
                              reads=[g.rps[bank], r_ps], writes=[r_po[pi]])
                        kb.dma("pool", S["MP"][c * 128:(c + 1) * 128, off + t0:off + t0 + T], po[:, pi, :T], reads=[r_po[pi]], writes=[g.RS["MP"]])


def phase_lru(g, l):
    nc, kb, I, S = g.nc, g.kb, g.I, g.S
    with nc.sbuf_tensor(nm("LXG"), [128, LT], BF16) as LXG, \
            nc.sbuf_tensor(nm("UB"), [128, LT], BF16) as UB, \
            nc.sbuf_tensor(nm("T1"), [128, LT], F32) as T1, \
            nc.sbuf_tensor(nm("T2"), [128, LT], F32) as T2, \
            nc.sbuf_tensor(nm("T3"), [128, LT], F32) as T3, \
            nc.sbuf_tensor(nm("T4"), [128, LT], F32) as T4, \
            nc.sbuf_tensor(nm("GA"), [128, 2, 8, 128], BF16) as GA, \
            nc.sbuf_tensor(nm("GX"), [128, 2, 8, 128], BF16) as GX, \
            nc.sbuf_tensor(nm("vc"), [128, NV, 8], F32) as vc, \
            nc.sbuf_tensor(nm("cn"), [128, 2, 2, 8], F32) as cn:
        r_LXG, r_UB, r_T1, r_T2, r_T3, r_T4, r_GA, r_vc, r_cn = (kb.res("l") for _ in range(9))
        kb.dma("pool", GA[:], I["gate_a_w"][l].rearrange("d c j k -> j d c k"), reads=[g.r_in], writes=[r_GA])
        kb.dma("pool", GX[:], I["gate_x_w"][l].rearrange("d c j k -> j d c k"), reads=[g.r_in], writes=[r_GA])
        kb.dma("sp", vc[:], I["vcol"][l], reads=[g.r_in], writes=[r_vc])
        for d in range(2):
            lam = vc[:, VI["lru_lambda%d" % d], :]
            kb.op("act", lambda e, d=d, lam=lam: e.activation(out=cn[:, 0, d, :], in_=lam, func=AF.Exp, scale=-1.0), reads=[r_vc], writes=[r_cn])
            kb.op("act", lambda e, d=d: e.activation(out=cn[:, 0, d, :], in_=cn[:, 0, d, :], func=AF.Ln, bias=1.0), reads=[r_cn], writes=[r_cn])
            kb.op("dve", lambda e, d=d: e.tensor_scalar(out=cn[:, 1, d, :], in0=cn[:, 0, d, :], scalar1=-16.0, scalar2=None, op0=ALU.mult),
                  reads=[r_cn], writes=[r_cn])
            kb.op("dve", lambda e, d=d: e.tensor_scalar(out=cn[:, 0, d, :], in0=cn[:, 0, d, :], scalar1=-8.0, scalar2=None, op0=ALU.mult),
                  reads=[r_cn], writes=[r_cn])
        segs = ((0, CTX), (CTX, LT))
        nb = 0
        for c in range(8):
            def col(name):
                return vc[:, VI[name], c:c + 1]
            kb.dma("sp", LXG[:], S["Z"][(8 + c) * 128:(9 + c) * 128, :], reads=[g.RS["Z"]], writes=[r_LXG])
            for (s0, s1) in segs:
                kb.op("dve", lambda e, s0=s0, s1=s1: e.tensor_scalar(out=T1[:, s0:s1], in0=LXG[:, s0:s1], scalar1=col("conv_w2"), scalar2=col("conv_b"),
                                                                    op0=ALU.mult, op1=ALU.add), reads=[r_LXG, r_vc], writes=[r_T1])
                for k, o in ((0, -2), (1, -1), (3, 1)):
                    a, b = max(s0, s0 - o), min(s1, s1 - o)
                    kb.op("dve", lambda e, a=a, b=b, o=o, k=k: e.scalar_tensor_tensor(out=T1[:, a:b], in0=LXG[:, a + o:b + o], scalar=col("conv_w%d" % k),
                                                                                    in1=T1[:, a:b], op0=ALU.mult, op1=ALU.add),
                          reads=[r_LXG, r_vc, r_T1], writes=[r_T1])
            kb.op("act", lambda e: e.activation(out=UB[:], in_=T1[:], func=AF.Copy), reads=[r_T1], writes=[r_UB])
            kb.dma("sp", LXG[:], S["Z"][(16 + c) * 128:(17 + c) * 128, :], reads=[g.RS["Z"]], writes=[r_LXG])
            for d in range(2):
                Rb, rR = T1, r_T1
                Ib, rI = (T2, r_T2) if d == 0 else (T4, r_T4)
                Ab, rA = T3, r_T3
                for (t0, T) in TILES:
                    b0, b1 = nb % 8, (nb + 1) % 8
                    nb += 2
                    kb.op("pe", lambda e, t0=t0, T=T, b0=b0: e.matmul(g.psum[b0][:, :T], lhsT=GA[:, d, c, :], rhs=UB[:, t0:t0 + T], start=True, stop=True),
                          reads=[r_GA, r_UB], writes=[g.rps[b0]])
                    kb.op("pe", lambda e, t0=t0, T=T, b1=b1: e.matmul(g.psum[b1][:, :T], lhsT=GX[:, d, c, :], rhs=UB[:, t0:t0 + T], start=True, stop=True),
                          reads=[r_GA, r_UB], writes=[g.rps[b1]])
                    kb.op("act", lambda e, t0=t0, T=T, b0=b0, Rb=Rb: e.activation(out=Rb[:, t0:t0 + T], in_=g.psum[b0][:, :T], func=AF.Sigmoid,
                                                                                 bias=col("gate_a_b%d" % d)), reads=[g.rps[b0], r_vc], writes=[rR])
                    kb.op("act", lambda e, t0=t0, T=T, b1=b1, Ib=Ib: e.activation(out=Ib[:, t0:t0 + T], in_=g.psum[b1][:, :T], func=AF.Sigmoid,
                                                                                 bias=col("gate_x_b%d" % d)), reads=[g.rps[b1], r_vc], writes=[rI])
                kb.op("act", lambda e, Ab=Ab, Rb=Rb: e.activation(out=Ab[:], in_=Rb[:], func=AF.Exp, scale=cn[:, 0, d, c:c + 1]), reads=[rR, r_cn], writes=[rA])
                kb.op("act", lambda e, Rb=Rb: e.activation(out=Rb[:], in_=Rb[:], func=AF.Exp, scale=cn[:, 1, d, c:c + 1]), reads=[rR, r_cn], writes=[rR])
                kb.op("act", lambda e, Rb=Rb: e.activation(out=Rb[:], in_=Rb[:], func=AF.Sqrt, bias=1.0, scale=-1.0), reads=[rR], writes=[rR])
                kb.op("dve", lambda e, Rb=Rb, Ib=Ib: e.tensor_tensor(out=Rb[:], in0=Rb[:], in1=Ib[:], op=ALU.mult), reads=[rR, rI], writes=[rR])
                kb.op("dve", lambda e, Rb=Rb: e.tensor_tensor(out=Rb[:], in0=Rb[:], in1=UB[:], op=ALU.mult), reads=[rR, r_UB], writes=[rR])
                if d == 0:
                    kb.op("dve", lambda e, Ab=Ab, Rb=Rb, Ib=Ib: e.tensor_tensor_scan(out=Ib[:, 0:CTX], data0=Ab[:, 0:CTX], data1=Rb[:, 0:CTX], initial=0.0,
                                                                                   op0=ALU.mult, op1=ALU.add), reads=[rA, rR], writes=[rI])
                    kb.op("dve", lambda e, Ab=Ab, Rb=Rb, Ib=Ib: e.tensor_tensor_scan(out=Ib[:, CTX:LT], data0=Ab[:, CTX:LT], data1=Rb[:, CTX:LT],
                                                                                   initial=Ib[:, CTX - 1:CTX], op0=ALU.mult, op1=ALU.add),
                          reads=[rA, rR, rI], writes=[rI])
                else:
                    kb.op("dve", lambda e, Ab=Ab, Rb=Rb, Ib=Ib: e.tensor_tensor_scan(out=Ib[:, CTX - 1::-1], data0=Ab[:, CTX - 1::-1], data1=Rb[:, CTX - 1::-1],
                                                                                   initial=0.0, op0=ALU.mult, op1=ALU.add), reads=[rA, rR], writes=[rI])
                    kb.op("dve", lambda e, Ab=Ab, Rb=Rb, Ib=Ib: e.tensor_tensor_scan(out=Ib[:, LT - 1:CTX - 1:-1], data0=Ab[:, LT - 1:CTX - 1:-1],
                                                                                   data1=Rb[:, LT - 1:CTX - 1:-1], initial=Ib[:, 0:1],
                                                                                   op0=ALU.mult, op1=ALU.add), reads=[rA, rR, rI], writes=[rI])
            kb.op("dve", lambda e: e.tensor_tensor(out=T2[:], in0=T2[:], in1=T4[:], op=ALU.add), reads=[r_T2, r_T4], writes=[r_T2])
            kb.op("dve", lambda e: e.tensor_tensor(out=T1[:], in0=LXG[:], in1=LXG[:], op=ALU.mult), reads=[r_LXG], writes=[r_T1])
            kb.op("dve", lambda e: e.tensor_scalar(out=T1[:], in0=T1[:], scalar1=0.044715, scalar2=1.0, op0=ALU.mult, op1=ALU.add), reads=[r_T1], writes=[r_T1])
            kb.op("dve", lambda e: e.tensor_tensor(out=T1[:], in0=T1[:], in1=LXG[:], op=ALU.mult), reads=[r_T1, r_LXG], writes=[r_T1])
            kb.op("act", lambda e: e.activation(out=T1[:], in_=T1[:], func=AF.Sigmoid, scale=1.5957691216057308), reads=[r_T1], writes=[r_T1])
            kb.op("dve", lambda e: e.tensor_tensor(out=T1[:], in0=T1[:], in1=LXG[:], op=ALU.mult), reads=[r_T1, r_LXG], writes=[r_T1])
            kb.op("dve", lambda e: e.tensor_tensor(out=UB[:], in0=T1[:], in1=T2[:], op=ALU.mult), reads=[r_T1, r_T2], writes=[r_UB])
            kb.dma("pool", S["YL"][c * 128:(c + 1) * 128, :], UB[:], reads=[r_UB], writes=[g.RS["YL"]])


def phase_sel(g):
    nc, kb, I, S = g.nc, g.kb, g.I, g.S
    with nc.sbuf_tensor(nm("hm"), [128, 2], F32) as hm, \
            nc.sbuf_tensor(nm("sa"), [128, 2, HALF], BF16) as sa, \
            nc.sbuf_tensor(nm("sbb"), [128, 2, HALF], BF16) as sbb, \
            nc.sbuf_tensor(nm("so"), [128, 2, HALF], BF16) as so, \
            nc.sbuf_tensor(nm("xa"), [128, 2, 4, D], F32) as xa, \
            nc.sbuf_tensor(nm("xb"), [128, 2, 4, D], F32) as xb, \
            nc.sbuf_tensor(nm("xo"), [128, 2, 4, D], F32) as xo:
        r_hm = kb.res("hm")
        r_sa = [kb.res("sa") for _ in range(2)]
        r_sb = [kb.res("sb") for _ in range(2)]
        r_so = [kb.res("so") for _ in range(2)]
        kb.dma("sp", hm[:], I["hm"], reads=[g.r_in], writes=[r_hm])
        n = 0
        for src, r_src, dst, nrows in ((S["Z"][24 * 128:27 * 128, :], g.RS["Z"], "CQl", 384), (S["Z"][30 * 128:54 * 128, :], g.RS["Z"], "Gl", 3072),
                                      (S["MP"], g.RS["MP"], "MPl", D), (S["YL"], g.RS["YL"], "YLl", D)):
            for rc in range(nrows // 128):
                b = n % 2
                n += 1
                rows = slice(rc * 128, (rc + 1) * 128)
                kb.dma("sp", sa[:, b, :], src[rows, CTX:CTX + HALF], reads=[r_src], writes=[r_sa[b]])
                kb.dma("sp", sbb[:, b, :], src[rows, CTX + HALF:LT], reads=[r_src], writes=[r_sb[b]])
                kb.op("dve", lambda e, b=b: e.tensor_scalar(out=so[:, b, :], in0=sa[:, b, :], scalar1=hm[:, 0:1], scalar2=None, op0=ALU.mult),
                      reads=[r_sa[b], r_hm], writes=[r_so[b]])
                kb.op("dve", lambda e, b=b: e.scalar_tensor_tensor(out=so[:, b, :], in0=sbb[:, b, :], scalar=hm[:, 1:2], in1=so[:, b, :],
                                                                  op0=ALU.mult, op1=ALU.add), reads=[r_sb[b], r_hm, r_so[b]], writes=[r_so[b]])
                kb.dma("pool", S[dst][rows, :], so[:, b, :], reads=[r_so[b]], writes=[g.RS[dst]])
        for i, (t0, T) in enumerate(LTILES):
            b = i % 2
            kb.dma("sp", xa[:, b], S["X2"][CTX + t0:CTX + t0 + T, :].rearrange("(s p) d -> p s d", p=128), reads=[g.RS["X2"]], writes=[r_sa[b]])
            kb.dma("sp", xb[:, b], S["X2"][CTX + HALF + t0:CTX + HALF + t0 + T, :].rearrange("(s p) d -> p s d", p=128), reads=[g.RS["X2"]], writes=[r_sb[b]])
            kb.op("dve", lambda e, b=b: e.tensor_scalar(out=xo[:, b], in0=xa[:, b], scalar1=hm[:, 0:1], scalar2=None, op0=ALU.mult),
                  reads=[r_sa[b], r_hm], writes=[r_so[b]])
            kb.op("dve", lambda e, b=b: e.scalar_tensor_tensor(out=xo[:, b], in0=xb[:, b], scalar=hm[:, 1:2], in1=xo[:, b],
                                                              op0=ALU.mult, op1=ALU.add), reads=[r_sb[b], r_hm, r_so[b]], writes=[r_so[b]])
            kb.dma("pool", S["Xl"][t0:t0 + T, :].rearrange("(s p) d -> p s d", p=128), xo[:, b], reads=[r_so[b]], writes=[g.RS["Xl"]])


def rms_feat(g, src, nk, t0, T, width, gcol, r_g, sq, r_sq, rb, r_rb, cnm, r_cn, r_src, bank):
    kb = g.kb
    for k in range(nk):
        kb.op("dve", lambda e, k=k: e.tensor_tensor(out=sq[:, k, :T], in0=src[:, k, t0:t0 + T], in1=src[:, k, t0:t0 + T], op=ALU.mult),
              reads=[r_src], writes=[r_sq])
    pb = g.psum[bank]
    for k in range(nk):
        kb.op("pe", lambda e, k=k: e.matmul(pb[:, :T], lhsT=g.ones[:], rhs=sq[:, k, :T], start=(k == 0), stop=(k == nk - 1)),
              reads=[g.r_ones, r_sq], writes=[g.rps[bank]], inc=(k == nk - 1))
    kb.op("act", lambda e: e.activation(out=rb[:, :T], in_=pb[:, :T], func=AF.Sqrt, bias=EPS, scale=1.0 / width), reads=[g.rps[bank]], writes=[r_rb])
    kb.op("dve", lambda e: e.reciprocal(out=rb[:, :T], in_=rb[:, :T]), reads=[r_rb], writes=[r_rb])
    for k in range(nk):
        kb.op("dve", lambda e, k=k: e.scalar_tensor_tensor(out=cnm[:, k, :T], in0=src[:, k, t0:t0 + T], scalar=gcol[:, k:k + 1], in1=rb[:, :T],
                                                          op0=ALU.mult, op1=ALU.mult), reads=[r_src, r_rb, r_g], writes=[r_cn])


def phase_kv(g, l):
    nc, kb, I, S = g.nc, g.kb, g.I, g.S
    with nc.sbuf_tensor(nm("CKV"), [128, 2, LT], BF16) as CKV, \
            nc.sbuf_tensor(nm("KRa"), [64, LT], BF16) as KRa, \
            nc.sbuf_tensor(nm("KRb"), [64, LT], BF16) as KRb, \
            nc.sbuf_tensor(nm("COS"), [64, SEQ], F32) as COS, \
            nc.sbuf_tensor(nm("SIN"), [64, SEQ], F32) as SIN, \
            nc.sbuf_tensor(nm("WUKV"), [128, 2, 2048], BF16) as WUKV, \
            nc.sbuf_tensor(nm("kvg"), [128, 2], F32) as kvg, \
            nc.sbuf_tensor(nm("sq"), [128, 2, 2, 512], BF16) as sq, \
            nc.sbuf_tensor(nm("rb"), [128, 2, 512], F32) as rb, \
            nc.sbuf_tensor(nm("cnm"), [128, 2, 2, 512], BF16) as cnm_all, \
            nc.sbuf_tensor(nm("ko"), [128, 8, 512], BF16) as ko, \
            nc.sbuf_tensor(nm("vo"), [128, 2, D], BF16) as vo, \
            nc.sbuf_tensor(nm("r1"), [64, 512], F32) as r1, \
            nc.sbuf_tensor(nm("r2"), [64, 512], F32) as r2, \
            nc.sbuf_tensor(nm("kro"), [64, 2, 512], BF16) as kro:
        r_CKV, r_KR, r_tab, r_W, r_g, r_r1, r_r2 = (kb.res("k") for _ in range(7))
        r_sq2, r_rb2, r_cn2 = ([kb.res("k") for _ in range(2)] for _ in range(3))
        r_ko = [kb.res("ko") for _ in range(8)]
        r_vo = [kb.res("vo") for _ in range(2)]
        r_kro = [kb.res("kro") for _ in range(2)]
        kb.dma("sp", CKV[:], S["Z"][27 * 128:29 * 128, :].rearrange("(k p) t -> p k t", p=128), reads=[g.RS["Z"]], writes=[r_CKV])
        kb.dma("sp", KRa[:], S["Z"][29 * 128:29 * 128 + 64, :], reads=[g.RS["Z"]], writes=[r_KR])
        kb.dma("sp", KRb[:], S["Z"][29 * 128 + 64:30 * 128, :], reads=[g.RS["Z"]], writes=[r_KR])
        kb.dma("sp", COS[:], I["cosT"], reads=[g.r_in], writes=[r_tab])
        kb.dma("sp", SIN[:], I["sinT"], reads=[g.r_in], writes=[r_tab])
        kb.dma("pool", WUKV[:], I["w_ukv"][l].rearrange("(k p) n -> p k n", p=128), reads=[g.r_in], writes=[r_W])
        kb.dma("sp", kvg[:], I["kvg_col"][l], reads=[g.r_in], writes=[r_g])
        nko = nvo = 0
        for ti, (t0, T) in enumerate(TILES):
            pb_ = ti % 2
            cnm, r_cn = cnm_all[:, pb_], r_cn2[pb_]
            rms_feat(g, CKV, 2, t0, T, 256.0, kvg, r_g, sq[:, pb_], r_sq2[pb_], rb[:, pb_], r_rb2[pb_], cnm, r_cn, r_CKV, 0)
            for h in range(8):
                bank = 1 + (h % 3)
                pb = g.psum[bank]
                for k in range(2):
                    kb.op("pe", lambda e, k=k, h=h, pb=pb: e.matmul(pb[:, :T], lhsT=WUKV[:, k, h * 128:(h + 1) * 128], rhs=cnm[:, k, :T],
                                                                    start=(k == 0), stop=(k == 1)), reads=[r_W, r_cn], writes=[g.rps[bank]], inc=(k == 1))
                ki = nko % 8
                nko += 1
                if h % 2 == 0:
                    kb.op("act", lambda e, pb=pb, ki=ki: e.activation(out=ko[:, ki, :T], in_=pb[:, :T], func=AF.Copy), reads=[g.rps[bank]], writes=[r_ko[ki]])
                else:
                    kb.op("dve", lambda e, pb=pb, ki=ki: e.tensor_copy(out=ko[:, ki, :T], in_=pb[:, :T]), reads=[g.rps[bank]], writes=[r_ko[ki]])
                kb.dma("pool", S["KN"][h, :, t0:t0 + T], ko[:, ki, :T], reads=[r_ko[ki]], writes=[g.RS["KN"]])
            for sub in range(T // 128):
                vi = nvo % 2
                nvo += 1
                for half in range(2):
                    bank = 4 + half
                    pb = g.psum[bank]
                    for k in range(2):
                        kb.op("pe", lambda e, k=k, half=half, pb=pb, sub=sub: e.matmul(
                            pb[:], lhsT=cnm[:, k, sub * 128:(sub + 1) * 128], rhs=WUKV[:, k, 1024 + half * 512:1536 + half * 512],
                            start=(k == 0), stop=(k == 1)), reads=[r_W, r_cn], writes=[g.rps[bank]], inc=(k == 1))
                    if half == 0:
                        kb.op("act", lambda e, pb=pb, vi=vi: e.activation(out=vo[:, vi, 0:512], in_=pb[:], func=AF.Copy), reads=[g.rps[bank]], writes=[r_vo[vi]])
                    else:
                        kb.op("dve", lambda e, pb=pb, vi=vi: e.tensor_copy(out=vo[:, vi, 512:1024], in_=pb[:]), reads=[g.rps[bank]], writes=[r_vo[vi]])
                kb.dma("pool", S["V"][t0 + sub * 128:t0 + (sub + 1) * 128, :], vo[:, vi, :], reads=[r_vo[vi]], writes=[g.RS["V"]])
            oi = ti % 2
            if ti == 0:
                kb.op("dve", lambda e, oi=oi: e.tensor_copy(out=kro[:, oi, :T], in_=KRa[:, t0:t0 + T]), reads=[r_KR], writes=[r_kro[oi]])
            else:
                q0 = t0 - CTX
                kb.op("dve", lambda e: e.tensor_tensor(out=r1[:, :T], in0=KRa[:, t0:t0 + T], in1=COS[:, q0:q0 + T], op=ALU.mult), reads=[r_KR, r_tab], writes=[r_r1])
                kb.op("dve", lambda e: e.tensor_tensor(out=r2[:, :T], in0=KRb[:, t0:t0 + T], in1=SIN[:, q0:q0 + T], op=ALU.mult), reads=[r_KR, r_tab], writes=[r_r2])
                kb.op("dve", lambda e, oi=oi: e.tensor_tensor(out=kro[:, oi, :T], in0=r1[:, :T], in1=r2[:, :T], op=ALU.add), reads=[r_r1, r_r2], writes=[r_kro[oi]])
            kb.dma("pool", S["KRD"][:, t0:t0 + T], kro[:, oi, :T], reads=[r_kro[oi]], writes=[g.RS["KRD"]])


def phase_q(g, l, loc=False):
    nc, kb, I, S = g.nc, g.kb, g.I, g.S
    NT = HALF if loc else LT
    NR = HALF if loc else SEQ
    tiles = [(t0, T, False) for (t0, T) in LTILES] if loc else [(t0, T, t0 == 0) for (t0, T) in TILES]
    qoff = 0 if loc else CTX
    cq_src, r_cq = (S["CQl"], g.RS["CQl"]) if loc else (S["Z"][24 * 128:27 * 128, :], g.RS["Z"])
    cos_src, sin_src = (I["cosQ"], I["sinQ"]) if loc else (I["cosT"], I["sinT"])
    QN_dst, QR_dst = ("QNl", "QRl") if loc else ("QN", "QR")
    with nc.sbuf_tensor(nm("CQ"), [128, 3, NT], BF16) as CQ, \
            nc.sbuf_tensor(nm("COS"), [64, NR], F32) as COS, \
            nc.sbuf_tensor(nm("SIN"), [64, NR], F32) as SIN, \
            nc.sbuf_tensor(nm("WUQ"), [128, 3, 2048], BF16) as WUQ, \
            nc.sbuf_tensor(nm("qg"), [128, 3], F32) as qg, \
            nc.sbuf_tensor(nm("sq"), [128, 2, 3, 512], BF16) as sq, \
            nc.sbuf_tensor(nm("rb"), [128, 2, 512], F32) as rb, \
            nc.sbuf_tensor(nm("cnm"), [128, 2, 3, 512], BF16) as cnm_all, \
            nc.sbuf_tensor(nm("qo"), [128, 8, 512], BF16) as qo, \
            nc.sbuf_tensor(nm("r1"), [64, 512], F32) as r1, \
            nc.sbuf_tensor(nm("r2"), [64, 512], F32) as r2, \
            nc.sbuf_tensor(nm("qro"), [64, 8, 512], BF16) as qro:
        r_CQ, r_tab, r_W, r_g, r_r1, r_r2 = (kb.res("q") for _ in range(6))
        r_sq2, r_rb2, r_cn2 = ([kb.res("q") for _ in range(2)] for _ in range(3))
        r_qo = [kb.res("qo") for _ in range(8)]
        r_qro = [kb.res("qro") for _ in range(8)]
        kb.dma("sp", CQ[:], cq_src.rearrange("(k p) t -> p k t", p=128), reads=[r_cq], writes=[r_CQ])
        kb.dma("sp", COS[:], cos_src, reads=[g.r_in], writes=[r_tab])
        kb.dma("sp", SIN[:], sin_src, reads=[g.r_in], writes=[r_tab])
        kb.dma("pool", WUQ[:], I["w_uq"][l].rearrange("(k p) n -> p k n", p=128), reads=[g.r_in], writes=[r_W])
        kb.dma("sp", qg[:], I["qg_col"][l], reads=[g.r_in], writes=[r_g])
        nq = 0
        for ti, (t0, T, isctx) in enumerate(tiles):
            pb_ = ti % 2
            cnm, r_cn = cnm_all[:, pb_], r_cn2[pb_]
            rms_feat(g, CQ, 3, t0, T, 384.0, qg, r_g, sq[:, pb_], r_sq2[pb_], rb[:, pb_], r_rb2[pb_], cnm, r_cn, r_CQ, 0)
            for h in range(8):
                qi = nq % 8
                nq += 1
                bn, br, bp = 1 + 3 * (h % 2), 2 + 3 * (h % 2), 3 + 3 * (h % 2)
                for k in range(3):
                    kb.op("pe", lambda e, k=k, h=h: e.matmul(g.psum[bn][:, :T], lhsT=WUQ[:, k, h * 256:h * 256 + 128], rhs=cnm[:, k, :T],
                                                             start=(k == 0), stop=(k == 2)), reads=[r_W, r_cn], writes=[g.rps[bn]], inc=(k == 2))
                for k in range(3):
                    kb.op("pe", lambda e, k=k, h=h: e.matmul(g.psum[br][0:64, :T], lhsT=WUQ[:, k, h * 256 + 128:h * 256 + 192], rhs=cnm[:, k, :T],
                                                             start=(k == 0), stop=(k == 2)), reads=[r_W, r_cn], writes=[g.rps[br]], inc=(k == 2))
                kb.op("act", lambda e, qi=qi: e.activation(out=qo[:, qi, :T], in_=g.psum[bn][:, :T], func=AF.Copy, scale=MLA_SCALE),
                      reads=[g.rps[bn]], writes=[r_qo[qi]])
                kb.dma("pool", S[QN_dst][h, :, t0:t0 + T], qo[:, qi, :T], reads=[r_qo[qi]], writes=[g.RS[QN_dst]])
                if isctx:
                    kb.op("act", lambda e, qi=qi: e.activation(out=qro[:, qi, :T], in_=g.psum[br][0:64, :T], func=AF.Copy, scale=MLA_SCALE),
                          reads=[g.rps[br]], writes=[r_qro[qi]])
                else:
                    q0 = t0 - qoff
                    for k in range(3):
                        kb.op("pe", lambda e, k=k, h=h: e.matmul(g.psum[bp][0:64, :T], lhsT=WUQ[:, k, h * 256 + 192:h * 256 + 256], rhs=cnm[:, k, :T],
                                                                 start=(k == 0), stop=(k == 2)), reads=[r_W, r_cn], writes=[g.rps[bp]], inc=(k == 2))
                    kb.op("dve", lambda e: e.tensor_tensor(out=r1[:, :T], in0=g.psum[br][0:64, :T], in1=COS[:, q0:q0 + T], op=ALU.mult),
                          reads=[g.rps[br], r_tab], writes=[r_r1])
                    kb.op("dve", lambda e: e.scalar_tensor_tensor(out=r2[:, :T], in0=g.psum[bp][0:64, :T], scalar=MLA_SCALE, in1=SIN[:, q0:q0 + T],
                                                                 op0=ALU.mult, op1=ALU.mult), reads=[g.rps[bp], r_tab], writes=[r_r2])
                    kb.op("dve", lambda e, qi=qi: e.scalar_tensor_tensor(out=qro[:, qi, :T], in0=r1[:, :T], scalar=MLA_SCALE, in1=r2[:, :T],
                                                                        op0=ALU.mult, op1=ALU.add), reads=[r_r1, r_r2], writes=[r_qro[qi]])
                kb.dma("pool", S[QR_dst][h, :, t0:t0 + T], qro[:, qi, :T], reads=[r_qro[qi]], writes=[g.RS[QR_dst]])


def phase_att(g, l, loc=False, att_heads=8):
    nc, kb, I, S = g.nc, g.kb, g.I, g.S
    NKT = LT // 128
    tiles = [(t0, T, False) for (t0, T) in LTILES] if loc else [(t0, T, t0 == 0) for (t0, T) in TILES]
    QN_src, QR_src, AT_dst = ("QNl", "QRl", "ATl") if loc else ("QN", "QR", "AT")
    with nc.sbuf_tensor(nm("KNh"), [128, LT], BF16) as KNh, \
            nc.sbuf_tensor(nm("KRD"), [128, LT], BF16) as KRD, \
            nc.sbuf_tensor(nm("Vh"), [128, NKT, 128], BF16) as Vh, \
            nc.sbuf_tensor(nm("QNb"), [128, 2, 512], BF16) as QNb, \
            nc.sbuf_tensor(nm("QRb"), [128, 2, 512], BF16) as QRb, \
            nc.sbuf_tensor(nm("PT"), [128, 8, 512], BF16) as PT, \
            nc.sbuf_tensor(nm("rl"), [128, 512], F32) as rl, \
            nc.sbuf_tensor(nm("ahl"), [128, 2, 512], BF16) as ahl, \
            nc.sbuf_tensor(nm("atmp"), [128, 512], F32) as atmp, \
            nc.sbuf_tensor(nm("ob"), [128, 2, 512], BF16) as ob:
        r_KN, r_KR, r_V, r_rl, r_ahl, r_atmp = (kb.res("a") for _ in range(6))
        r_Q = [kb.res("Q") for _ in range(2)]
        r_PT = [kb.res("PT") for _ in range(8)]
        r_ob = [kb.res("ob") for _ in range(2)]
        kb.dma("sp", KRD[0:64, :], S["KRD"], reads=[g.RS["KRD"]], writes=[r_KR])
        kb.dma("sp", KRD[64:128, :], S["KRD"], reads=[g.RS["KRD"]], writes=[r_KR])
        bLp, bLr = 6, 7
        nq = 0
        for h in range(att_heads):
            kb.dma("sp", KNh[:], S["KN"][h], reads=[g.RS["KN"]], writes=[r_KN])
            kb.dma("sp", Vh[:], S["V"][:, h * 128:(h + 1) * 128].rearrange("(kt p) d -> p kt d", p=128), reads=[g.RS["V"]], writes=[r_V])
            for ti, (t0, T, isctx) in enumerate(tiles):
                nk = 2 if isctx else NKT
                npair = nk // 2
                ngrp = (nk + 3) // 4
                qi = nq % 2
                nq += 1
                bO = 4 + qi
                kb.dma("sp", QNb[:, qi, :T], S[QN_src][h, :, t0:t0 + T], reads=[g.RS[QN_src]], writes=[r_Q[qi]])
                kb.dma("sp", QRb[0:64, qi, :T], S[QR_src][h, :, t0:t0 + T], reads=[g.RS[QR_src]], writes=[r_Q[qi]])
                kb.dma("sp", QRb[64:128, qi, :T], S[QR_src][h, :, t0:t0 + T], reads=[g.RS[QR_src]], writes=[r_Q[qi]])

                def emit_S_pair(p):
                    k0, k1 = 2 * p, 2 * p + 1
                    b0, b1 = k0 % 4, k1 % 4
                    kb.op("pe", lambda e: e.matmul(g.psum[b0][:, :T], lhsT=KNh[:, k0 * 128:(k0 + 1) * 128], rhs=QNb[:, qi, :T], start=True, stop=False),
                          reads=[r_KN, r_Q[qi]], writes=[g.rps[b0]], inc=False)
                    kb.op("pe", lambda e: e.matmul(g.psum[b1][:, :T], lhsT=KNh[:, k1 * 128:(k1 + 1) * 128], rhs=QNb[:, qi, :T], start=True, stop=False),
                          reads=[r_KN, r_Q[qi]], writes=[g.rps[b1]], inc=False)
                    kb.op("pe", lambda e: e.matmul(g.psum[b0][:, :T], lhsT=KRD[0:64, k0 * 128:(k0 + 1) * 128], rhs=QRb[0:64, qi, :T], start=False, stop=True,
                                                   tile_position=(0, 0)), reads=[r_KR, r_Q[qi]], writes=[g.rps[b0]], inc=False)
                    kb.op("pe", lambda e: e.matmul(g.psum[b1][:, :T], lhsT=KRD[64:128, k1 * 128:(k1 + 1) * 128], rhs=QRb[64:128, qi, :T], start=False, stop=True,
                                                   tile_position=(64, 0)), reads=[r_KR, r_Q[qi]], writes=[g.rps[b1], g.rps[b0]])

                emit_S_pair(0)
                for p in range(npair):
                    for kt in (2 * p, 2 * p + 1):
                        kb.op("act", lambda e, kt=kt: e.activation(out=PT[:, kt % 8, :T], in_=g.psum[kt % 4][:, :T], func=AF.Exp),
                              reads=[g.rps[kt % 4]], writes=[r_PT[kt % 8]])
                    if p + 1 < npair:
                        emit_S_pair(p + 1)
                    for kt in (2 * p, 2 * p + 1):
                        kb.op("pe", lambda e, kt=kt: e.matmul(g.psum[bO][:, :T], lhsT=Vh[:, kt, :], rhs=PT[:, kt % 8, :T], start=(kt == 0), stop=(kt == nk - 1)),
                              reads=[r_V, r_PT[kt % 8]], writes=[g.rps[bO]], inc=(kt % 2 == 1))
                    if p % 2 == 1 or p == npair - 1:
                        gi = p // 2
                        kts = [kt for kt in range(4 * gi, 4 * gi + 4) if kt < nk]
                        for kt in kts:
                            j = kt % 4
                            lastg = max(gg for gg in range(ngrp) if 4 * gg + j < nk)
                            kb.op("pe", lambda e, kt=kt, j=j, lastg=lastg: e.matmul(
                                g.psum[bLp][32 * j:32 * j + 32, :T], lhsT=g.ones[:, 0:32], rhs=PT[:, kt % 8, :T], start=(gi == 0), stop=(gi == lastg),
                                tile_position=(0, 32 * j)), reads=[g.r_ones, r_PT[kt % 8]], writes=[g.rps[bLp]], inc=(kt == kts[-1]))
                KR_ = 128 if nk >= 4 else 32 * nk
                kb.op("dve", lambda e: e.tensor_copy(out=ahl[:KR_, 0, :T], in_=g.psum[bLp][:KR_, :T]), reads=[g.rps[bLp]], writes=[r_ahl])
                kb.op("dve", lambda e: e.tensor_tensor(out=atmp[:KR_, :T], in0=g.psum[bLp][:KR_, :T], in1=ahl[:KR_, 0, :T], op=ALU.subtract),
                      reads=[g.rps[bLp], r_ahl], writes=[r_atmp])
                kb.op("dve", lambda e: e.tensor_copy(out=ahl[:KR_, 1, :T], in_=atmp[:KR_, :T]), reads=[r_atmp], writes=[r_ahl])
                kb.op("pe", lambda e: e.matmul(g.psum[bLr][:, :T], lhsT=g.ones[:KR_, :], rhs=ahl[:KR_, 0, :T], start=True, stop=False),
                      reads=[g.r_ones, r_ahl], writes=[g.rps[bLr]], inc=False)
                kb.op("pe", lambda e: e.matmul(g.psum[bLr][:, :T], lhsT=g.ones[:KR_, :], rhs=ahl[:KR_, 1, :T], start=False, stop=True),
                      reads=[g.r_ones, r_ahl], writes=[g.rps[bLr]])
                kb.op("dve", lambda e: e.reciprocal(out=rl[:, :T], in_=g.psum[bLr][:, :T]), reads=[g.rps[bLr]], writes=[r_rl])
                kb.op("dve", lambda e: e.scalar_tensor_tensor(out=ob[:, qi, :T], in0=g.psum[bO][:, :T], scalar=32.0, in1=rl[:, :T], op0=ALU.mult, op1=ALU.mult),
                      reads=[g.rps[bO], r_rl], writes=[r_ob[qi]])
                kb.dma("pool", S[AT_dst][h * 128:(h + 1) * 128, t0:t0 + T], ob[:, qi, :T], reads=[r_ob[qi]], writes=[g.RS[AT_dst]])


def resid_epilogue(g, srcs, r_srcs, xsub, r_x, gr_idx, y1, r_y1, ss2, r_ss, junk, dst_ap, dst_res):
    kb = g.kb
    for cb in range(2):
        kb.op("act", lambda e, cb=cb: e.activation(out=junk[:, 0:512], in_=srcs[cb], func=AF.Square, accum_out=ss2[:, cb:cb + 1]),
              reads=[r_srcs[cb]], writes=[r_ss])
    kb.op("dve", lambda e: e.tensor_tensor(out=ss2[:, 2:3], in0=ss2[:, 0:1], in1=ss2[:, 1:2], op=ALU.add), reads=[r_ss], writes=[r_ss])
    kb.op("act", lambda e: e.activation(out=ss2[:, 3:4], in_=ss2[:, 2:3], func=AF.Sqrt, bias=EPS, scale=1.0 / D), reads=[r_ss], writes=[r_ss])
    kb.op("dve", lambda e: e.reciprocal(out=ss2[:, 3:4], in_=ss2[:, 3:4]), reads=[r_ss], writes=[r_ss])
    for cb in range(2):
        kb.op("dve", lambda e, cb=cb: e.scalar_tensor_tensor(out=y1[:, cb * 512:(cb + 1) * 512], in0=srcs[cb], scalar=ss2[:, 3:4],
                                                            in1=g.GR[:, gr_idx, cb * 512:(cb + 1) * 512], op0=ALU.mult, op1=ALU.mult),
              reads=[r_srcs[cb], r_ss, g.r_mod], writes=[r_y1])
    kb.op("dve", lambda e: e.tensor_tensor(out=y1[:], in0=y1[:], in1=xsub, op=ALU.add), reads=[r_y1, r_x], writes=[r_y1])
    kb.dma("pool", dst_ap, y1[:], reads=[r_y1], writes=[dst_res])


def phase_D1(g, l, Xsrc, r_X, loc=False):
    nc, kb, I, S = g.nc, g.kb, g.I, g.S
    tiles = [(t0, T, False) for (t0, T) in LTILES] if loc else [(t0, T, t0 == 0) for (t0, T) in TILES]
    srcs = ("MPl", "YLl", "ATl") if loc else ("MP", "YL", "AT")
    g_src, r_gsrc = (S["Gl"], g.RS["Gl"]) if loc else (S["Z"][30 * 128:54 * 128, :], g.RS["Z"])
    if loc:
        Xsrc, r_X = S["Xl"], g.RS["Xl"]
    X1_dst = "X1l" if loc else "X1"
    with nc.sbuf_tensor(nm("WP"), [128, 4, 8, D], BF16) as WP, \
            nc.sbuf_tensor(nm("act3"), [128, 2, 3, 8, 512], BF16) as act3, \
            nc.sbuf_tensor(nm("gts"), [128, 24, 512], BF16) as gts, \
            nc.sbuf_tensor(nm("xt"), [128, 4, D], F32) as xt, \
            nc.sbuf_tensor(nm("mgT"), [128, 8, 512], BF16) as mgT, \
            nc.sbuf_tensor(nm("ta"), [128, 512], F32) as ta, \
            nc.sbuf_tensor(nm("tb"), [128, 512], F32) as tb, \
            nc.sbuf_tensor(nm("y1"), [128, 2, D], F32) as y1, \
            nc.sbuf_tensor(nm("ss2"), [128, 2, 4], F32) as ss2, \
            nc.sbuf_tensor(nm("stg"), [128, 2, 1024], F32) as stg, \
            nc.sbuf_tensor(nm("junk"), [128, 512], BF16) as junk:
        r_stg = [kb.res("stg") for _ in range(2)]
        cnt = [0]
        r_WP = [kb.res("WP") for _ in range(4)]
        r_act = [kb.res("act3") for _ in range(2)]
        r_gts, r_xt, r_mg, r_ta, r_tb = (kb.res("d") for _ in range(5))
        r_y1 = [kb.res("y1") for _ in range(2)]
        r_ss = [kb.res("ss") for _ in range(2)]
        for wi, n in enumerate(("pool_proj", "lru_proj", "mla_proj", "w_out")):
            wsrc = I[n][l].rearrange("(k p) n -> p k n", p=128)
            for k in range(8):
                load_w(g, WP[:, wi, k, :], wsrc[:, k, :], r_WP[wi], stg, r_stg, cnt)
        ny = 0
        for ti, (t0, T, isctx) in enumerate(tiles):
            sel = 1 if isctx else 0
            nsub = T // 128
            ab = ti % 2
            for si, n in enumerate(srcs):
                kb.dma("sp", act3[:, ab, si, :, :T], S[n][:, t0:t0 + T].rearrange("(k p) t -> p k t", p=128), reads=[g.RS[n]], writes=[r_act[ab]])
            kb.dma("sp", gts[:, :, :T], g_src[:, t0:t0 + T].rearrange("(j p) t -> p j t", p=128), reads=[r_gsrc], writes=[r_gts])
            kb.dma("sp", xt[:, :nsub, :], Xsrc[t0:t0 + T, :].rearrange("(s p) d -> p s d", p=128), reads=[r_X], writes=[r_xt])
            for oc in range(8):
                banks = [(oc % 2) * 3 + i for i in range(3)]
                for si in range(3):
                    for k in range(8):
                        kb.op("pe", lambda e, si=si, k=k: e.matmul(g.psum[banks[si]][:, :T], lhsT=WP[:, si, k, oc * 128:(oc + 1) * 128],
                                                                  rhs=act3[:, ab, si, k, :T], start=(k == 0), stop=(k == 7)),
                              reads=[r_WP[si], r_act[ab]], writes=[g.rps[banks[si]]], inc=(k == 7))
                kb.op("dve", lambda e: e.tensor_tensor(out=ta[:, :T], in0=g.psum[banks[0]][:, :T], in1=gts[:, oc, :T], op=ALU.mult),
                      reads=[g.rps[banks[0]], r_gts], writes=[r_ta])
                kb.op("dve", lambda e: e.tensor_tensor(out=tb[:, :T], in0=g.psum[banks[1]][:, :T], in1=gts[:, 8 + oc, :T], op=ALU.mult),
                      reads=[g.rps[banks[1]], r_gts], writes=[r_tb])
                kb.op("dve", lambda e: e.tensor_tensor(out=ta[:, :T], in0=ta[:, :T], in1=tb[:, :T], op=ALU.add), reads=[r_ta, r_tb], writes=[r_ta])
                kb.op("dve", lambda e: e.tensor_tensor(out=tb[:, :T], in0=g.psum[banks[2]][:, :T], in1=gts[:, 16 + oc, :T], op=ALU.mult),
                      reads=[g.rps[banks[2]], r_gts], writes=[r_tb])
                kb.op("dve", lambda e: e.tensor_tensor(out=mgT[:, oc, :T], in0=ta[:, :T], in1=tb[:, :T], op=ALU.add), reads=[r_ta, r_tb], writes=[r_mg])
            for sub in range(nsub):
                for cb in range(2):
                    bank = 6 + cb
                    for k in range(8):
                        kb.op("pe", lambda e, k=k, cb=cb, bank=bank: e.matmul(g.psum[bank][:], lhsT=mgT[:, k, sub * 128:(sub + 1) * 128],
                                                                             rhs=WP[:, 3, k, cb * 512:(cb + 1) * 512], start=(k == 0), stop=(k == 7)),
                              reads=[r_WP[3], r_mg], writes=[g.rps[bank]], inc=(k == 7))
                yi = ny % 2
                ny += 1
                resid_epilogue(g, [g.psum[6][:], g.psum[7][:]], [g.rps[6], g.rps[7]], xt[:, sub, :], r_xt, 0 + sel, y1[:, yi], r_y1[yi],
                               ss2[:, yi], r_ss[yi], junk, S[X1_dst][t0 + sub * 128:t0 + (sub + 1) * 128, :], g.RS[X1_dst])


def phase_ffn_up(g, w1, w3, nf, Xsrc, r_X, HT, tiles, l):
    nc, kb, I, S = g.nc, g.kb, g.I, g.S
    dense = HT is None
    with nc.sbuf_tensor(nm("W1"), [128, 8, nf * 128], BF16) as W1, \
            nc.sbuf_tensor(nm("W3"), [128, 8, nf * 128], BF16) as W3, \
            nc.sbuf_tensor(nm("stg"), [128, 2, nf * 128], F32) as stg, \
            nc.sbuf_tensor(nm("XT"), [128, 4, D if dense else 8], F32) as XT, \
            nc.sbuf_tensor(nm("xn"), [128, 4, D if dense else 8], BF16) as xn, \
            nc.sbuf_tensor(nm("junk"), [128, D if dense else 8], BF16) as junk, \
            nc.sbuf_tensor(nm("hT"), [128, 2, 8, 512], BF16) as hT, \
            nc.sbuf_tensor(nm("ss"), [128, 4], F32) as ss, \
            nc.sbuf_tensor(nm("rstd"), [128, 4], F32) as rstd, \
            nc.sbuf_tensor(nm("sg"), [128, 2, 512], F32) as sg, \
            nc.sbuf_tensor(nm("ao"), [128, 8, 512], BF16) as ao:
        r_W1 = [kb.res("W1") for _ in range(8)]
        r_W3 = [kb.res("W3") for _ in range(8)]
        r_XT, r_xn, r_ss = (kb.res("f") for _ in range(3))
        r_hT = [kb.res("hT") for _ in range(2)]
        r_sg = [kb.res("sg") for _ in range(2)]
        r_ao = [kb.res("ao") for _ in range(8)]
        w1v = w1.rearrange("(k p) n -> p k n", p=128)
        w3v = w3.rearrange("(k p) n -> p k n", p=128)
        r_stg = [kb.res("stg") for _ in range(2)]
        cnt = [0]
        for k in range(8):
            load_w(g, W1[:, k, :], w1v[:, k, :], r_W1[k], stg, r_stg, cnt)
        for k in range(8):
            load_w(g, W3[:, k, :], w3v[:, k, :], r_W3[k], stg, r_stg, cnt)
        na = 0
        for ti, (t0, T) in enumerate(tiles):
            sel = 1 if t0 == 0 else 0
            nsub = T // 128
            b = ti % 2
            if HT is None:
                kb.dma("sp", XT[:, :nsub, :], Xsrc[t0:t0 + T, :].rearrange("(s p) d -> p s d", p=128), reads=[r_X], writes=[r_XT])
                norm_transpose(g, XT, r_XT, nsub, T, g.G2, g.mv[:, 24:32, :], sel, hT[:, b], r_hT[b], xn, r_xn, ss, rstd, r_ss, junk, [0, 1, 2, 3])
            else:
                kb.dma("sp", hT[:, b, :, :T], HT[:, t0:t0 + T].rearrange("(k p) t -> p k t", p=128), reads=[g.RS["H2T"]], writes=[r_hT[b]])
            for f in range(nf):
                b1, b3 = 4 + (f % 2) * 2, 5 + (f % 2) * 2
                for k in range(8):
                    kb.op("pe", lambda e, k=k: e.matmul(g.psum[b1][:, :T], lhsT=W1[:, k, f * 128:(f + 1) * 128], rhs=hT[:, b, k, :T],
                                                        start=(k == 0), stop=(k == 7)), reads=[r_W1[k], r_hT[b]], writes=[g.rps[b1]], inc=(k == 7))
                for k in range(8):
                    kb.op("pe", lambda e, k=k: e.matmul(g.psum[b3][:, :T], lhsT=W3[:, k, f * 128:(f + 1) * 128], rhs=hT[:, b, k, :T],
                                                        start=(k == 0), stop=(k == 7)), reads=[r_W3[k], r_hT[b]], writes=[g.rps[b3]], inc=(k == 7))
                si = f % 2
                ai = na % 8
                na += 1
                kb.op("act", lambda e: e.activation(out=sg[:, si, :T], in_=g.psum[b1][:, :T], func=AF.Silu), reads=[g.rps[b1]], writes=[r_sg[si]])
                kb.op("dve", lambda e: e.tensor_tensor(out=ao[:, ai, :T], in0=g.psum[b3][:, :T], in1=sg[:, si, :T], op=ALU.mult),
                      reads=[g.rps[b3], r_sg[si]], writes=[r_ao[ai]])
                kb.dma("pool", S["FA"][f * 128:(f + 1) * 128, t0:t0 + T], ao[:, ai, :T], reads=[r_ao[ai]], writes=[g.RS["FA"]])


def phase_ffn_down(g, w2, nf, tiles, l, first, last, e, dst, dst_res, dst_off, x1, r_x1):
    nc, kb, I, S = g.nc, g.kb, g.I, g.S
    moe = not (first and last)
    ya_in, ya_out = ("YA0", "YA1") if e % 2 == 1 else ("YA1", "YA0")
    with nc.sbuf_tensor(nm("W2"), [128, nf, D], BF16) as W2, \
            nc.sbuf_tensor(nm("aT"), [128, 2, nf, 512], BF16) as aT, \
            nc.sbuf_tensor(nm("xt"), [128, 4, D], F32) as xt, \
            nc.sbuf_tensor(nm("ya"), [128, 4, D], F32) as ya, \
            nc.sbuf_tensor(nm("gt"), [128, 4, NEXP], F32) as gt, \
            nc.sbuf_tensor(nm("y1"), [128, 2, D], F32) as y1, \
            nc.sbuf_tensor(nm("ss2"), [128, 2, 4], F32) as ss2, \
            nc.sbuf_tensor(nm("stg"), [128, 2, 1024], F32) as stg, \
            nc.sbuf_tensor(nm("junk"), [128, 512], BF16) as junk:
        r_stg = [kb.res("stg") for _ in range(2)]
        cnt = [0]
        r_W2, r_xt, r_ya, r_gt = (kb.res("w") for _ in range(4))
        r_aT = [kb.res("aT") for _ in range(2)]
        r_y1 = [kb.res("y1") for _ in range(2)]
        r_ss = [kb.res("ss") for _ in range(2)]
        w2v = w2.rearrange("(f p) n -> p f n", p=128)
        for f in range(nf):
            load_w(g, W2[:, f, :], w2v[:, f, :], r_W2, stg, r_stg, cnt)
        ny = 0
        for ti, (t0, T) in enumerate(tiles):
            sel = 1 if t0 == 0 else 0
            nsub = T // 128
            ab = ti % 2
            kb.dma("sp", aT[:, ab, :, :T], S["FA"][0:nf * 128, t0:t0 + T].rearrange("(f p) t -> p f t", p=128), reads=[g.RS["FA"]], writes=[r_aT[ab]])
            if last:
                kb.dma("sp", xt[:, :nsub, :], x1[t0:t0 + T, :].rearrange("(s p) d -> p s d", p=128), reads=[r_x1], writes=[r_xt])
            if moe:
                kb.dma("sp", gt[:, :nsub, :], S["GT"][t0:t0 + T, :].rearrange("(s p) e -> p s e", p=128), reads=[g.RS["GT"]], writes=[r_gt])
                if not first:
                    kb.dma("sp", ya[:, :nsub, :], S[ya_in][t0:t0 + T, :].rearrange("(s p) d -> p s d", p=128), reads=[g.RS[ya_in]], writes=[r_ya])
            for sub in range(nsub):
                bb = 4 * (sub % 2)
                for cb in range(2):
                    bank = bb + cb
                    for f in range(nf):
                        kb.op("pe", lambda e_, f=f, cb=cb, bank=bank: e_.matmul(g.psum[bank][:], lhsT=aT[:, ab, f, sub * 128:(sub + 1) * 128],
                                                                               rhs=W2[:, f, cb * 512:(cb + 1) * 512], start=(f == 0), stop=(f == nf - 1)),
                              reads=[r_W2, r_aT[ab]], writes=[g.rps[bank]], inc=(f == nf - 1))
                yi = ny % 2
                ny += 1
                dst_ap = dst[t0 - dst_off + sub * 128:t0 - dst_off + (sub + 1) * 128, :]
                if not moe:
                    resid_epilogue(g, [g.psum[bb][:], g.psum[bb + 1][:]], [g.rps[bb], g.rps[bb + 1]], xt[:, sub, :], r_xt, 2 + sel, y1[:, yi], r_y1[yi],
                                   ss2[:, yi], r_ss[yi], junk, dst_ap, dst_res)
                else:
                    for cb in range(2):
                        yv = ya[:, sub, cb * 512:(cb + 1) * 512]
                        if first:
                            kb.op("dve", lambda e_, cb=cb, yv=yv: e_.tensor_scalar(out=yv, in0=g.psum[bb + cb][:], scalar1=gt[:, sub, e:e + 1], scalar2=None,
                                                                                  op0=ALU.mult), reads=[g.rps[bb + cb], r_gt], writes=[r_ya])
                        else:
                            kb.op("dve", lambda e_, cb=cb, yv=yv: e_.scalar_tensor_tensor(out=yv, in0=g.psum[bb + cb][:], scalar=gt[:, sub, e:e + 1], in1=yv,
                                                                                         op0=ALU.mult, op1=ALU.add), reads=[g.rps[bb + cb], r_gt, r_ya], writes=[r_ya])
                    if last:
                        resid_epilogue(g, [ya[:, sub, 0:512], ya[:, sub, 512:1024]], [r_ya, r_ya], xt[:, sub, :], r_xt, 2, y1[:, yi], r_y1[yi],
                                       ss2[:, yi], r_ss[yi], junk, dst_ap, dst_res)
            if moe and not last:
                kb.dma("pool", S[ya_out][t0:t0 + T, :].rearrange("(s p) d -> p s d", p=128), ya[:, :nsub, :], reads=[r_ya], writes=[g.RS[ya_out]])


def phase_router(g, l, x1, r_x1):
    nc, kb, I, S = g.nc, g.kb, g.I, g.S
    with nc.sbuf_tensor(nm("RW"), [128, NEXP, D], F32) as RW, \
            nc.sbuf_tensor(nm("XT"), [128, 2, 4, D], F32) as XT, \
            nc.sbuf_tensor(nm("xn"), [128, 4, D], BF16) as xn, \
            nc.sbuf_tensor(nm("h2"), [128, D], F32) as h2, \
            nc.sbuf_tensor(nm("junkf"), [128, D], F32) as junkf, \
            nc.sbuf_tensor(nm("junk"), [128, D], BF16) as junk, \
            nc.sbuf_tensor(nm("hT"), [128, 2, 8, 512], BF16) as hT, \
            nc.sbuf_tensor(nm("ss"), [128, 2, 4], F32) as ss, \
            nc.sbuf_tensor(nm("rstd"), [128, 2, 4], F32) as rstd, \
            nc.sbuf_tensor(nm("lg"), [128, 4, NEXP], F32) as lg, \
            nc.sbuf_tensor(nm("mx"), [128, 4, 8], F32) as mx, \
            nc.sbuf_tensor(nm("sm"), [128, 4, 4], F32) as sm, \
            nc.sbuf_tensor(nm("ge"), [128, 4, NEXP], F32) as ge, \
            nc.sbuf_tensor(nm("mk"), [128, 4, NEXP], F32) as mk, \
            nc.sbuf_tensor(nm("go"), [128, 2, 4, NEXP], F32) as go:
        r_RW, r_xn, r_h2, r_lg, r_mx, r_sm, r_ge, r_mk = (kb.res("r") for _ in range(8))
        r_XT = [kb.res("XT") for _ in range(2)]
        r_hT = [kb.res("hT") for _ in range(2)]
        r_ss = [kb.res("ss") for _ in range(2)]
        r_go = [kb.res("go") for _ in range(2)]
        for e in range(NEXP):
            kb.dma("sp", RW[:, e, :], I["router_wT"][0, e].partition_broadcast(128), reads=[g.r_in], writes=[r_RW])
        for ti, (t0, T) in enumerate(LTILES):
            nsub = T // 128
            b = ti % 2
            kb.dma("sp", XT[:, b, :nsub, :], x1[t0:t0 + T, :].rearrange("(s p) d -> p s d", p=128), reads=[r_x1], writes=[r_XT[b]])
            norm_transpose(g, XT[:, b], r_XT[b], nsub, T, g.G2, g.mv[:, 24:32, :], 0, hT[:, b], r_hT[b], xn, r_xn,
                           ss[:, b], rstd[:, b], r_ss[b], junk, [0, 1, 2, 3])
            kb.dma("pool", S["H2T"][:, t0:t0 + T].rearrange("(k p) t -> p k t", p=128), hT[:, b, :, :T], reads=[r_hT[b]], writes=[g.RS["H2T"]])
            for s in range(nsub):
                kb.op("dve", lambda e_, s=s: e_.scalar_tensor_tensor(out=h2[:], in0=XT[:, b, s, :], scalar=rstd[:, b, s:s + 1], in1=g.MR[:, 0, :],
                                                                    op0=ALU.mult, op1=ALU.mult), reads=[r_XT[b], r_ss[b], g.r_mod], writes=[r_h2])
                kb.op("dve", lambda e_: e_.tensor_tensor(out=h2[:], in0=h2[:], in1=g.MR[:, 1, :], op=ALU.add), reads=[r_h2, g.r_mod], writes=[r_h2])
                for e in range(NEXP):
                    kb.op("dve", lambda e_, e=e, s=s: e_.scalar_tensor_tensor(out=junkf[:], in0=h2[:], scalar=1.0, in1=RW[:, e, :], op0=ALU.mult, op1=ALU.mult,
                                                                             accum_out=lg[:, s, e:e + 1]), reads=[r_h2, r_RW], writes=[r_lg])
                kb.op("dve", lambda e_, s=s: e_.max(out=mx[:, s, :], in_=lg[:, s, :]), reads=[r_lg], writes=[r_mx])
                kb.op("dve", lambda e_, s=s: e_.tensor_scalar(out=sm[:, s, 0:1], in0=mx[:, s, 0:1], scalar1=-1.0, scalar2=None, op0=ALU.mult),
                      reads=[r_mx], writes=[r_sm])
                kb.op("act", lambda e_, s=s: e_.activation(out=ge[:, s, :], in_=lg[:, s, :], func=AF.Exp, bias=sm[:, s, 0:1]), reads=[r_lg, r_sm], writes=[r_ge])
                kb.op("dve", lambda e_, s=s: e_.tensor_scalar(out=mk[:, s, :], in0=lg[:, s, :], scalar1=mx[:, s, 1:2], scalar2=None, op0=ALU.is_ge),
                      reads=[r_lg, r_mx], writes=[r_mk])
                kb.op("dve", lambda e_, s=s: e_.scalar_tensor_tensor(out=ge[:, s, :], in0=ge[:, s, :], scalar=1.0, in1=mk[:, s, :], op0=ALU.mult, op1=ALU.mult,
                                                                    accum_out=sm[:, s, 1:2]), reads=[r_ge, r_mk], writes=[r_ge, r_sm])
                kb.op("dve", lambda e_, s=s: e_.reciprocal(out=sm[:, s, 2:3], in_=sm[:, s, 1:2]), reads=[r_sm], writes=[r_sm])
                kb.op("dve", lambda e_, s=s: e_.tensor_scalar(out=go[:, b, s, :], in0=ge[:, s, :], scalar1=sm[:, s, 2:3], scalar2=None, op0=ALU.mult),
                      reads=[r_ge, r_sm], writes=[r_go[b]])
            kb.dma("pool", S["GT"][t0:t0 + T, :].rearrange("(s p) e -> p s e", p=128), go[:, b, :nsub, :], reads=[r_go[b]], writes=[g.RS["GT"]])


def rope_perm():
    p = np.arange(64)
    half = (p % 32) // 16
    return np.where(half == 0, p + 16, p - 16)


def rope_tables():
    rows = SEQ // 64
    t = np.arange(SEQ)
    row = (t // 64).astype(np.float32)
    col = (t % 64).astype(np.float32)
    inv = (np.float32(10000.0) ** (-np.arange(16, dtype=np.float32) / np.float32(16))).astype(np.float32)
    cosT = np.zeros((64, SEQ), np.float32)
    sinT = np.zeros((64, SEQ), np.float32)
    for p in range(64):
        axis, half, f = p // 32, (p % 32) // 16, p % 16
        ang = (row if axis == 0 else col) * inv[f]
        cosT[p] = np.cos(ang)
        sinT[p] = np.sin(ang) * (-1.0 if half == 0 else 1.0)
    return cosT, sinT


def host_inputs(inp):
    perm = rope_perm()
    w_in = inp["w_in"]
    w_in_aug = np.concatenate([w_in[:, :, :3776], w_in[:, :, 3712:3776][:, :, perm], w_in[:, :, 3776:]], axis=2)
    w_uq = inp["w_uq"].reshape(2, 384, 8, 192)
    w_uq_aug = np.concatenate([w_uq, w_uq[:, :, :, 128:][:, :, :, perm]], axis=3).reshape(2, 384, 2048)
    w_ukv = inp["w_ukv"].reshape(2, 256, 8, 256)
    w_ukv_aug = np.concatenate([w_ukv[:, :, :, :128].reshape(2, 256, 1024), w_ukv[:, :, :, 128:].reshape(2, 256, 1024)], axis=2)
    cosT, sinT = rope_tables()
    shared = {k: np.ascontiguousarray(v) for k, v in inp.items() if k not in ("x", "c", "ctx", "c_ctx", "w_in", "w_uq", "w_ukv")}
    shared["w_in"] = np.ascontiguousarray(w_in_aug)
    shared["w_uq"] = np.ascontiguousarray(w_uq_aug)
    shared["w_ukv"] = np.ascontiguousarray(w_ukv_aug)
    shared["ident"] = np.eye(128, dtype=np.float32)
    del shared["router_w"]
    shared["router_wT"] = np.ascontiguousarray(inp["router_w"].transpose(0, 2, 1))
    shared["mod_b_col"] = np.ascontiguousarray(inp["mod_b"].reshape(2, 48, 128).transpose(0, 2, 1))
    vecs = {"pre_mix_g": inp["pre_mix_g"], "pre_ffn_g": inp["pre_ffn_g"], "pool_scale": inp["pool_scale"], "conv_b": inp["conv_b"]}
    for k in range(4):
        vecs["conv_w%d" % k] = inp["conv_w"][:, k]
    for d in range(2):
        vecs["gate_a_b%d" % d] = inp["gate_a_b"][:, d]
        vecs["gate_x_b%d" % d] = inp["gate_x_b"][:, d]
        vecs["lru_lambda%d" % d] = inp["lru_lambda"][:, d]
    vc = np.stack([vecs[n] for n in VNAMES], axis=1)
    shared["vcol"] = np.ascontiguousarray(vc.reshape(2, NV, 8, 128).transpose(0, 3, 1, 2))
    shared["qg_col"] = np.ascontiguousarray(inp["q_norm_g"].reshape(2, 3, 128).transpose(0, 2, 1))
    shared["kvg_col"] = np.ascontiguousarray(inp["kv_norm_g"].reshape(2, 2, 128).transpose(0, 2, 1))
    shared["cosT"] = cosT
    shared["sinT"] = sinT
    maps = []
    for c in range(8):
        b, half = c % 4, c // 4
        m = dict(shared)
        m["cosQ"] = np.ascontiguousarray(cosT[:, half * HALF:(half + 1) * HALF])
        m["sinQ"] = np.ascontiguousarray(sinT[:, half * HALF:(half + 1) * HALF])
        hmv = np.zeros((128, 2), np.float32)
        hmv[:, half] = 1.0
        m["hm"] = hmv
        m["xall"] = np.ascontiguousarray(np.concatenate([inp["ctx"][b], inp["x"][b]], axis=0))
        cc = np.stack([inp["c"][b], inp["c_ctx"]], axis=1)
        m["ccol"] = np.ascontiguousarray(cc.reshape(8, 128, 2).transpose(1, 0, 2))
        maps.append(m)
    return maps


def kernel(**inputs):
    inp = {k: np.asarray(v) for k, v in inputs.items()}
    maps = host_inputs(inp)
    nc = bass.Bass("TRN2", target_bir_lowering=False)
    build(nc)
    res = run_bass_kernel_spmd(nc, maps, core_ids=list(range(8)))
    return np.stack([np.concatenate([res.results[b]["out"], res.results[b + 4]["out"]], axis=0) for b in range(4)], axis=0).astype(np.float32)
```

```python
import numpy as np
import concourse.bass as bass
import concourse.mybir as mybir
from concourse.bass_utils import run_bass_kernel_spmd

F32 = mybir.dt.float32
BF16 = mybir.dt.bfloat16
AF = mybir.ActivationFunctionType
ALU = mybir.AluOpType
AX = mybir.AxisListType

D = 1024
SEQ = 8192
CTX = 256
LT = SEQ + CTX
NCH = 54
ZW = NCH * 128
EPS = 1e-6
DFF = 2816
EFF = 3584
NEXP = 8
MLA_SCALE = 192 ** -0.5
VNAMES = ["pre_mix_g", "pre_ffn_g", "pool_scale", "conv_b", "conv_w0", "conv_w1", "conv_w2", "conv_w3",
          "gate_a_b0", "gate_a_b1", "gate_x_b0", "gate_x_b1", "lru_lambda0", "lru_lambda1"]
NV = len(VNAMES)
VI = {n: i for i, n in enumerate(VNAMES)}
TILES = [(0, 256)] + [(256 + 512 * i, 512) for i in range(16)]
HALF = SEQ // 2
LTILES = [(512 * i, 512) for i in range(8)]


class Res:
    __slots__ = ("name", "w", "r", "dsem", "dcnt", "dq")

    def __init__(self, name):
        self.name = name
        self.w = None
        self.r = {}
        self.dsem = None
        self.dcnt = 0
        self.dq = None


class KB:
    def __init__(self, nc):
        self.nc = nc
        self.eng = {"pe": nc.tensor, "dve": nc.vector, "act": nc.scalar, "pool": nc.gpsimd, "sp": nc.sync}
        self.esem = {n: nc.alloc_semaphore("es_" + n) for n in self.eng}
        self.ecnt = {n: 0 for n in self.eng}
        self.seen = {n: {} for n in self.eng}
        self.nres = 0
        self.ndsem = 0
        self.local = []
        self.persist = []
        self.free_dsems = {"pool": [], "sp": []}

    def res(self, name="r", persist=False):
        self.nres += 1
        r = Res(name + str(self.nres))
        (self.persist if persist else self.local).append(r)
        return r

    def end_phase(self):
        deps = [(self.esem[e], self.ecnt[e]) for e in self.eng if self.ecnt[e] > 0]
        for r in self.local + self.persist:
            if r.dsem is not None:
                deps.append((r.dsem, r.dcnt))
        for en in self.eng:
            self._wait(en, deps)
        for r in self.local:
            if r.dsem is not None:
                self.free_dsems[r.dq].append((r.dsem, r.dcnt))
                r.dsem = None
                r.dq = None
        self.local = []
        for r in self.persist:
            r.w = None
            r.r = {}

    def _deps(self, reads, writes, dma=False):
        deps = []
        for r in reads:
            if r.w is not None:
                deps.append(r.w)
        for w in writes:
            if w.w is not None and not (dma and w.dsem is not None and w.w[0] is w.dsem):
                deps.append(w.w)
            for s, v in w.r.values():
                deps.append((s, v))
        return deps

    def _wait(self, en, deps):
        own = self.esem[en]
        seen = self.seen[en]
        for sem, val in deps:
            if sem is own and en == "pe":
                continue
            key = id(sem)
            if seen.get(key, 0) >= val:
                continue
            self.eng[en].wait_ge(sem, val)
            seen[key] = val

    def _record(self, tok, reads, writes):
        key = id(tok[0])
        for r in reads:
            old = r.r.get(key)
            if old is None or old[1] < tok[1]:
                r.r[key] = tok
        for w in writes:
            w.w = tok
            w.r = {}

    def op(self, en, fn, reads=(), writes=(), inc=True):
        self._wait(en, self._deps(reads, writes))
        inst = fn(self.eng[en])
        if inc:
            self.ecnt[en] += 1
            inst.then_inc(self.esem[en], 1)
            tok = (self.esem[en], self.ecnt[en])
        else:
            tok = (self.esem[en], self.ecnt[en] + 1)
        self._record(tok, reads, writes)
        return inst

    def dma(self, en, out, in_, reads=(), writes=()):
        self._wait(en, self._deps(reads, writes, dma=True))
        inst = self.eng[en].dma_start(out=out, in_=in_)
        w = writes[0]
        if w.dsem is None:
            w.dq = en
            if self.free_dsems[en]:
                w.dsem, w.dcnt = self.free_dsems[en].pop()
            else:
                w.dsem = self.nc.alloc_semaphore("ds_" + w.name)
                self.ndsem += 1
        assert w.dq == en, (w.name, w.dq, en)
        w.dcnt += 16
        inst.then_inc(w.dsem, 16)
        tok = (w.dsem, w.dcnt)
        self._record(tok, reads, writes)
        return inst

    def wait_all(self, en, ress):
        deps = []
        for r in ress:
            if r.w is not None:
                deps.append(r.w)
        self._wait(en, deps)


class Ctx:
    pass


def g_dbg_layer(dbg):
    return 1 if "L1" in dbg else 0


def g_att_heads(dbg):
    for d in dbg:
        if d.startswith("heads"):
            return int(d[5:])
    return 8


_uid = [0]


def nm(s):
    _uid[0] += 1
    return "t%d_%s" % (_uid[0], s)


def build(nc, n_layers=2, dbg=(), stop_after=None):
    kb = KB(nc)
    g = Ctx()
    g.nc, g.kb = nc, kb
    g.dbg = {}

    def din(name, shape, dt=F32):
        return nc.dram_tensor(name, list(shape), dt, kind="ExternalInput").ap()

    def dscr(name, shape, dt):
        t = nc.dram_tensor(name, list(shape), dt, kind="Internal").ap()
        return t

    I = {}
    I["xall"] = din("xall", [LT, D])
    I["ccol"] = din("ccol", [128, 8, 2])
    I["ident"] = din("ident", [128, 128])
    I["cosT"] = din("cosT", [64, SEQ])
    I["sinT"] = din("sinT", [64, SEQ])
    I["cosQ"] = din("cosQ", [64, HALF])
    I["sinQ"] = din("sinQ", [64, HALF])
    I["hm"] = din("hm", [128, 2])
    I["mod_w"] = din("mod_w", [2, D, 6 * D])
    I["mod_b"] = din("mod_b", [2, 6 * D])
    I["mod_b_col"] = din("mod_b_col", [2, 128, 48])
    I["vcol"] = din("vcol", [2, 128, NV, 8])
    I["qg_col"] = din("qg_col", [2, 128, 3])
    I["kvg_col"] = din("kvg_col", [2, 128, 2])
    for n in ("pre_mix_g", "post_mix_g", "pre_ffn_g", "post_ffn_g", "pool_scale", "conv_b"):
        I[n] = din(n, [2, D])
    I["w_in"] = din("w_in", [2, D, ZW])
    I["pool_w"] = din("pool_w", [2, 4, 256, 256])
    I["pool_proj"] = din("pool_proj", [2, D, D])
    I["conv_w"] = din("conv_w", [2, 4, D])
    I["gate_a_w"] = din("gate_a_w", [2, 2, 8, 128, 128])
    I["gate_x_w"] = din("gate_x_w", [2, 2, 8, 128, 128])
    I["gate_a_b"] = din("gate_a_b", [2, 2, D])
    I["gate_x_b"] = din("gate_x_b", [2, 2, D])
    I["lru_lambda"] = din("lru_lambda", [2, 2, D])
    I["lru_proj"] = din("lru_proj", [2, D, D])
    I["q_norm_g"] = din("q_norm_g", [2, 384])
    I["w_uq"] = din("w_uq", [2, 384, 2048])
    I["kv_norm_g"] = din("kv_norm_g", [2, 256])
    I["w_ukv"] = din("w_ukv", [2, 256, 2048])
    I["mla_proj"] = din("mla_proj", [2, D, D])
    I["w_out"] = din("w_out", [2, D, D])
    I["ffn_w1"] = din("ffn_w1", [1, D, DFF])
    I["ffn_w3"] = din("ffn_w3", [1, D, DFF])
    I["ffn_w2"] = din("ffn_w2", [1, DFF, D])
    I["router_wT"] = din("router_wT", [1, NEXP, D])
    I["moe_w1"] = din("moe_w1", [1, NEXP, D, EFF])
    I["moe_w3"] = din("moe_w3", [1, NEXP, D, EFF])
    I["moe_w2"] = din("moe_w2", [1, NEXP, EFF, D])
    g.I = I
    out = nc.dram_tensor("out", [HALF, D], F32, kind="ExternalOutput").ap()
    g.out = out
    g.r_out = kb.res("out", persist=True)

    S = {}
    S["Z"] = dscr("sZ", [ZW, LT], BF16)
    for n in ("MP", "YL", "AT"):
        S[n] = dscr("s" + n, [D, LT], BF16)
    S["KN"] = dscr("sKN", [8, 128, LT], BF16)
    S["KRD"] = dscr("sKRD", [64, LT], BF16)
    S["V"] = dscr("sV", [LT, D], BF16)
    S["QN"] = dscr("sQN", [8, 128, LT], BF16)
    S["QR"] = dscr("sQR", [8, 64, LT], BF16)
    S["X1"] = dscr("sX1", [LT, D], F32)
    S["X2"] = dscr("sX2", [LT, D], F32)
    S["FA"] = dscr("sFA", [EFF, LT], BF16)
    S["H2T"] = dscr("sH2T", [D, LT], BF16)
    S["GT"] = dscr("sGT", [LT, NEXP], F32)
    S["YA0"] = dscr("sYA0", [LT, D], F32)
    S["YA1"] = dscr("sYA1", [LT, D], F32)
    S["CQl"] = dscr("sCQl", [384, HALF], BF16)
    S["Gl"] = dscr("sGl", [3072, HALF], BF16)
    S["MPl"] = dscr("sMPl", [D, HALF], BF16)
    S["YLl"] = dscr("sYLl", [D, HALF], BF16)
    S["ATl"] = dscr("sATl", [D, HALF], BF16)
    S["Xl"] = dscr("sXl", [HALF, D], F32)
    S["X1l"] = dscr("sX1l", [HALF, D], F32)
    S["QNl"] = dscr("sQNl", [8, 128, HALF], BF16)
    S["QRl"] = dscr("sQRl", [8, 64, HALF], BF16)
    g.S = S
    g.RS = {k: kb.res("s" + k, persist=True) for k in S}
    g.r_in = kb.res("inputs", persist=True)

    def dbg_out(name, src_ap, src_res, shape, dt):
        o = nc.dram_tensor("dbg_" + name, list(shape), dt, kind="ExternalOutput").ap()
        r = kb.res("dbg" + name, persist=True)
        kb.dma("sp", o, src_ap, reads=[src_res], writes=[r])
        g.dbg[name] = r

    def sb(name, shape, dt):
        return nc.alloc_sbuf_tensor(nm(name), list(shape), dt)

    g.ident = sb("ident", [128, 128], BF16)
    g.r_ident = kb.res("ident", persist=True)
    kb.dma("pool", g.ident[:], I["ident"], reads=[g.r_in], writes=[g.r_ident])
    g.ones = sb("ones", [128, 128], BF16)
    g.r_ones = kb.res("ones", persist=True)
    kb.op("dve", lambda e: e.memset(g.ones[:], 1.0), writes=[g.r_ones])
    g.mv = sb("mv", [128, 48, 2], F32)
    g.G1 = sb("G1", [128, 8, 2], F32)
    g.G2 = sb("G2", [128, 8, 2], F32)
    g.GR = sb("GR", [128, 4, D], F32)
    g.MR = sb("MR", [128, 2, D], F32)
    g.r_mod = kb.res("mod", persist=True)
    g.psum = [nc.alloc_psum_tensor("ps%d" % i, [128, 512], F32) for i in range(8)]
    g.rps = [kb.res("ps", persist=True) for i in range(8)]

    def dbgS(name, dt=BF16):
        if name in dbg:
            dbg_out(name, S[name], g.RS[name], list(S[name].shape), dt)

    for l in range(n_layers):
        dl = (l == g_dbg_layer(dbg))
        Xsrc, r_X = (I["xall"], g.r_in) if l == 0 else (S["X2"], g.RS["X2"])
        phase_mod(g, l)
        kb.end_phase()
        phase_A(g, l, Xsrc, r_X)
        kb.end_phase()
        if dl:
            dbgS("Z")
        if stop_after == "A":
            break
        phase_pool(g, l)
        kb.end_phase()
        if dl:
            dbgS("MP")
        if stop_after == "pool":
            break
        phase_lru(g, l)
        kb.end_phase()
        if dl:
            dbgS("YL")
        if stop_after == "lru":
            break
        phase_kv(g, l)
        kb.end_phase()
        loc = (l == n_layers - 1) and n_layers == 2
        if loc:
            phase_sel(g)
            kb.end_phase()
        phase_q(g, l, loc)
        kb.end_phase()
        if dl:
            dbgS("KN"); dbgS("KRD"); dbgS("V"); dbgS("QN"); dbgS("QR")
        if stop_after == "kvq":
            break
        phase_att(g, l, loc, att_heads=g_att_heads(dbg))
        kb.end_phase()
        if dl:
            dbgS("AT")
        if stop_after == "att":
            break
        phase_D1(g, l, Xsrc, r_X, loc)
        kb.end_phase()
        if dl:
            dbgS("X1", F32)
        if stop_after == "D1":
            break
        if l == 0:
            phase_ffn_up(g, I["ffn_w1"][0], I["ffn_w3"][0], DFF // 128, S["X1"], g.RS["X1"], None, TILES, l)
            kb.end_phase()
            phase_ffn_down(g, I["ffn_w2"][0], DFF // 128, TILES, l, first=True, last=True, e=0, dst=S["X2"], dst_res=g.RS["X2"], dst_off=0,
                           x1=S["X1"], r_x1=g.RS["X1"])
            kb.end_phase()
            if dl:
                dbgS("X2", F32)
        else:
            phase_router(g, l, S["X1l"], g.RS["X1l"])
            kb.end_phase()
            if dl:
                dbgS("GT", F32)
            for e in range(NEXP):
                phase_ffn_up(g, I["moe_w1"][0, e], I["moe_w3"][0, e], EFF // 128, None, None, S["H2T"], LTILES, l)
                kb.end_phase()
                phase_ffn_down(g, I["moe_w2"][0, e], EFF // 128, LTILES, l, first=(e == 0), last=(e == NEXP - 1), e=e,
                               dst=g.out, dst_res=g.r_out, dst_off=0, x1=S["X1l"], r_x1=g.RS["X1l"])
                kb.end_phase()

    fin = [g.r_out] + list(g.dbg.values())
    kb.wait_all("sp", fin)
    return g


def load_w(g, dst, src, r_dst, stg, r_stg, cnt):
    kb = g.kb
    n = dst.shape[-1]
    cw = stg.shape[-1]
    for c0 in range(0, n, cw):
        c1 = min(n, c0 + cw)
        b = cnt[0] % 2
        cnt[0] += 1
        kb.dma("sp", stg[:, b, :c1 - c0], src[:, c0:c1], reads=[g.r_in], writes=[r_stg[b]])
        if b == 0:
            kb.op("dve", lambda e, b=b, c0=c0, c1=c1: e.tensor_copy(out=dst[:, c0:c1], in_=stg[:, b, :c1 - c0]), reads=[r_stg[b]], writes=[r_dst])
        else:
            kb.op("act", lambda e, b=b, c0=c0, c1=c1: e.activation(out=dst[:, c0:c1], in_=stg[:, b, :c1 - c0], func=AF.Copy), reads=[r_stg[b]], writes=[r_dst])


def phase_mod(g, l):
    nc, kb, I = g.nc, g.kb, g.I
    with nc.sbuf_tensor(nm("MW"), [128, 8, 6 * D], BF16) as MW, \
            nc.sbuf_tensor(nm("cc"), [128, 8, 2], F32) as cc, \
            nc.sbuf_tensor(nm("sc"), [128, 8, 2], BF16) as sc, \
            nc.sbuf_tensor(nm("scb"), [128, 8, 2, 128], BF16) as scb, \
            nc.sbuf_tensor(nm("modb_col"), [128, 48], F32) as modb_col, \
            nc.sbuf_tensor(nm("gcol"), [128, 2, 8], F32) as gcol, \
            nc.sbuf_tensor(nm("modb_bc"), [128, 2, D], F32) as modb_bc, \
            nc.sbuf_tensor(nm("postg_bc"), [128, 2, D], F32) as postg_bc, \
            nc.sbuf_tensor(nm("modb_bc2"), [128, 2, D], F32) as modb_bc2, \
            nc.sbuf_tensor(nm("preg_bc"), [128, D], F32) as preg_bc, \
            nc.sbuf_tensor(nm("stg"), [128, 2, 2048], F32) as stg, \
            nc.sbuf_tensor(nm("mtmp"), [128, 512], F32) as mtmp:
        r_stg = [kb.res("stg") for _ in range(2)]
        cnt = [0]
        r_MW = [kb.res("MW") for _ in range(8)]
        r_c, r_sc, r_scb, r_mb, r_gc, r_bc, r_tmp, r_bc2 = (kb.res("m") for _ in range(8))
        mw = I["mod_w"][l].rearrange("(k p) n -> p k n", p=128)
        for k in range(8):
            load_w(g, MW[:, k, :], mw[:, k, :], r_MW[k], stg, r_stg, cnt)
        kb.dma("sp", cc[:], I["ccol"], reads=[g.r_in], writes=[r_c])
        kb.dma("sp", modb_col[:], I["mod_b_col"][l], reads=[g.r_in], writes=[r_mb])
        kb.dma("sp", gcol[:], I["vcol"][l, :, 0:2, :], reads=[g.r_in], writes=[r_gc])
        kb.dma("sp", modb_bc[:, 0, :], I["mod_b"][l, 2 * D:3 * D].partition_broadcast(128), reads=[g.r_in], writes=[r_bc])
        kb.dma("sp", modb_bc[:, 1, :], I["mod_b"][l, 5 * D:6 * D].partition_broadcast(128), reads=[g.r_in], writes=[r_bc])
        kb.dma("sp", postg_bc[:, 0, :], I["post_mix_g"][l].partition_broadcast(128), reads=[g.r_in], writes=[r_bc])
        kb.dma("sp", postg_bc[:, 1, :], I["post_ffn_g"][l].partition_broadcast(128), reads=[g.r_in], writes=[r_bc])
        kb.op("act", lambda e: e.activation(out=sc[:], in_=cc[:], func=AF.Silu), reads=[r_c], writes=[r_sc])
        for k in range(8):
            for s in range(2):
                kb.op("dve", lambda e, k=k, s=s: e.tensor_copy(out=scb[:, k, s, :], in_=sc[:, k, s:s + 1].to_broadcast([128, 128])),
                      reads=[r_sc], writes=[r_scb])
        for j in range(48):
            bank = 0 if j < 24 else 3
            ps = g.psum[bank]
            c0 = (j % 24) * 16
            for k in range(8):
                kb.op("pe", lambda e, j=j, k=k, ps=ps, c0=c0: e.matmul(ps[:, c0:c0 + 2], lhsT=MW[:, k, j * 128:(j + 1) * 128], rhs=sc[:, k, :],
                                                                      start=(k == 0), stop=(k == 7)),
                      reads=[r_MW[k], r_sc], writes=[g.rps[bank]], inc=(j % 24 == 23 and k == 7))
        for hb, bank in enumerate((0, 3)):
            ps = g.psum[bank]
            kb.op("dve", lambda e, ps=ps, hb=hb: e.tensor_tensor(
                out=g.mv[:, hb * 24:(hb + 1) * 24, :], in0=ps[:, 0:384].rearrange("p (j s) -> p j s", s=16)[:, :, 0:2],
                in1=modb_col[:, hb * 24:(hb + 1) * 24].unsqueeze(2).to_broadcast([128, 24, 2]), op=ALU.add),
                reads=[g.rps[bank], r_mb], writes=[g.r_mod])
        kb.op("dve", lambda e: e.scalar_tensor_tensor(out=g.G1[:], in0=g.mv[:, 8:16, :], scalar=1.0,
                                                      in1=gcol[:, 0, :].unsqueeze(2).to_broadcast([128, 8, 2]), op0=ALU.add, op1=ALU.mult),
              reads=[g.r_mod, r_gc], writes=[g.r_mod])
        kb.op("dve", lambda e: e.scalar_tensor_tensor(out=g.G2[:], in0=g.mv[:, 32:40, :], scalar=1.0,
                                                      in1=gcol[:, 1, :].unsqueeze(2).to_broadcast([128, 8, 2]), op0=ALU.add, op1=ALU.mult),
              reads=[g.r_mod, r_gc], writes=[g.r_mod])
        if l == 1:
            kb.dma("sp", modb_bc2[:, 0, :], I["mod_b"][l, 3 * D:4 * D].partition_broadcast(128), reads=[g.r_in], writes=[r_bc2])
            kb.dma("sp", modb_bc2[:, 1, :], I["mod_b"][l, 4 * D:5 * D].partition_broadcast(128), reads=[g.r_in], writes=[r_bc2])
            kb.dma("sp", preg_bc[:], I["pre_ffn_g"][l].partition_broadcast(128), reads=[g.r_in], writes=[r_bc2])
            for part in range(2):
                col0 = (3 + part) * D
                for cb in range(2):
                    bank = 1 + cb
                    pb = g.psum[bank]
                    for k in range(8):
                        kb.op("pe", lambda e, k=k, pb=pb, c0=col0 + cb * 512: e.matmul(
                            pb[:], lhsT=scb[:, k, 0, :], rhs=MW[:, k, c0:c0 + 512], start=(k == 0), stop=(k == 7)),
                            reads=[r_MW[k], r_scb], writes=[g.rps[bank]], inc=(k == 7))
                    cs = slice(cb * 512, (cb + 1) * 512)
                    if part == 0:
                        kb.op("dve", lambda e, pb=pb, cs=cs: e.tensor_tensor(out=g.MR[:, 1, cs], in0=pb[:], in1=modb_bc2[:, 0, cs], op=ALU.add),
                              reads=[g.rps[bank], r_bc2], writes=[g.r_mod])
                    else:
                        kb.op("dve", lambda e, pb=pb, cs=cs: e.tensor_tensor(out=mtmp[:], in0=pb[:], in1=modb_bc2[:, 1, cs], op=ALU.add),
                              reads=[g.rps[bank], r_bc2], writes=[r_tmp])
                        kb.op("dve", lambda e, cs=cs: e.scalar_tensor_tensor(out=g.MR[:, 0, cs], in0=mtmp[:], scalar=1.0, in1=preg_bc[:, cs],
                                                                            op0=ALU.add, op1=ALU.mult), reads=[r_tmp, r_bc2], writes=[g.r_mod])
        n = 0
        for part in range(2):
            col0 = (2 if part == 0 else 5) * D
            for s in range(2):
                for cb in range(2):
                    bank = 1 + (n % 2)
                    n += 1
                    pb = g.psum[bank]
                    for k in range(8):
                        kb.op("pe", lambda e, k=k, s=s, pb=pb, c0=col0 + cb * 512: e.matmul(
                            pb[:], lhsT=scb[:, k, s, :], rhs=MW[:, k, c0:c0 + 512], start=(k == 0), stop=(k == 7)),
                            reads=[r_MW[k], r_scb], writes=[g.rps[bank]], inc=(k == 7))
                    kb.op("dve", lambda e, pb=pb, part=part, cb=cb: e.tensor_tensor(
                        out=mtmp[:], in0=pb[:], in1=modb_bc[:, part, cb * 512:(cb + 1) * 512], op=ALU.add),
                        reads=[g.rps[bank], r_bc], writes=[r_tmp])
                    kb.op("dve", lambda e, part=part, s=s, cb=cb: e.tensor_tensor(
                        out=g.GR[:, part * 2 + s, cb * 512:(cb + 1) * 512], in0=mtmp[:],
                        in1=postg_bc[:, part, cb * 512:(cb + 1) * 512], op=ALU.mult),
                        reads=[r_tmp, r_bc], writes=[g.r_mod])


def rms_rows(g, xt, r_x, nsub, ss, rstd, r_ss, junk, width=D):
    kb = g.kb
    for s in range(nsub):
        kb.op("act", lambda e, s=s: e.activation(out=junk[:], in_=xt[:, s, :], func=AF.Square, accum_out=ss[:, s:s + 1]),
              reads=[r_x], writes=[r_ss])
    kb.op("act", lambda e: e.activation(out=rstd[:, :nsub], in_=ss[:, :nsub], func=AF.Sqrt, bias=EPS, scale=1.0 / width),
          reads=[r_ss], writes=[r_ss])
    kb.op("dve", lambda e: e.reciprocal(out=rstd[:, :nsub], in_=rstd[:, :nsub]), reads=[r_ss], writes=[r_ss])


def norm_transpose(g, xt, r_x, nsub, T, Gv, Sv, sel, hT, r_hT, xn, r_xn, ss, rstd, r_ss, junk, pt_banks):
    kb = g.kb
    rms_rows(g, xt, r_x, nsub, ss, rstd, r_ss, junk)
    for s in range(nsub):
        kb.op("dve", lambda e, s=s: e.tensor_scalar(out=xn[:, s, :], in0=xt[:, s, :], scalar1=rstd[:, s:s + 1], scalar2=None, op0=ALU.mult),
              reads=[r_x, r_ss], writes=[r_xn])
    for k in range(8):
        bank = pt_banks[k // 2]
        pv = g.psum[bank][:].bitcast(BF16)
        off = (k % 2) * 512
        for s in range(nsub):
            kb.op("pe", lambda e, k=k, s=s, pv=pv, off=off: e.transpose(pv[:, off + s * 128:off + (s + 1) * 128],
                                                                       xn[:, s, k * 128:(k + 1) * 128], g.ident[:]),
                  reads=[r_xn, g.r_ident], writes=[g.rps[bank]], inc=(s == nsub - 1))
        kb.op("act", lambda e, k=k, pv=pv, off=off: e.activation(out=hT[:, k, :T], in_=pv[:, off:off + T], func=AF.Identity,
                                                                 bias=Sv[:, k, sel:sel + 1], scale=Gv[:, k, sel:sel + 1]),
              reads=[g.rps[bank], g.r_mod], writes=[r_hT])


def phase_A(g, l, Xsrc, r_X):
    nc, kb, I, S = g.nc, g.kb, g.I, g.S
    with nc.sbuf_tensor(nm("WIN"), [128, 8, ZW], BF16) as WIN, \
            nc.sbuf_tensor(nm("XT"), [128, 1, 4, D], F32) as XT, \
            nc.sbuf_tensor(nm("xn"), [128, 4, D], BF16) as xn, \
            nc.sbuf_tensor(nm("junk"), [128, D], BF16) as junk, \
            nc.sbuf_tensor(nm("hT"), [128, 2, 8, 512], BF16) as hT, \
            nc.sbuf_tensor(nm("ss"), [128, 2, 4], F32) as ss, \
            nc.sbuf_tensor(nm("rstd"), [128, 2, 4], F32) as rstd, \
            nc.sbuf_tensor(nm("stg"), [128, 2, 1152], F32) as stg, \
            nc.sbuf_tensor(nm("zo"), [128, 12, 512], BF16) as zo:
        r_stg = [kb.res("stg") for _ in range(2)]
        cnt = [0]
        r_W = [kb.res("WIN") for _ in range(8)]
        r_XT = [kb.res("XT") for _ in range(1)]
        r_xn = kb.res("xn")
        r_hT = [kb.res("hT") for _ in range(2)]
        r_ss = [kb.res("ss") for _ in range(2)]
        r_zo = [kb.res("zo") for _ in range(12)]
        wv = I["w_in"][l].rearrange("(k p) n -> p k n", p=128)
        for k in range(8):
            load_w(g, WIN[:, k, :], wv[:, k, :], r_W[k], stg, r_stg, cnt)
        nz = 0
        for ti, (t0, T) in enumerate(TILES):
            sel = 1 if ti == 0 else 0
            nsub = T // 128
            b = ti % 2
            kb.dma("sp", XT[:, 0, :nsub, :], Xsrc[t0:t0 + T, :].rearrange("(s p) d -> p s d", p=128), reads=[r_X], writes=[r_XT[0]])
            norm_transpose(g, XT[:, 0], r_XT[0], nsub, T, g.G1, g.mv[:, 0:8, :], sel, hT[:, b], r_hT[b], xn, r_xn,
                           ss[:, b], rstd[:, b], r_ss[b], junk, [0, 1, 2, 3])
            for j in range(NCH):
                bank = 4 + (j % 4)
                pb = g.psum[bank]
                for k in range(8):
                    kb.op("pe", lambda e, j=j, k=k, pb=pb: e.matmul(pb[:, :T], lhsT=WIN[:, k, j * 128:(j + 1) * 128], rhs=hT[:, b, k, :T],
                                                                    start=(k == 0), stop=(k == 7)),
                          reads=[r_W[k], r_hT[b]], writes=[g.rps[bank]], inc=(k == 7))
                zi = nz % 12
                nz += 1
                if j >= 30:
                    kb.op("act", lambda e, pb=pb, zi=zi: e.activation(out=zo[:, zi, :T], in_=pb[:, :T], func=AF.Sigmoid),
                          reads=[g.rps[bank]], writes=[r_zo[zi]])
                else:
                    kb.op("dve", lambda e, pb=pb, zi=zi: e.tensor_copy(out=zo[:, zi, :T], in_=pb[:, :T]),
                          reads=[g.rps[bank]], writes=[r_zo[zi]])
                kb.dma("pool", S["Z"][j * 128:(j + 1) * 128, t0:t0 + T], zo[:, zi, :T], reads=[r_zo[zi]], writes=[g.RS["Z"]])


def phase_pool(g, l):
    nc, kb, I, S = g.nc, g.kb, g.I, g.S
    LPM = SEQ + 32
    with nc.sbuf_tensor(nm("ub"), [128, 2, LPM], BF16) as ub, \
            nc.sbuf_tensor(nm("T1"), [128, LPM], F32) as T1, \
            nc.sbuf_tensor(nm("T2"), [128, LPM], F32) as T2, \
            nc.sbuf_tensor(nm("M"), [128, 2, SEQ], BF16) as M, \
            nc.sbuf_tensor(nm("RC"), [128, SEQ], F32) as RC, \
            nc.sbuf_tensor(nm("PW"), [128, 4, 2, 256], BF16) as PW, \
            nc.sbuf_tensor(nm("pscol"), [128, 8], F32) as pscol, \
            nc.sbuf_tensor(nm("po"), [128, 8, 512], BF16) as po:
        r_ub = [kb.res("ub") for _ in range(2)]
        r_T1, r_T2, r_RC, r_PW, r_ps = (kb.res("p") for _ in range(5))
        r_M = [kb.res("M") for _ in range(2)]
        r_po = [kb.res("po") for _ in range(8)]
        kb.dma("pool", PW[:], I["pool_w"][l].rearrange("g (ic p) j -> p g ic j", p=128), reads=[g.r_in], writes=[r_PW])
        kb.dma("sp", pscol[:], I["vcol"][l, :, VI["pool_scale"], :], reads=[g.r_in], writes=[r_ps])
        npo = 0
        nb = 0
        for gi in range(4):
            w = 2 << gi
            hw = w // 2
            for (off, L) in ((0, CTX), (CTX, SEQ)):
                LP = L + 32
                kb.op("dve", lambda e: e.memset(RC[:, :L], 1.0 / w), writes=[r_RC])
                for t in range(hw):
                    kb.op("dve", lambda e, t=t: e.memset(RC[:, t:t + 1], 1.0 / (t + hw)), writes=[r_RC])
                for t in range(L - hw + 1, L):
                    kb.op("dve", lambda e, t=t: e.memset(RC[:, t:t + 1], 1.0 / (L - t + hw)), writes=[r_RC])
                for ch in range(2):
                    c = 2 * gi + ch
                    u = ub[:, ch, :]
                    kb.op("dve", lambda e, u=u: e.memset(u[:, 0:16], 0.0), writes=[r_ub[ch]])
                    kb.op("dve", lambda e, u=u: e.memset(u[:, 16 + L:32 + L], 0.0), writes=[r_ub[ch]])
                    kb.dma("sp", u[:, 16:16 + L], S["Z"][c * 128:(c + 1) * 128, off:off + L], reads=[g.RS["Z"]], writes=[r_ub[ch]])
                    kb.op("dve", lambda e, u=u: e.tensor_tensor(out=T1[:, 1:LP], in0=u[:, 0:LP - 1], in1=u[:, 1:LP], op=ALU.add),
                          reads=[r_ub[ch]], writes=[r_T1])
                    Sb, rS, Ob, rO = T1, r_T1, T2, r_T2
                    if w >= 4:
                        kb.op("dve", lambda e: e.tensor_tensor(out=T2[:, 2:LP - 1], in0=T1[:, 1:LP - 2], in1=T1[:, 3:LP], op=ALU.add),
                              reads=[r_T1], writes=[r_T2])
                        Sb, rS, Ob, rO = T2, r_T2, T1, r_T1
                    if w >= 8:
                        kb.op("dve", lambda e: e.tensor_tensor(out=T1[:, 4:LP - 3], in0=T2[:, 2:LP - 5], in1=T2[:, 6:LP - 1], op=ALU.add),
                              reads=[r_T2], writes=[r_T1])
                        Sb, rS, Ob, rO = T1, r_T1, T2, r_T2
                    if w >= 16:
                        kb.op("dve", lambda e: e.tensor_tensor(out=T2[:, 8:LP - 7], in0=T1[:, 4:LP - 11], in1=T1[:, 12:LP - 3], op=ALU.add),
                              reads=[r_T1], writes=[r_T2])
                        Sb, rS, Ob, rO = T2, r_T2, T1, r_T1
                    kb.op("dve", lambda e, Sb=Sb, Ob=Ob: e.tensor_tensor(out=Ob[:, 16:16 + L], in0=Sb[:, 16:16 + L], in1=RC[:, :L], op=ALU.mult),
                          reads=[rS, r_RC], writes=[rO])
                    kb.op("dve", lambda e, Ob=Ob, u=u, ch=ch: e.tensor_tensor(out=M[:, ch, :L], in0=Ob[:, 16:16 + L], in1=u[:, 16:16 + L], op=ALU.subtract),
                          reads=[rO, r_ub[ch]], writes=[r_M[ch]])
                for t0 in range(0, L, 512):
                    T = min(512, L - t0)
                    for jc in range(2):
                        bank = nb % 8
                        nb += 1
                        pb = g.psum[bank]
                        for ic in range(2):
                            kb.op("pe", lambda e, ic=ic, jc=jc, pb=pb, t0=t0, T=T: e.matmul(
                                pb[:, :T], lhsT=PW[:, gi, ic, jc * 128:(jc + 1) * 128], rhs=M[:, ic, t0:t0 + T], start=(ic == 0), stop=(ic == 1)),
                                reads=[r_PW, r_M[ic]], writes=[g.rps[bank]], inc=(ic == 1))
                        pi = npo % 8
                        npo += 1
                        c = 2 * gi + jc
                        kb.op("act", lambda e, pb=pb, pi=pi, T=T, c=c: e.activation(out=po[:, pi, :T], in_=pb[:, :T], func=AF.Identity,
                                                                                   scale=pscol[:, c:c + 1]),
                              reads=[g.rps[bank], r_ps], writes=[r_po[pi]])
                        kb.dma("pool", S["MP"][c * 128:(c + 1) * 128, off + t0:off + t0 + T], po[:, pi, :T], reads=[r_po[pi]], writes=[g.RS["MP"]])


def phase_lru(g, l):
    nc, kb, I, S = g.nc, g.kb, g.I, g.S
    with nc.sbuf_tensor(nm("LXG"), [128, LT], BF16) as LXG, \
            nc.sbuf_tensor(nm("UB"), [128, LT], BF16) as UB, \
            nc.sbuf_tensor(nm("T1"), [128, LT], F32) as T1, \
            nc.sbuf_tensor(nm("T2"), [128, LT], F32) as T2, \
            nc.sbuf_tensor(nm("T3"), [128, LT], F32) as T3, \
            nc.sbuf_tensor(nm("T4"), [128, LT], F32) as T4, \
            nc.sbuf_tensor(nm("GA"), [128, 2, 8, 128], BF16) as GA, \
            nc.sbuf_tensor(nm("GX"), [128, 2, 8, 128], BF16) as GX, \
            nc.sbuf_tensor(nm("vc"), [128, NV, 8], F32) as vc, \
            nc.sbuf_tensor(nm("cn"), [128, 2, 2, 8], F32) as cn:
        r_LXG, r_UB, r_T1, r_T2, r_T3, r_T4, r_GA, r_vc, r_cn = (kb.res("l") for _ in range(9))
        kb.dma("pool", GA[:], I["gate_a_w"][l].rearrange("d c j k -> j d c k"), reads=[g.r_in], writes=[r_GA])
        kb.dma("pool", GX[:], I["gate_x_w"][l].rearrange("d c j k -> j d c k"), reads=[g.r_in], writes=[r_GA])
        kb.dma("sp", vc[:], I["vcol"][l], reads=[g.r_in], writes=[r_vc])
        for d in range(2):
            lam = vc[:, VI["lru_lambda%d" % d], :]
            kb.op("act", lambda e, d=d, lam=lam: e.activation(out=cn[:, 0, d, :], in_=lam, func=AF.Exp, scale=-1.0), reads=[r_vc], writes=[r_cn])
            kb.op("act", lambda e, d=d: e.activation(out=cn[:, 0, d, :], in_=cn[:, 0, d, :], func=AF.Ln, bias=1.0), reads=[r_cn], writes=[r_cn])
            kb.op("dve", lambda e, d=d: e.tensor_scalar(out=cn[:, 1, d, :], in0=cn[:, 0, d, :], scalar1=-16.0, scalar2=None, op0=ALU.mult),
                  reads=[r_cn], writes=[r_cn])
            kb.op("dve", lambda e, d=d: e.tensor_scalar(out=cn[:, 0, d, :], in0=cn[:, 0, d, :], scalar1=-8.0, scalar2=None, op0=ALU.mult),
                  reads=[r_cn], writes=[r_cn])
        segs = ((0, CTX), (CTX, LT))
        nb = 0
        for c in range(8):
            def col(name):
                return vc[:, VI[name], c:c + 1]
            kb.dma("sp", LXG[:], S["Z"][(8 + c) * 128:(9 + c) * 128, :], reads=[g.RS["Z"]], writes=[r_LXG])
            for (s0, s1) in segs:
                kb.op("dve", lambda e, s0=s0, s1=s1: e.tensor_scalar(out=T1[:, s0:s1], in0=LXG[:, s0:s1], scalar1=col("conv_w2"), scalar2=col("conv_b"),
                                                                    op0=ALU.mult, op1=ALU.add), reads=[r_LXG, r_vc], writes=[r_T1])
                for k, o in ((0, -2), (1, -1), (3, 1)):
                    a, b = max(s0, s0 - o), min(s1, s1 - o)
                    kb.op("dve", lambda e, a=a, b=b, o=o, k=k: e.scalar_tensor_tensor(out=T1[:, a:b], in0=LXG[:, a + o:b + o], scalar=col("conv_w%d" % k),
                                                                                    in1=T1[:, a:b], op0=ALU.mult, op1=ALU.add),
                          reads=[r_LXG, r_vc, r_T1], writes=[r_T1])
            kb.op("act", lambda e: e.activation(out=UB[:], in_=T1[:], func=AF.Copy), reads=[r_T1], writes=[r_UB])
            kb.dma("sp", LXG[:], S["Z"][(16 + c) * 128:(17 + c) * 128, :], reads=[g.RS["Z"]], writes=[r_LXG])
            for d in range(2):
                Rb, rR = T1, r_T1
                Ib, rI = (T2, r_T2) if d == 0 else (T4, r_T4)
                Ab, rA = T3, r_T3
                for (t0, T) in TILES:
                    b0, b1 = nb % 8, (nb + 1) % 8
                    nb += 2
                    kb.op("pe", lambda e, t0=t0, T=T, b0=b0: e.matmul(g.psum[b0][:, :T], lhsT=GA[:, d, c, :], rhs=UB[:, t0:t0 + T], start=True, stop=True),
                          reads=[r_GA, r_UB], writes=[g.rps[b0]])
                    kb.op("pe", lambda e, t0=t0, T=T, b1=b1: e.matmul(g.psum[b1][:, :T], lhsT=GX[:, d, c, :], rhs=UB[:, t0:t0 + T], start=True, stop=True),
                          reads=[r_GA, r_UB], writes=[g.rps[b1]])
                    kb.op("act", lambda e, t0=t0, T=T, b0=b0, Rb=Rb: e.activation(out=Rb[:, t0:t0 + T], in_=g.psum[b0][:, :T], func=AF.Sigmoid,
                                                                                 bias=col("gate_a_b%d" % d)), reads=[g.rps[b0], r_vc], writes=[rR])
                    kb.op("act", lambda e, t0=t0, T=T, b1=b1, Ib=Ib: e.activation(out=Ib[:, t0:t0 + T], in_=g.psum[b1][:, :T], func=AF.Sigmoid,
                                                                                 bias=col("gate_x_b%d" % d)), reads=[g.rps[b1], r_vc], writes=[rI])
                kb.op("act", lambda e, Ab=Ab, Rb=Rb: e.activation(out=Ab[:], in_=Rb[:], func=AF.Exp, scale=cn[:, 0, d, c:c + 1]), reads=[rR, r_cn], writes=[rA])
                kb.op("act", lambda e, Rb=Rb: e.activation(out=Rb[:], in_=Rb[:], func=AF.Exp, scale=cn[:, 1, d, c:c + 1]), reads=[rR, r_cn], writes=[rR])
                kb.op("act", lambda e, Rb=Rb: e.activation(out=Rb[:], in_=Rb[:], func=AF.Sqrt, bias=1.0, scale=-1.0), reads=[rR], writes=[rR])
                kb.op("dve", lambda e, Rb=Rb, Ib=Ib: e.tensor_tensor(out=Rb[:], in0=Rb[:], in1=Ib[:], op=ALU.mult), reads=[rR, rI], writes=[rR])
                kb.op("dve", lambda e, Rb=Rb: e.tensor_tensor(out=Rb[:], in0=Rb[:], in1=UB[:], op=ALU.mult), reads=[rR, r_UB], writes=[rR])
                if d == 0:
                    kb.op("dve", lambda e, Ab=Ab, Rb=Rb, Ib=Ib: e.tensor_tensor_scan(out=Ib[:, 0:CTX], data0=Ab[:, 0:CTX], data1=Rb[:, 0:CTX], initial=0.0,
                                                                                   op0=ALU.mult, op1=ALU.add), reads=[rA, rR], writes=[rI])
                    kb.op("dve", lambda e, Ab=Ab, Rb=Rb, Ib=Ib: e.tensor_tensor_scan(out=Ib[:, CTX:LT], data0=Ab[:, CTX:LT], data1=Rb[:, CTX:LT],
                                                                                   initial=Ib[:, CTX - 1:CTX], op0=ALU.mult, op1=ALU.add),
                          reads=[rA, rR, rI], writes=[rI])
                else:
                    kb.op("dve", lambda e, Ab=Ab, Rb=Rb, Ib=Ib: e.tensor_tensor_scan(out=Ib[:, CTX - 1::-1], data0=Ab[:, CTX - 1::-1], data1=Rb[:, CTX - 1::-1],
                                                                                   initial=0.0, op0=ALU.mult, op1=ALU.add), reads=[rA, rR], writes=[rI])
                    kb.op("dve", lambda e, Ab=Ab, Rb=Rb, Ib=Ib: e.tensor_tensor_scan(out=Ib[:, LT - 1:CTX - 1:-1], data0=Ab[:, LT - 1:CTX - 1:-1],
                                                                                   data1=Rb[:, LT - 1:CTX - 1:-1], initial=Ib[:, 0:1],
                                                                                   op0=ALU.mult, op1=ALU.add), reads=[rA, rR, rI], writes=[rI])
            kb.op("dve", lambda e: e.tensor_tensor(out=T2[:], in0=T2[:], in1=T4[:], op=ALU.add), reads=[r_T2, r_T4], writes=[r_T2])
            kb.op("dve", lambda e: e.tensor_tensor(out=T1[:], in0=LXG[:], in1=LXG[:], op=ALU.mult), reads=[r_LXG], writes=[r_T1])
            kb.op("dve", lambda e: e.tensor_scalar(out=T1[:], in0=T1[:], scalar1=0.044715, scalar2=1.0, op0=ALU.mult, op1=ALU.add), reads=[r_T1], writes=[r_T1])
            kb.op("dve", lambda e: e.tensor_tensor(out=T1[:], in0=T1[:], in1=LXG[:], op=ALU.mult), reads=[r_T1, r_LXG], writes=[r_T1])
            kb.op("act", lambda e: e.activation(out=T1[:], in_=T1[:], func=AF.Sigmoid, scale=1.5957691216057308), reads=[r_T1], writes=[r_T1])
            kb.op("dve", lambda e: e.tensor_tensor(out=T1[:], in0=T1[:], in1=LXG[:], op=ALU.mult), reads=[r_T1, r_LXG], writes=[r_T1])
            kb.op("dve", lambda e: e.tensor_tensor(out=UB[:], in0=T1[:], in1=T2[:], op=ALU.mult), reads=[r_T1, r_T2], writes=[r_UB])
            kb.dma("pool", S["YL"][c * 128:(c + 1) * 128, :], UB[:], reads=[r_UB], writes=[g.RS["YL"]])


def phase_sel(g):
    nc, kb, I, S = g.nc, g.kb, g.I, g.S
    with nc.sbuf_tensor(nm("hm"), [128, 2], F32) as hm, \
            nc.sbuf_tensor(nm("sa"), [128, 2, HALF], BF16) as sa, \
            nc.sbuf_tensor(nm("sbb"), [128, 2, HALF], BF16) as sbb, \
            nc.sbuf_tensor(nm("so"), [128, 2, HALF], BF16) as so, \
            nc.sbuf_tensor(nm("xa"), [128, 2, 4, D], F32) as xa, \
            nc.sbuf_tensor(nm("xb"), [128, 2, 4, D], F32) as xb, \
            nc.sbuf_tensor(nm("xo"), [128, 2, 4, D], F32) as xo:
        r_hm = kb.res("hm")
        r_sa = [kb.res("sa") for _ in range(2)]
        r_sb = [kb.res("sb") for _ in range(2)]
        r_so = [kb.res("so") for _ in range(2)]
        kb.dma("sp", hm[:], I["hm"], reads=[g.r_in], writes=[r_hm])
        n = 0
        for src, r_src, dst, nrows in ((S["Z"][24 * 128:27 * 128, :], g.RS["Z"], "CQl", 384), (S["Z"][30 * 128:54 * 128, :], g.RS["Z"], "Gl", 3072),
                                      (S["MP"], g.RS["MP"], "MPl", D), (S["YL"], g.RS["YL"], "YLl", D)):
            for rc in range(nrows // 128):
                b = n % 2
                n += 1
                rows = slice(rc * 128, (rc + 1) * 128)
                kb.dma("sp", sa[:, b, :], src[rows, CTX:CTX + HALF], reads=[r_src], writes=[r_sa[b]])
                kb.dma("sp", sbb[:, b, :], src[rows, CTX + HALF:LT], reads=[r_src], writes=[r_sb[b]])
                kb.op("dve", lambda e, b=b: e.tensor_scalar(out=so[:, b, :], in0=sa[:, b, :], scalar1=hm[:, 0:1], scalar2=None, op0=ALU.mult),
                      reads=[r_sa[b], r_hm], writes=[r_so[b]])
                kb.op("dve", lambda e, b=b: e.scalar_tensor_tensor(out=so[:, b, :], in0=sbb[:, b, :], scalar=hm[:, 1:2], in1=so[:, b, :],
                                                                  op0=ALU.mult, op1=ALU.add), reads=[r_sb[b], r_hm, r_so[b]], writes=[r_so[b]])
                kb.dma("pool", S[dst][rows, :], so[:, b, :], reads=[r_so[b]], writes=[g.RS[dst]])
        for i, (t0, T) in enumerate(LTILES):
            b = i % 2
            kb.dma("sp", xa[:, b], S["X2"][CTX + t0:CTX + t0 + T, :].rearrange("(s p) d -> p s d", p=128), reads=[g.RS["X2"]], writes=[r_sa[b]])
            kb.dma("sp", xb[:, b], S["X2"][CTX + HALF + t0:CTX + HALF + t0 + T, :].rearrange("(s p) d -> p s d", p=128), reads=[g.RS["X2"]], writes=[r_sb[b]])
            kb.op("dve", lambda e, b=b: e.tensor_scalar(out=xo[:, b], in0=xa[:, b], scalar1=hm[:, 0:1], scalar2=None, op0=ALU.mult),
                  reads=[r_sa[b], r_hm], writes=[r_so[b]])
            kb.op("dve", lambda e, b=b: e.scalar_tensor_tensor(out=xo[:, b], in0=xb[:, b], scalar=hm[:, 1:2], in1=xo[:, b],
                                                              op0=ALU.mult, op1=ALU.add), reads=[r_sb[b], r_hm, r_so[b]], writes=[r_so[b]])
            kb.dma("pool", S["Xl"][t0:t0 + T, :].rearrange("(s p) d -> p s d", p=128), xo[:, b], reads=[r_so[b]], writes=[g.RS["Xl"]])


def rms_feat(g, src, nk, t0, T, width, gcol, r_g, sq, r_sq, rb, r_rb, cnm, r_cn, r_src, bank):
    kb = g.kb
    for k in range(nk):
        kb.op("dve", lambda e, k=k: e.tensor_tensor(out=sq[:, k, :T], in0=src[:, k, t0:t0 + T], in1=src[:, k, t0:t0 + T], op=ALU.mult),
              reads=[r_src], writes=[r_sq])
    pb = g.psum[bank]
    for k in range(nk):
        kb.op("pe", lambda e, k=k: e.matmul(pb[:, :T], lhsT=g.ones[:], rhs=sq[:, k, :T], start=(k == 0), stop=(k == nk - 1)),
              reads=[g.r_ones, r_sq], writes=[g.rps[bank]], inc=(k == nk - 1))
    kb.op("act", lambda e: e.activation(out=rb[:, :T], in_=pb[:, :T], func=AF.Sqrt, bias=EPS, scale=1.0 / width), reads=[g.rps[bank]], writes=[r_rb])
    kb.op("dve", lambda e: e.reciprocal(out=rb[:, :T], in_=rb[:, :T]), reads=[r_rb], writes=[r_rb])
    for k in range(nk):
        kb.op("dve", lambda e, k=k: e.scalar_tensor_tensor(out=cnm[:, k, :T], in0=src[:, k, t0:t0 + T], scalar=gcol[:, k:k + 1], in1=rb[:, :T],
                                                          op0=ALU.mult, op1=ALU.mult), reads=[r_src, r_rb, r_g], writes=[r_cn])


def phase_kv(g, l):
    nc, kb, I, S = g.nc, g.kb, g.I, g.S
    with nc.sbuf_tensor(nm("CKV"), [128, 2, LT], BF16) as CKV, \
            nc.sbuf_tensor(nm("KRa"), [64, LT], BF16) as KRa, \
            nc.sbuf_tensor(nm("KRb"), [64, LT], BF16) as KRb, \
            nc.sbuf_tensor(nm("COS"), [64, SEQ], F32) as COS, \
            nc.sbuf_tensor(nm("SIN"), [64, SEQ], F32) as SIN, \
            nc.sbuf_tensor(nm("WUKV"), [128, 2, 2048], BF16) as WUKV, \
            nc.sbuf_tensor(nm("kvg"), [128, 2], F32) as kvg, \
            nc.sbuf_tensor(nm("sq"), [128, 2, 2, 512], BF16) as sq, \
            nc.sbuf_tensor(nm("rb"), [128, 2, 512], F32) as rb, \
            nc.sbuf_tensor(nm("cnm"), [128, 2, 2, 512], BF16) as cnm_all, \
            nc.sbuf_tensor(nm("ko"), [128, 8, 512], BF16) as ko, \
            nc.sbuf_tensor(nm("vo"), [128, 2, D], BF16) as vo, \
            nc.sbuf_tensor(nm("r1"), [64, 512], F32) as r1, \
            nc.sbuf_tensor(nm("r2"), [64, 512], F32) as r2, \
            nc.sbuf_tensor(nm("kro"), [64, 2, 512], BF16) as kro:
        r_CKV, r_KR, r_tab, r_W, r_g, r_r1, r_r2 = (kb.res("k") for _ in range(7))
        r_sq2, r_rb2, r_cn2 = ([kb.res("k") for _ in range(2)] for _ in range(3))
        r_ko = [kb.res("ko") for _ in range(8)]
        r_vo = [kb.res("vo") for _ in range(2)]
        r_kro = [kb.res("kro") for _ in range(2)]
        kb.dma("sp", CKV[:], S["Z"][27 * 128:29 * 128, :].rearrange("(k p) t -> p k t", p=128), reads=[g.RS["Z"]], writes=[r_CKV])
        kb.dma("sp", KRa[:], S["Z"][29 * 128:29 * 128 + 64, :], reads=[g.RS["Z"]], writes=[r_KR])
        kb.dma("sp", KRb[:], S["Z"][29 * 128 + 64:30 * 128, :], reads=[g.RS["Z"]], writes=[r_KR])
        kb.dma("sp", COS[:], I["cosT"], reads=[g.r_in], writes=[r_tab])
        kb.dma("sp", SIN[:], I["sinT"], reads=[g.r_in], writes=[r_tab])
        kb.dma("pool", WUKV[:], I["w_ukv"][l].rearrange("(k p) n -> p k n", p=128), reads=[g.r_in], writes=[r_W])
        kb.dma("sp", kvg[:], I["kvg_col"][l], reads=[g.r_in], writes=[r_g])
        nko = nvo = 0
        for ti, (t0, T) in enumerate(TILES):
            pb_ = ti % 2
            cnm, r_cn = cnm_all[:, pb_], r_cn2[pb_]
            rms_feat(g, CKV, 2, t0, T, 256.0, kvg, r_g, sq[:, pb_], r_sq2[pb_], rb[:, pb_], r_rb2[pb_], cnm, r_cn, r_CKV, 0)
            for h in range(8):
                bank = 1 + (h % 3)
                pb = g.psum[bank]
                for k in range(2):
                    kb.op("pe", lambda e, k=k, h=h, pb=pb: e.matmul(pb[:, :T], lhsT=WUKV[:, k, h * 128:(h + 1) * 128], rhs=cnm[:, k, :T],
                                                                    start=(k == 0), stop=(k == 1)), reads=[r_W, r_cn], writes=[g.rps[bank]], inc=(k == 1))
                ki = nko % 8
                nko += 1
                if h % 2 == 0:
                    kb.op("act", lambda e, pb=pb, ki=ki: e.activation(out=ko[:, ki, :T], in_=pb[:, :T], func=AF.Copy), reads=[g.rps[bank]], writes=[r_ko[ki]])
                else:
                    kb.op("dve", lambda e, pb=pb, ki=ki: e.tensor_copy(out=ko[:, ki, :T], in_=pb[:, :T]), reads=[g.rps[bank]], writes=[r_ko[ki]])
                kb.dma("pool", S["KN"][h, :, t0:t0 + T], ko[:, ki, :T], reads=[r_ko[ki]], writes=[g.RS["KN"]])
            for sub in range(T // 128):
                vi = nvo % 2
                nvo += 1
                for half in range(2):
                    bank = 4 + half
                    pb = g.psum[bank]
                    for k in range(2):
                        kb.op("pe", lambda e, k=k, half=half, pb=pb, sub=sub: e.matmul(
                            pb[:], lhsT=cnm[:, k, sub * 128:(sub + 1) * 128], rhs=WUKV[:, k, 1024 + half * 512:1536 + half * 512],
                            start=(k == 0), stop=(k == 1)), reads=[r_W, r_cn], writes=[g.rps[bank]], inc=(k == 1))
                    if half == 0:
                        kb.op("act", lambda e, pb=pb, vi=vi: e.activation(out=vo[:, vi, 0:512], in_=pb[:], func=AF.Copy), reads=[g.rps[bank]], writes=[r_vo[vi]])
                    else:
                        kb.op("dve", lambda e, pb=pb, vi=vi: e.tensor_copy(out=vo[:, vi, 512:1024], in_=pb[:]), reads=[g.rps[bank]], writes=[r_vo[vi]])
                kb.dma("pool", S["V"][t0 + sub * 128:t0 + (sub + 1) * 128, :], vo[:, vi, :], reads=[r_vo[vi]], writes=[g.RS["V"]])
            oi = ti % 2
            if ti == 0:
                kb.op("dve", lambda e, oi=oi: e.tensor_copy(out=kro[:, oi, :T], in_=KRa[:, t0:t0 + T]), reads=[r_KR], writes=[r_kro[oi]])
            else:
                q0 = t0 - CTX
                kb.op("dve", lambda e: e.tensor_tensor(out=r1[:, :T], in0=KRa[:, t0:t0 + T], in1=COS[:, q0:q0 + T], op=ALU.mult), reads=[r_KR, r_tab], writes=[r_r1])
                kb.op("dve", lambda e: e.tensor_tensor(out=r2[:, :T], in0=KRb[:, t0:t0 + T], in1=SIN[:, q0:q0 + T], op=ALU.mult), reads=[r_KR, r_tab], writes=[r_r2])
                kb.op("dve", lambda e, oi=oi: e.tensor_tensor(out=kro[:, oi, :T], in0=r1[:, :T], in1=r2[:, :T], op=ALU.add), reads=[r_r1, r_r2], writes=[r_kro[oi]])
            kb.dma("pool", S["KRD"][:, t0:t0 + T], kro[:, oi, :T], reads=[r_kro[oi]], writes=[g.RS["KRD"]])


def phase_q(g, l, loc=False):
    nc, kb, I, S = g.nc, g.kb, g.I, g.S
    NT = HALF if loc else LT
    NR = HALF if loc else SEQ
    tiles = [(t0, T, False) for (t0, T) in LTILES] if loc else [(t0, T, t0 == 0) for (t0, T) in TILES]
    qoff = 0 if loc else CTX
    cq_src, r_cq = (S["CQl"], g.RS["CQl"]) if loc else (S["Z"][24 * 128:27 * 128, :], g.RS["Z"])
    cos_src, sin_src = (I["cosQ"], I["sinQ"]) if loc else (I["cosT"], I["sinT"])
    QN_dst, QR_dst = ("QNl", "QRl") if loc else ("QN", "QR")
    with nc.sbuf_tensor(nm("CQ"), [128, 3, NT], BF16) as CQ, \
            nc.sbuf_tensor(nm("COS"), [64, NR], F32) as COS, \
            nc.sbuf_tensor(nm("SIN"), [64, NR], F32) as SIN, \
            nc.sbuf_tensor(nm("WUQ"), [128, 3, 2048], BF16) as WUQ, \
            nc.sbuf_tensor(nm("qg"), [128, 3], F32) as qg, \
            nc.sbuf_tensor(nm("sq"), [128, 2, 3, 512], BF16) as sq, \
            nc.sbuf_tensor(nm("rb"), [128, 2, 512], F32) as rb, \
            nc.sbuf_tensor(nm("cnm"), [128, 2, 3, 512], BF16) as cnm_all, \
            nc.sbuf_tensor(nm("qo"), [128, 8, 512], BF16) as qo, \
            nc.sbuf_tensor(nm("r1"), [64, 512], F32) as r1, \
            nc.sbuf_tensor(nm("r2"), [64, 512], F32) as r2, \
            nc.sbuf_tensor(nm("qro"), [64, 8, 512], BF16) as qro:
        r_CQ, r_tab, r_W, r_g, r_r1, r_r2 = (kb.res("q") for _ in range(6))
        r_sq2, r_rb2, r_cn2 = ([kb.res("q") for _ in range(2)] for _ in range(3))
        r_qo = [kb.res("qo") for _ in range(8)]
        r_qro = [kb.res("qro") for _ in range(8)]
        kb.dma("sp", CQ[:], cq_src.rearrange("(k p) t -> p k t", p=128), reads=[r_cq], writes=[r_CQ])
        kb.dma("sp", COS[:], cos_src, reads=[g.r_in], writes=[r_tab])
        kb.dma("sp", SIN[:], sin_src, reads=[g.r_in], writes=[r_tab])
        kb.dma("pool", WUQ[:], I["w_uq"][l].rearrange("(k p) n -> p k n", p=128), reads=[g.r_in], writes=[r_W])
        kb.dma("sp", qg[:], I["qg_col"][l], reads=[g.r_in], writes=[r_g])
        nq = 0
        for ti, (t0, T, isctx) in enumerate(tiles):
            pb_ = ti % 2
            cnm, r_cn = cnm_all[:, pb_], r_cn2[pb_]
            rms_feat(g, CQ, 3, t0, T, 384.0, qg, r_g, sq[:, pb_], r_sq2[pb_], rb[:, pb_], r_rb2[pb_], cnm, r_cn, r_CQ, 0)
            for h in range(8):
                qi = nq % 8
                nq += 1
                bn, br, bp = 1 + 3 * (h % 2), 2 + 3 * (h % 2), 3 + 3 * (h % 2)
                for k in range(3):
                    kb.op("pe", lambda e, k=k, h=h: e.matmul(g.psum[bn][:, :T], lhsT=WUQ[:, k, h * 256:h * 256 + 128], rhs=cnm[:, k, :T],
                                                             start=(k == 0), stop=(k == 2)), reads=[r_W, r_cn], writes=[g.rps[bn]], inc=(k == 2))
                for k in range(3):
                    kb.op("pe", lambda e, k=k, h=h: e.matmul(g.psum[br][0:64, :T], lhsT=WUQ[:, k, h * 256 + 128:h * 256 + 192], rhs=cnm[:, k, :T],
                                                             start=(k == 0), stop=(k == 2)), reads=[r_W, r_cn], writes=[g.rps[br]], inc=(k == 2))
                kb.op("act", lambda e, qi=qi: e.activation(out=qo[:, qi, :T], in_=g.psum[bn][:, :T], func=AF.Copy, scale=MLA_SCALE),
                      reads=[g.rps[bn]], writes=[r_qo[qi]])
                kb.dma("pool", S[QN_dst][h, :, t0:t0 + T], qo[:, qi, :T], reads=[r_qo[qi]], writes=[g.RS[QN_dst]])
                if isctx:
                    kb.op("act", lambda e, qi=qi: e.activation(out=qro[:, qi, :T], in_=g.psum[br][0:64, :T], func=AF.Copy, scale=MLA_SCALE),
                          reads=[g.rps[br]], writes=[r_qro[qi]])
                else:
                    q0 = t0 - qoff
                    for k in range(3):
                        kb.op("pe", lambda e, k=k, h=h: e.matmul(g.psum[bp][0:64, :T], lhsT=WUQ[:, k, h * 256 + 192:h * 256 + 256], rhs=cnm[:, k, :T],
                                                                 start=(k == 0), stop=(k == 2)), reads=[r_W, r_cn], writes=[g.rps[bp]], inc=(k == 2))
                    kb.op("dve", lambda e: e.tensor_tensor(out=r1[:, :T], in0=g.psum[br][0:64, :T], in1=COS[:, q0:q0 + T], op=ALU.mult),
                          reads=[g.rps[br], r_tab], writes=[r_r1])
                    kb.op("dve", lambda e: e.scalar_tensor_tensor(out=r2[:, :T], in0=g.psum[bp][0:64, :T], scalar=MLA_SCALE, in1=SIN[:, q0:q0 + T],
                                                                 op0=ALU.mult, op1=ALU.mult), reads=[g.rps[bp], r_tab], writes=[r_r2])
                    kb.op("dve", lambda e, qi=qi: e.scalar_tensor_tensor(out=qro[:, qi, :T], in0=r1[:, :T], scalar=MLA_SCALE, in1=r2[:, :T],
                                                                        op0=ALU.mult, op1=ALU.add), reads=[r_r1, r_r2], writes=[r_qro[qi]])
                kb.dma("pool", S[QR_dst][h, :, t0:t0 + T], qro[:, qi, :T], reads=[r_qro[qi]], writes=[g.RS[QR_dst]])


def phase_att(g, l, loc=False, att_heads=8):
    nc, kb, I, S = g.nc, g.kb, g.I, g.S
    NKT = LT // 128
    tiles = [(t0, T, False) for (t0, T) in LTILES] if loc else [(t0, T, t0 == 0) for (t0, T) in TILES]
    QN_src, QR_src, AT_dst = ("QNl", "QRl", "ATl") if loc else ("QN", "QR", "AT")
    with nc.sbuf_tensor(nm("KNh"), [128, LT], BF16) as KNh, \
            nc.sbuf_tensor(nm("KRD"), [128, LT], BF16) as KRD, \
            nc.sbuf_tensor(nm("Vh"), [128, NKT, 128], BF16) as Vh, \
            nc.sbuf_tensor(nm("QNb"), [128, 2, 512], BF16) as QNb, \
            nc.sbuf_tensor(nm("QRb"), [128, 2, 512], BF16) as QRb, \
            nc.sbuf_tensor(nm("PT"), [128, 8, 512], BF16) as PT, \
            nc.sbuf_tensor(nm("rl"), [128, 512], F32) as rl, \
            nc.sbuf_tensor(nm("ahl"), [128, 2, 512], BF16) as ahl, \
            nc.sbuf_tensor(nm("atmp"), [128, 512], F32) as atmp, \
            nc.sbuf_tensor(nm("ob"), [128, 2, 512], BF16) as ob:
        r_KN, r_KR, r_V, r_rl, r_ahl, r_atmp = (kb.res("a") for _ in range(6))
        r_Q = [kb.res("Q") for _ in range(2)]
        r_PT = [kb.res("PT") for _ in range(8)]
        r_ob = [kb.res("ob") for _ in range(2)]
        kb.dma("sp", KRD[0:64, :], S["KRD"], reads=[g.RS["KRD"]], writes=[r_KR])
        kb.dma("sp", KRD[64:128, :], S["KRD"], reads=[g.RS["KRD"]], writes=[r_KR])
        bLp, bLr = 6, 7
        nq = 0
        pending = [None]
        for h in range(att_heads):
            kb.dma("sp", KNh[:], S["KN"][h], reads=[g.RS["KN"]], writes=[r_KN])
            kb.dma("sp", Vh[:], S["V"][:, h * 128:(h + 1) * 128].rearrange("(kt p) d -> p kt d", p=128), reads=[g.RS["V"]], writes=[r_V])
            for ti, (t0, T, isctx) in enumerate(tiles):
                nk = 2 if isctx else NKT
                npair = nk // 2
                ngrp = (nk + 3) // 4
                qi = nq % 2
                nq += 1
                bO = 4 + qi
                kb.dma("sp", QNb[:, qi, :T], S[QN_src][h, :, t0:t0 + T], reads=[g.RS[QN_src]], writes=[r_Q[qi]])
                kb.dma("sp", QRb[0:64, qi, :T], S[QR_src][h, :, t0:t0 + T], reads=[g.RS[QR_src]], writes=[r_Q[qi]])
                kb.dma("sp", QRb[64:128, qi, :T], S[QR_src][h, :, t0:t0 + T], reads=[g.RS[QR_src]], writes=[r_Q[qi]])

                def emit_S_pair(p):
                    k0, k1 = 2 * p, 2 * p + 1
                    b0, b1 = k0 % 4, k1 % 4
                    kb.op("pe", lambda e: e.matmul(g.psum[b0][:, :T], lhsT=KNh[:, k0 * 128:(k0 + 1) * 128], rhs=QNb[:, qi, :T], start=True, stop=False),
                          reads=[r_KN, r_Q[qi]], writes=[g.rps[b0]], inc=False)
                    kb.op("pe", lambda e: e.matmul(g.psum[b1][:, :T], lhsT=KNh[:, k1 * 128:(k1 + 1) * 128], rhs=QNb[:, qi, :T], start=True, stop=False),
                          reads=[r_KN, r_Q[qi]], writes=[g.rps[b1]], inc=False)
                    kb.op("pe", lambda e: e.matmul(g.psum[b0][:, :T], lhsT=KRD[0:64, k0 * 128:(k0 + 1) * 128], rhs=QRb[0:64, qi, :T], start=False, stop=True,
                                                   tile_position=(0, 0)), reads=[r_KR, r_Q[qi]], writes=[g.rps[b0]], inc=False)
                    kb.op("pe", lambda e: e.matmul(g.psum[b1][:, :T], lhsT=KRD[64:128, k1 * 128:(k1 + 1) * 128], rhs=QRb[64:128, qi, :T], start=False, stop=True,
                                                   tile_position=(64, 0)), reads=[r_KR, r_Q[qi]], writes=[g.rps[b1], g.rps[b0]])

                emit_S_pair(0)
                for p in range(npair):
                    for kt in (2 * p, 2 * p + 1):
                        kb.op("act", lambda e, kt=kt: e.activation(out=PT[:, kt % 8, :T], in_=g.psum[kt % 4][:, :T], func=AF.Exp),
                              reads=[g.rps[kt % 4]], writes=[r_PT[kt % 8]])
                    if p + 1 < npair:
                        emit_S_pair(p + 1)
                    if p == 0 and pending[0] is not None:
                        pending[0]()
                        pending[0] = None
                    for kt in (2 * p, 2 * p + 1):
                        kb.op("pe", lambda e, kt=kt: e.matmul(g.psum[bO][:, :T], lhsT=Vh[:, kt, :], rhs=PT[:, kt % 8, :T], start=(kt == 0), stop=(kt == nk - 1)),
                              reads=[r_V, r_PT[kt % 8]], writes=[g.rps[bO]], inc=(kt % 2 == 1))
                    if p % 2 == 1 or p == npair - 1:
                        gi = p // 2
                        kts = [kt for kt in range(4 * gi, 4 * gi + 4) if kt < nk]
                        for kt in kts:
                            j = kt % 4
                            lastg = max(gg for gg in range(ngrp) if 4 * gg + j < nk)
                            kb.op("pe", lambda e, kt=kt, j=j, lastg=lastg: e.matmul(
                                g.psum[bLp][32 * j:32 * j + 32, :T], lhsT=g.ones[:, 0:32], rhs=PT[:, kt % 8, :T], start=(gi == 0), stop=(gi == lastg),
                                tile_position=(0, 32 * j)), reads=[g.r_ones, r_PT[kt % 8]], writes=[g.rps[bLp]], inc=(kt == kts[-1]))
                KR_ = 128 if nk >= 4 else 32 * nk
                kb.op("dve", lambda e: e.tensor_copy(out=ahl[:KR_, 0, :T], in_=g.psum[bLp][:KR_, :T]), reads=[g.rps[bLp]], writes=[r_ahl])
                kb.op("dve", lambda e: e.tensor_tensor(out=atmp[:KR_, :T], in0=g.psum[bLp][:KR_, :T], in1=ahl[:KR_, 0, :T], op=ALU.subtract),
                      reads=[g.rps[bLp], r_ahl], writes=[r_atmp])
                kb.op("dve", lambda e: e.tensor_copy(out=ahl[:KR_, 1, :T], in_=atmp[:KR_, :T]), reads=[r_atmp], writes=[r_ahl])
                def finish(T=T, KR_=KR_, qi=qi, bO=bO, h=h, t0=t0):
                    kb.op("pe", lambda e: e.matmul(g.psum[bLr][:, :T], lhsT=g.ones[:KR_, :], rhs=ahl[:KR_, 0, :T], start=True, stop=False),
                          reads=[g.r_ones, r_ahl], writes=[g.rps[bLr]], inc=False)
                    kb.op("pe", lambda e: e.matmul(g.psum[bLr][:, :T], lhsT=g.ones[:KR_, :], rhs=ahl[:KR_, 1, :T], start=False, stop=True),
                          reads=[g.r_ones, r_ahl], writes=[g.rps[bLr]])
                    kb.op("dve", lambda e: e.reciprocal(out=rl[:, :T], in_=g.psum[bLr][:, :T]), reads=[g.rps[bLr]], writes=[r_rl])
                    kb.op("dve", lambda e: e.scalar_tensor_tensor(out=ob[:, qi, :T], in0=g.psum[bO][:, :T], scalar=32.0, in1=rl[:, :T], op0=ALU.mult, op1=ALU.mult),
                          reads=[g.rps[bO], r_rl], writes=[r_ob[qi]])
                    kb.dma("pool", S[AT_dst][h * 128:(h + 1) * 128, t0:t0 + T], ob[:, qi, :T], reads=[r_ob[qi]], writes=[g.RS[AT_dst]])

                pending[0] = finish
        if pending[0] is not None:
            pending[0]()


def resid_epilogue(g, srcs, r_srcs, xsub, r_x, gr_idx, y1, r_y1, ss2, r_ss, junk, dst_ap, dst_res):
    kb = g.kb
    for cb in range(2):
        kb.op("act", lambda e, cb=cb: e.activation(out=junk[:, 0:512], in_=srcs[cb], func=AF.Square, accum_out=ss2[:, cb:cb + 1]),
              reads=[r_srcs[cb]], writes=[r_ss])
    kb.op("dve", lambda e: e.tensor_tensor(out=ss2[:, 2:3], in0=ss2[:, 0:1], in1=ss2[:, 1:2], op=ALU.add), reads=[r_ss], writes=[r_ss])
    kb.op("act", lambda e: e.activation(out=ss2[:, 3:4], in_=ss2[:, 2:3], func=AF.Sqrt, bias=EPS, scale=1.0 / D), reads=[r_ss], writes=[r_ss])
    kb.op("dve", lambda e: e.reciprocal(out=ss2[:, 3:4], in_=ss2[:, 3:4]), reads=[r_ss], writes=[r_ss])
    for cb in range(2):
        kb.op("dve", lambda e, cb=cb: e.scalar_tensor_tensor(out=y1[:, cb * 512:(cb + 1) * 512], in0=srcs[cb], scalar=ss2[:, 3:4],
                                                            in1=g.GR[:, gr_idx, cb * 512:(cb + 1) * 512], op0=ALU.mult, op1=ALU.mult),
              reads=[r_srcs[cb], r_ss, g.r_mod], writes=[r_y1])
    kb.op("dve", lambda e: e.tensor_tensor(out=y1[:], in0=y1[:], in1=xsub, op=ALU.add), reads=[r_y1, r_x], writes=[r_y1])
    kb.dma("pool", dst_ap, y1[:], reads=[r_y1], writes=[dst_res])


def phase_D1(g, l, Xsrc, r_X, loc=False):
    nc, kb, I, S = g.nc, g.kb, g.I, g.S
    tiles = [(t0, T, False) for (t0, T) in LTILES] if loc else [(t0, T, t0 == 0) for (t0, T) in TILES]
    srcs = ("MPl", "YLl", "ATl") if loc else ("MP", "YL", "AT")
    g_src, r_gsrc = (S["Gl"], g.RS["Gl"]) if loc else (S["Z"][30 * 128:54 * 128, :], g.RS["Z"])
    if loc:
        Xsrc, r_X = S["Xl"], g.RS["Xl"]
    X1_dst = "X1l" if loc else "X1"
    with nc.sbuf_tensor(nm("WP"), [128, 4, 8, D], BF16) as WP, \
            nc.sbuf_tensor(nm("act3"), [128, 2, 3, 8, 512], BF16) as act3, \
            nc.sbuf_tensor(nm("gts"), [128, 24, 512], BF16) as gts, \
            nc.sbuf_tensor(nm("xt"), [128, 4, D], F32) as xt, \
            nc.sbuf_tensor(nm("mgT"), [128, 8, 512], BF16) as mgT, \
            nc.sbuf_tensor(nm("ta"), [128, 512], F32) as ta, \
            nc.sbuf_tensor(nm("tb"), [128, 512], F32) as tb, \
            nc.sbuf_tensor(nm("y1"), [128, 2, D], F32) as y1, \
            nc.sbuf_tensor(nm("ss2"), [128, 2, 4], F32) as ss2, \
            nc.sbuf_tensor(nm("stg"), [128, 2, 1024], F32) as stg, \
            nc.sbuf_tensor(nm("junk"), [128, 512], BF16) as junk:
        r_stg = [kb.res("stg") for _ in range(2)]
        cnt = [0]
        r_WP = [kb.res("WP") for _ in range(4)]
        r_act = [kb.res("act3") for _ in range(2)]
        r_gts, r_xt, r_mg, r_ta, r_tb = (kb.res("d") for _ in range(5))
        r_y1 = [kb.res("y1") for _ in range(2)]
        r_ss = [kb.res("ss") for _ in range(2)]
        for wi, n in enumerate(("pool_proj", "lru_proj", "mla_proj", "w_out")):
            wsrc = I[n][l].rearrange("(k p) n -> p k n", p=128)
            for k in range(8):
                load_w(g, WP[:, wi, k, :], wsrc[:, k, :], r_WP[wi], stg, r_stg, cnt)
        ny = 0
        for ti, (t0, T, isctx) in enumerate(tiles):
            sel = 1 if isctx else 0
            nsub = T // 128
            ab = ti % 2
            for si, n in enumerate(srcs):
                kb.dma("sp", act3[:, ab, si, :, :T], S[n][:, t0:t0 + T].rearrange("(k p) t -> p k t", p=128), reads=[g.RS[n]], writes=[r_act[ab]])
            kb.dma("sp", gts[:, :, :T], g_src[:, t0:t0 + T].rearrange("(j p) t -> p j t", p=128), reads=[r_gsrc], writes=[r_gts])
            kb.dma("sp", xt[:, :nsub, :], Xsrc[t0:t0 + T, :].rearrange("(s p) d -> p s d", p=128), reads=[r_X], writes=[r_xt])
            for oc in range(8):
                banks = [(oc % 2) * 3 + i for i in range(3)]
                for si in range(3):
                    for k in range(8):
                        kb.op("pe", lambda e, si=si, k=k: e.matmul(g.psum[banks[si]][:, :T], lhsT=WP[:, si, k, oc * 128:(oc + 1) * 128],
                                                                  rhs=act3[:, ab, si, k, :T], start=(k == 0), stop=(k == 7)),
                              reads=[r_WP[si], r_act[ab]], writes=[g.rps[banks[si]]], inc=(k == 7))
                kb.op("dve", lambda e: e.tensor_tensor(out=ta[:, :T], in0=g.psum[banks[0]][:, :T], in1=gts[:, oc, :T], op=ALU.mult),
                      reads=[g.rps[banks[0]], r_gts], writes=[r_ta])
                kb.op("dve", lambda e: e.tensor_tensor(out=tb[:, :T], in0=g.psum[banks[1]][:, :T], in1=gts[:, 8 + oc, :T], op=ALU.mult),
                      reads=[g.rps[banks[1]], r_gts], writes=[r_tb])
                kb.op("dve", lambda e: e.tensor_tensor(out=ta[:, :T], in0=ta[:, :T], in1=tb[:, :T], op=ALU.add), reads=[r_ta, r_tb], writes=[r_ta])
                kb.op("dve", lambda e: e.tensor_tensor(out=tb[:, :T], in0=g.psum[banks[2]][:, :T], in1=gts[:, 16 + oc, :T], op=ALU.mult),
                      reads=[g.rps[banks[2]], r_gts], writes=[r_tb])
                kb.op("dve", lambda e: e.tensor_tensor(out=mgT[:, oc, :T], in0=ta[:, :T], in1=tb[:, :T], op=ALU.add), reads=[r_ta, r_tb], writes=[r_mg])
            for sub in range(nsub):
                for cb in range(2):
                    bank = 6 + cb
                    for k in range(8):
                        kb.op("pe", lambda e, k=k, cb=cb, bank=bank: e.matmul(g.psum[bank][:], lhsT=mgT[:, k, sub * 128:(sub + 1) * 128],
                                                                             rhs=WP[:, 3, k, cb * 512:(cb + 1) * 512], start=(k == 0), stop=(k == 7)),
                              reads=[r_WP[3], r_mg], writes=[g.rps[bank]], inc=(k == 7))
                yi = ny % 2
                ny += 1
                resid_epilogue(g, [g.psum[6][:], g.psum[7][:]], [g.rps[6], g.rps[7]], xt[:, sub, :], r_xt, 0 + sel, y1[:, yi], r_y1[yi],
                               ss2[:, yi], r_ss[yi], junk, S[X1_dst][t0 + sub * 128:t0 + (sub + 1) * 128, :], g.RS[X1_dst])


def phase_ffn_up(g, w1, w3, nf, Xsrc, r_X, HT, tiles, l):
    nc, kb, I, S = g.nc, g.kb, g.I, g.S
    dense = HT is None
    with nc.sbuf_tensor(nm("W1"), [128, 8, nf * 128], BF16) as W1, \
            nc.sbuf_tensor(nm("W3"), [128, 8, nf * 128], BF16) as W3, \
            nc.sbuf_tensor(nm("stg"), [128, 2, nf * 128], F32) as stg, \
            nc.sbuf_tensor(nm("XT"), [128, 4, D if dense else 8], F32) as XT, \
            nc.sbuf_tensor(nm("xn"), [128, 4, D if dense else 8], BF16) as xn, \
            nc.sbuf_tensor(nm("junk"), [128, D if dense else 8], BF16) as junk, \
            nc.sbuf_tensor(nm("hT"), [128, 2, 8, 512], BF16) as hT, \
            nc.sbuf_tensor(nm("ss"), [128, 4], F32) as ss, \
            nc.sbuf_tensor(nm("rstd"), [128, 4], F32) as rstd, \
            nc.sbuf_tensor(nm("sg"), [128, 2, 512], F32) as sg, \
            nc.sbuf_tensor(nm("ao"), [128, 8, 512], BF16) as ao:
        r_W1 = [kb.res("W1") for _ in range(8)]
        r_W3 = [kb.res("W3") for _ in range(8)]
        r_XT, r_xn, r_ss = (kb.res("f") for _ in range(3))
        r_hT = [kb.res("hT") for _ in range(2)]
        r_sg = [kb.res("sg") for _ in range(2)]
        r_ao = [kb.res("ao") for _ in range(8)]
        w1v = w1.rearrange("(k p) n -> p k n", p=128)
        w3v = w3.rearrange("(k p) n -> p k n", p=128)
        r_stg = [kb.res("stg") for _ in range(2)]
        cnt = [0]
        for k in range(8):
            load_w(g, W1[:, k, :], w1v[:, k, :], r_W1[k], stg, r_stg, cnt)
        for k in range(8):
            load_w(g, W3[:, k, :], w3v[:, k, :], r_W3[k], stg, r_stg, cnt)
        na = 0
        for ti, (t0, T) in enumerate(tiles):
            sel = 1 if t0 == 0 else 0
            nsub = T // 128
            b = ti % 2
            if HT is None:
                kb.dma("sp", XT[:, :nsub, :], Xsrc[t0:t0 + T, :].rearrange("(s p) d -> p s d", p=128), reads=[r_X], writes=[r_XT])
                norm_transpose(g, XT, r_XT, nsub, T, g.G2, g.mv[:, 24:32, :], sel, hT[:, b], r_hT[b], xn, r_xn, ss, rstd, r_ss, junk, [0, 1, 2, 3])
            else:
                kb.dma("sp", hT[:, b, :, :T], HT[:, t0:t0 + T].rearrange("(k p) t -> p k t", p=128), reads=[g.RS["H2T"]], writes=[r_hT[b]])
            for f in range(nf):
                b1, b3 = 4 + (f % 2) * 2, 5 + (f % 2) * 2
                for k in range(8):
                    kb.op("pe", lambda e, k=k: e.matmul(g.psum[b1][:, :T], lhsT=W1[:, k, f * 128:(f + 1) * 128], rhs=hT[:, b, k, :T],
                                                        start=(k == 0), stop=(k == 7)), reads=[r_W1[k], r_hT[b]], writes=[g.rps[b1]], inc=(k == 7))
                for k in range(8):
                    kb.op("pe", lambda e, k=k: e.matmul(g.psum[b3][:, :T], lhsT=W3[:, k, f * 128:(f + 1) * 128], rhs=hT[:, b, k, :T],
                                                        start=(k == 0), stop=(k == 7)), reads=[r_W3[k], r_hT[b]], writes=[g.rps[b3]], inc=(k == 7))
                si = f % 2
                ai = na % 8
                na += 1
                kb.op("act", lambda e: e.activation(out=sg[:, si, :T], in_=g.psum[b1][:, :T], func=AF.Silu), reads=[g.rps[b1]], writes=[r_sg[si]])
                kb.op("dve", lambda e: e.tensor_tensor(out=ao[:, ai, :T], in0=g.psum[b3][:, :T], in1=sg[:, si, :T], op=ALU.mult),
                      reads=[g.rps[b3], r_sg[si]], writes=[r_ao[ai]])
                kb.dma("pool", S["FA"][f * 128:(f + 1) * 128, t0:t0 + T], ao[:, ai, :T], reads=[r_ao[ai]], writes=[g.RS["FA"]])


def phase_ffn_down(g, w2, nf, tiles, l, first, last, e, dst, dst_res, dst_off, x1, r_x1):
    nc, kb, I, S = g.nc, g.kb, g.I, g.S
    moe = not (first and last)
    ya_in, ya_out = ("YA0", "YA1") if e % 2 == 1 else ("YA1", "YA0")
    with nc.sbuf_tensor(nm("W2"), [128, nf, D], BF16) as W2, \
            nc.sbuf_tensor(nm("aT"), [128, 2, nf, 512], BF16) as aT, \
            nc.sbuf_tensor(nm("xt"), [128, 4, D], F32) as xt, \
            nc.sbuf_tensor(nm("ya"), [128, 4, D], F32) as ya, \
            nc.sbuf_tensor(nm("gt"), [128, 4, NEXP], F32) as gt, \
            nc.sbuf_tensor(nm("y1"), [128, 2, D], F32) as y1, \
            nc.sbuf_tensor(nm("ss2"), [128, 2, 4], F32) as ss2, \
            nc.sbuf_tensor(nm("stg"), [128, 2, 1024], F32) as stg, \
            nc.sbuf_tensor(nm("junk"), [128, 512], BF16) as junk:
        r_stg = [kb.res("stg") for _ in range(2)]
        cnt = [0]
        r_W2, r_xt, r_ya, r_gt = (kb.res("w") for _ in range(4))
        r_aT = [kb.res("aT") for _ in range(2)]
        r_y1 = [kb.res("y1") for _ in range(2)]
        r_ss = [kb.res("ss") for _ in range(2)]
        w2v = w2.rearrange("(f p) n -> p f n", p=128)
        for f in range(nf):
            load_w(g, W2[:, f, :], w2v[:, f, :], r_W2, stg, r_stg, cnt)
        ny = 0
        for ti, (t0, T) in enumerate(tiles):
            sel = 1 if t0 == 0 else 0
            nsub = T // 128
            ab = ti % 2
            kb.dma("sp", aT[:, ab, :, :T], S["FA"][0:nf * 128, t0:t0 + T].rearrange("(f p) t -> p f t", p=128), reads=[g.RS["FA"]], writes=[r_aT[ab]])
            if last:
                kb.dma("sp", xt[:, :nsub, :], x1[t0:t0 + T, :].rearrange("(s p) d -> p s d", p=128), reads=[r_x1], writes=[r_xt])
            if moe:
                kb.dma("sp", gt[:, :nsub, :], S["GT"][t0:t0 + T, :].rearrange("(s p) e -> p s e", p=128), reads=[g.RS["GT"]], writes=[r_gt])
                if not first:
                    kb.dma("sp", ya[:, :nsub, :], S[ya_in][t0:t0 + T, :].rearrange("(s p) d -> p s d", p=128), reads=[g.RS[ya_in]], writes=[r_ya])
            for sub in range(nsub):
                bb = 4 * (sub % 2)
                for cb in range(2):
                    bank = bb + cb
                    for f in range(nf):
                        kb.op("pe", lambda e_, f=f, cb=cb, bank=bank: e_.matmul(g.psum[bank][:], lhsT=aT[:, ab, f, sub * 128:(sub + 1) * 128],
                                                                               rhs=W2[:, f, cb * 512:(cb + 1) * 512], start=(f == 0), stop=(f == nf - 1)),
                              reads=[r_W2, r_aT[ab]], writes=[g.rps[bank]], inc=(f == nf - 1))
                yi = ny % 2
                ny += 1
                dst_ap = dst[t0 - dst_off + sub * 128:t0 - dst_off + (sub + 1) * 128, :]
                if not moe:
                    resid_epilogue(g, [g.psum[bb][:], g.psum[bb + 1][:]], [g.rps[bb], g.rps[bb + 1]], xt[:, sub, :], r_xt, 2 + sel, y1[:, yi], r_y1[yi],
                                   ss2[:, yi], r_ss[yi], junk, dst_ap, dst_res)
                else:
                    for cb in range(2):
                        yv = ya[:, sub, cb * 512:(cb + 1) * 512]
                        if first:
                            kb.op("dve", lambda e_, cb=cb, yv=yv: e_.tensor_scalar(out=yv, in0=g.psum[bb + cb][:], scalar1=gt[:, sub, e:e + 1], scalar2=None,
                                                                                  op0=ALU.mult), reads=[g.rps[bb + cb], r_gt], writes=[r_ya])
                        else:
                            kb.op("dve", lambda e_, cb=cb, yv=yv: e_.scalar_tensor_tensor(out=yv, in0=g.psum[bb + cb][:], scalar=gt[:, sub, e:e + 1], in1=yv,
                                                                                         op0=ALU.mult, op1=ALU.add), reads=[g.rps[bb + cb], r_gt, r_ya], writes=[r_ya])
                    if last:
                        resid_epilogue(g, [ya[:, sub, 0:512], ya[:, sub, 512:1024]], [r_ya, r_ya], xt[:, sub, :], r_xt, 2, y1[:, yi], r_y1[yi],
                                       ss2[:, yi], r_ss[yi], junk, dst_ap, dst_res)
            if moe and not last:
                kb.dma("pool", S[ya_out][t0:t0 + T, :].rearrange("(s p) d -> p s d", p=128), ya[:, :nsub, :], reads=[r_ya], writes=[g.RS[ya_out]])


def phase_router(g, l, x1, r_x1):
    nc, kb, I, S = g.nc, g.kb, g.I, g.S
    with nc.sbuf_tensor(nm("RW"), [128, NEXP, D], F32) as RW, \
            nc.sbuf_tensor(nm("XT"), [128, 2, 4, D], F32) as XT, \
            nc.sbuf_tensor(nm("xn"), [128, 4, D], BF16) as xn, \
            nc.sbuf_tensor(nm("h2"), [128, D], F32) as h2, \
            nc.sbuf_tensor(nm("junkf"), [128, D], F32) as junkf, \
            nc.sbuf_tensor(nm("junk"), [128, D], BF16) as junk, \
            nc.sbuf_tensor(nm("hT"), [128, 2, 8, 512], BF16) as hT, \
            nc.sbuf_tensor(nm("ss"), [128, 2, 4], F32) as ss, \
            nc.sbuf_tensor(nm("rstd"), [128, 2, 4], F32) as rstd, \
            nc.sbuf_tensor(nm("lg"), [128, 4, NEXP], F32) as lg, \
            nc.sbuf_tensor(nm("mx"), [128, 4, 8], F32) as mx, \
            nc.sbuf_tensor(nm("sm"), [128, 4, 4], F32) as sm, \
            nc.sbuf_tensor(nm("ge"), [128, 4, NEXP], F32) as ge, \
            nc.sbuf_tensor(nm("mk"), [128, 4, NEXP], F32) as mk, \
            nc.sbuf_tensor(nm("go"), [128, 2, 4, NEXP], F32) as go:
        r_RW, r_xn, r_h2, r_lg, r_mx, r_sm, r_ge, r_mk = (kb.res("r") for _ in range(8))
        r_XT = [kb.res("XT") for _ in range(2)]
        r_hT = [kb.res("hT") for _ in range(2)]
        r_ss = [kb.res("ss") for _ in range(2)]
        r_go = [kb.res("go") for _ in range(2)]
        for e in range(NEXP):
            kb.dma("sp", RW[:, e, :], I["router_wT"][0, e].partition_broadcast(128), reads=[g.r_in], writes=[r_RW])
        for ti, (t0, T) in enumerate(LTILES):
            nsub = T // 128
            b = ti % 2
            kb.dma("sp", XT[:, b, :nsub, :], x1[t0:t0 + T, :].rearrange("(s p) d -> p s d", p=128), reads=[r_x1], writes=[r_XT[b]])
            norm_transpose(g, XT[:, b], r_XT[b], nsub, T, g.G2, g.mv[:, 24:32, :], 0, hT[:, b], r_hT[b], xn, r_xn,
                           ss[:, b], rstd[:, b], r_ss[b], junk, [0, 1, 2, 3])
            kb.dma("pool", S["H2T"][:, t0:t0 + T].rearrange("(k p) t -> p k t", p=128), hT[:, b, :, :T], reads=[r_hT[b]], writes=[g.RS["H2T"]])
            for s in range(nsub):
                kb.op("dve", lambda e_, s=s: e_.scalar_tensor_tensor(out=h2[:], in0=XT[:, b, s, :], scalar=rstd[:, b, s:s + 1], in1=g.MR[:, 0, :],
                                                                    op0=ALU.mult, op1=ALU.mult), reads=[r_XT[b], r_ss[b], g.r_mod], writes=[r_h2])
                kb.op("dve", lambda e_: e_.tensor_tensor(out=h2[:], in0=h2[:], in1=g.MR[:, 1, :], op=ALU.add), reads=[r_h2, g.r_mod], writes=[r_h2])
                for e in range(NEXP):
                    kb.op("dve", lambda e_, e=e, s=s: e_.scalar_tensor_tensor(out=junkf[:], in0=h2[:], scalar=1.0, in1=RW[:, e, :], op0=ALU.mult, op1=ALU.mult,
                                                                             accum_out=lg[:, s, e:e + 1]), reads=[r_h2, r_RW], writes=[r_lg])
                kb.op("dve", lambda e_, s=s: e_.max(out=mx[:, s, :], in_=lg[:, s, :]), reads=[r_lg], writes=[r_mx])
                kb.op("dve", lambda e_, s=s: e_.tensor_scalar(out=sm[:, s, 0:1], in0=mx[:, s, 0:1], scalar1=-1.0, scalar2=None, op0=ALU.mult),
                      reads=[r_mx], writes=[r_sm])
                kb.op("act", lambda e_, s=s: e_.activation(out=ge[:, s, :], in_=lg[:, s, :], func=AF.Exp, bias=sm[:, s, 0:1]), reads=[r_lg, r_sm], writes=[r_ge])
                kb.op("dve", lambda e_, s=s: e_.tensor_scalar(out=mk[:, s, :], in0=lg[:, s, :], scalar1=mx[:, s, 1:2], scalar2=None, op0=ALU.is_ge),
                      reads=[r_lg, r_mx], writes=[r_mk])
                kb.op("dve", lambda e_, s=s: e_.scalar_tensor_tensor(out=ge[:, s, :], in0=ge[:, s, :], scalar=1.0, in1=mk[:, s, :], op0=ALU.mult, op1=ALU.mult,
                                                                    accum_out=sm[:, s, 1:2]), reads=[r_ge, r_mk], writes=[r_ge, r_sm])
                kb.op("dve", lambda e_, s=s: e_.reciprocal(out=sm[:, s, 2:3], in_=sm[:, s, 1:2]), reads=[r_sm], writes=[r_sm])
                kb.op("dve", lambda e_, s=s: e_.tensor_scalar(out=go[:, b, s, :], in0=ge[:, s, :], scalar1=sm[:, s, 2:3], scalar2=None, op0=ALU.mult),
                      reads=[r_ge, r_sm], writes=[r_go[b]])
            kb.dma("pool", S["GT"][t0:t0 + T, :].rearrange("(s p) e -> p s e", p=128), go[:, b, :nsub, :], reads=[r_go[b]], writes=[g.RS["GT"]])


def rope_perm():
    p = np.arange(64)
    half = (p % 32) // 16
    return np.where(half == 0, p + 16, p - 16)


def rope_tables():
    rows = SEQ // 64
    t = np.arange(SEQ)
    row = (t // 64).astype(np.float32)
    col = (t % 64).astype(np.float32)
    inv = (np.float32(10000.0) ** (-np.arange(16, dtype=np.float32) / np.float32(16))).astype(np.float32)
    cosT = np.zeros((64, SEQ), np.float32)
    sinT = np.zeros((64, SEQ), np.float32)
    for p in range(64):
        axis, half, f = p // 32, (p % 32) // 16, p % 16
        ang = (row if axis == 0 else col) * inv[f]
        cosT[p] = np.cos(ang)
        sinT[p] = np.sin(ang) * (-1.0 if half == 0 else 1.0)
    return cosT, sinT


def host_inputs(inp):
    perm = rope_perm()
    w_in = inp["w_in"]
    w_in_aug = np.concatenate([w_in[:, :, :3776], w_in[:, :, 3712:3776][:, :, perm], w_in[:, :, 3776:]], axis=2)
    w_uq = inp["w_uq"].reshape(2, 384, 8, 192)
    w_uq_aug = np.concatenate([w_uq, w_uq[:, :, :, 128:][:, :, :, perm]], axis=3).reshape(2, 384, 2048)
    w_ukv = inp["w_ukv"].reshape(2, 256, 8, 256)
    w_ukv_aug = np.concatenate([w_ukv[:, :, :, :128].reshape(2, 256, 1024), w_ukv[:, :, :, 128:].reshape(2, 256, 1024)], axis=2)
    cosT, sinT = rope_tables()
    shared = {k: np.ascontiguousarray(v) for k, v in inp.items() if k not in ("x", "c", "ctx", "c_ctx", "w_in", "w_uq", "w_ukv")}
    shared["w_in"] = np.ascontiguousarray(w_in_aug)
    shared["w_uq"] = np.ascontiguousarray(w_uq_aug)
    shared["w_ukv"] = np.ascontiguousarray(w_ukv_aug)
    shared["ident"] = np.eye(128, dtype=np.float32)
    del shared["router_w"]
    shared["router_wT"] = np.ascontiguousarray(inp["router_w"].transpose(0, 2, 1))
    shared["mod_b_col"] = np.ascontiguousarray(inp["mod_b"].reshape(2, 48, 128).transpose(0, 2, 1))
    vecs = {"pre_mix_g": inp["pre_mix_g"], "pre_ffn_g": inp["pre_ffn_g"], "pool_scale": inp["pool_scale"], "conv_b": inp["conv_b"]}
    for k in range(4):
        vecs["conv_w%d" % k] = inp["conv_w"][:, k]
    for d in range(2):
        vecs["gate_a_b%d" % d] = inp["gate_a_b"][:, d]
        vecs["gate_x_b%d" % d] = inp["gate_x_b"][:, d]
        vecs["lru_lambda%d" % d] = inp["lru_lambda"][:, d]
    vc = np.stack([vecs[n] for n in VNAMES], axis=1)
    shared["vcol"] = np.ascontiguousarray(vc.reshape(2, NV, 8, 128).transpose(0, 3, 1, 2))
    shared["qg_col"] = np.ascontiguousarray(inp["q_norm_g"].reshape(2, 3, 128).transpose(0, 2, 1))
    shared["kvg_col"] = np.ascontiguousarray(inp["kv_norm_g"].reshape(2, 2, 128).transpose(0, 2, 1))
    shared["cosT"] = cosT
    shared["sinT"] = sinT
    maps = []
    for c in range(8):
        b, half = c % 4, c // 4
        m = dict(shared)
        m["cosQ"] = np.ascontiguousarray(cosT[:, half * HALF:(half + 1) * HALF])
        m["sinQ"] = np.ascontiguousarray(sinT[:, half * HALF:(half + 1) * HALF])
        hmv = np.zeros((128, 2), np.float32)
        hmv[:, half] = 1.0
        m["hm"] = hmv
        m["xall"] = np.ascontiguousarray(np.concatenate([inp["ctx"][b], inp["x"][b]], axis=0))
        cc = np.stack([inp["c"][b], inp["c_ctx"]], axis=1)
        m["ccol"] = np.ascontiguousarray(cc.reshape(8, 128, 2).transpose(1, 0, 2))
        maps.append(m)
    return maps


def kernel(**inputs):
    inp = {k: np.asarray(v) for k, v in inputs.items()}
    maps = host_inputs(inp)
    nc = bass.Bass("TRN2", target_bir_lowering=False)
    build(nc)
    res = run_bass_kernel_spmd(nc, maps, core_ids=list(range(8)))
    return np.stack([np.concatenate([res.results[b]["out"], res.results[b + 4]["out"]], axis=0) for b in range(4)], axis=0).astype(np.float32)
```

```python
import numpy as np
import concourse.bass as bass
import concourse.mybir as mybir
from concourse.bass_utils import run_bass_kernel_spmd

F32 = mybir.dt.float32
BF16 = mybir.dt.bfloat16
AF = mybir.ActivationFunctionType
ALU = mybir.AluOpType
AX = mybir.AxisListType

D = 1024
SEQ = 8192
CTX = 256
LT = SEQ + CTX
NCH = 54
ZW = NCH * 128
EPS = 1e-6
DFF = 2816
EFF = 3584
NEXP = 8
MLA_SCALE = 192 ** -0.5
VNAMES = ["pre_mix_g", "pre_ffn_g", "pool_scale", "conv_b", "conv_w0", "conv_w1", "conv_w2", "conv_w3",
          "gate_a_b0", "gate_a_b1", "gate_x_b0", "gate_x_b1", "lru_lambda0", "lru_lambda1"]
NV = len(VNAMES)
VI = {n: i for i, n in enumerate(VNAMES)}
TILES = [(0, 256)] + [(256 + 512 * i, 512) for i in range(16)]
HALF = SEQ // 2
LTILES = [(512 * i, 512) for i in range(8)]


class Res:
    __slots__ = ("name", "w", "r", "dsem", "dcnt", "dq")

    def __init__(self, name):
        self.name = name
        self.w = None
        self.r = {}
        self.dsem = None
        self.dcnt = 0
        self.dq = None


class KB:
    def __init__(self, nc):
        self.nc = nc
        self.eng = {"pe": nc.tensor, "dve": nc.vector, "act": nc.scalar, "pool": nc.gpsimd, "sp": nc.sync}
        self.esem = {n: nc.alloc_semaphore("es_" + n) for n in self.eng}
        self.ecnt = {n: 0 for n in self.eng}
        self.seen = {n: {} for n in self.eng}
        self.nres = 0
        self.ndsem = 0
        self.local = []
        self.persist = []
        self.free_dsems = {"pool": [], "sp": []}

    def res(self, name="r", persist=False):
        self.nres += 1
        r = Res(name + str(self.nres))
        (self.persist if persist else self.local).append(r)
        return r

    def end_phase(self):
        deps = [(self.esem[e], self.ecnt[e]) for e in self.eng if self.ecnt[e] > 0]
        for r in self.local + self.persist:
            if r.dsem is not None:
                deps.append((r.dsem, r.dcnt))
        for en in self.eng:
            self._wait(en, deps)
        for r in self.local:
            if r.dsem is not None:
                self.free_dsems[r.dq].append((r.dsem, r.dcnt))
                r.dsem = None
                r.dq = None
        self.local = []
        for r in self.persist:
            r.w = None
            r.r = {}

    def _deps(self, reads, writes, dma=False):
        deps = []
        for r in reads:
            if r.w is not None:
                deps.append(r.w)
        for w in writes:
            if w.w is not None and not (dma and w.dsem is not None and w.w[0] is w.dsem):
                deps.append(w.w)
            for s, v in w.r.values():
                deps.append((s, v))
        return deps

    def _wait(self, en, deps):
        own = self.esem[en]
        seen = self.seen[en]
        for sem, val in deps:
            if sem is own and en == "pe":
                continue
            key = id(sem)
            if seen.get(key, 0) >= val:
                continue
            self.eng[en].wait_ge(sem, val)
            seen[key] = val

    def _record(self, tok, reads, writes):
        key = id(tok[0])
        for r in reads:
            old = r.r.get(key)
            if old is None or old[1] < tok[1]:
                r.r[key] = tok
        for w in writes:
            w.w = tok
            w.r = {}

    def op(self, en, fn, reads=(), writes=(), inc=True):
        self._wait(en, self._deps(reads, writes))
        inst = fn(self.eng[en])
        if inc:
            self.ecnt[en] += 1
            inst.then_inc(self.esem[en], 1)
            tok = (self.esem[en], self.ecnt[en])
        else:
            tok = (self.esem[en], self.ecnt[en] + 1)
        self._record(tok, reads, writes)
        return inst

    def dma(self, en, out, in_, reads=(), writes=()):
        self._wait(en, self._deps(reads, writes, dma=True))
        inst = self.eng[en].dma_start(out=out, in_=in_)
        w = writes[0]
        if w.dsem is None:
            w.dq = en
            if self.free_dsems[en]:
                w.dsem, w.dcnt = self.free_dsems[en].pop()
            else:
                w.dsem = self.nc.alloc_semaphore("ds_" + w.name)
                self.ndsem += 1
        assert w.dq == en, (w.name, w.dq, en)
        w.dcnt += 16
        inst.then_inc(w.dsem, 16)
        tok = (w.dsem, w.dcnt)
        self._record(tok, reads, writes)
        return inst

    def wait_all(self, en, ress):
        deps = []
        for r in ress:
            if r.w is not None:
                deps.append(r.w)
        self._wait(en, deps)


class Ctx:
    pass


def g_dbg_layer(dbg):
    return 1 if "L1" in dbg else 0


def g_att_heads(dbg):
    for d in dbg:
        if d.startswith("heads"):
            return int(d[5:])
    return 8


_uid = [0]


def nm(s):
    _uid[0] += 1
    return "t%d_%s" % (_uid[0], s)


def build(nc, n_layers=2, dbg=(), stop_after=None):
    kb = KB(nc)
    g = Ctx()
    g.nc, g.kb = nc, kb
    g.dbg = {}

    def din(name, shape, dt=F32):
        return nc.dram_tensor(name, list(shape), dt, kind="ExternalInput").ap()

    def dscr(name, shape, dt):
        t = nc.dram_tensor(name, list(shape), dt, kind="Internal").ap()
        return t

    I = {}
    I["xall"] = din("xall", [LT, D])
    I["ccol"] = din("ccol", [128, 8, 2])
    I["ident"] = din("ident", [128, 128])
    I["cosT"] = din("cosT", [64, SEQ])
    I["sinT"] = din("sinT", [64, SEQ])
    I["cosQ"] = din("cosQ", [64, HALF])
    I["sinQ"] = din("sinQ", [64, HALF])
    I["hm"] = din("hm", [128, 2])
    I["mod_w"] = din("mod_w", [2, D, 6 * D])
    I["mod_b"] = din("mod_b", [2, 6 * D])
    I["mod_b_col"] = din("mod_b_col", [2, 128, 48])
    I["vcol"] = din("vcol", [2, 128, NV, 8])
    I["qg_col"] = din("qg_col", [2, 128, 3])
    I["kvg_col"] = din("kvg_col", [2, 128, 2])
    for n in ("pre_mix_g", "post_mix_g", "pre_ffn_g", "post_ffn_g", "pool_scale", "conv_b"):
        I[n] = din(n, [2, D])
    I["w_in"] = din("w_in", [2, D, ZW])
    I["pool_w"] = din("pool_w", [2, 4, 256, 256])
    I["pool_proj"] = din("pool_proj", [2, D, D])
    I["conv_w"] = din("conv_w", [2, 4, D])
    I["gate_a_w"] = din("gate_a_w", [2, 2, 8, 128, 128])
    I["gate_x_w"] = din("gate_x_w", [2, 2, 8, 128, 128])
    I["gate_a_b"] = din("gate_a_b", [2, 2, D])
    I["gate_x_b"] = din("gate_x_b", [2, 2, D])
    I["lru_lambda"] = din("lru_lambda", [2, 2, D])
    I["lru_proj"] = din("lru_proj", [2, D, D])
    I["q_norm_g"] = din("q_norm_g", [2, 384])
    I["w_uq"] = din("w_uq", [2, 384, 2048])
    I["kv_norm_g"] = din("kv_norm_g", [2, 256])
    I["w_ukv"] = din("w_ukv", [2, 256, 2048])
    I["mla_proj"] = din("mla_proj", [2, D, D])
    I["w_out"] = din("w_out", [2, D, D])
    I["ffn_w1"] = din("ffn_w1", [1, D, DFF])
    I["ffn_w3"] = din("ffn_w3", [1, D, DFF])
    I["ffn_w2"] = din("ffn_w2", [1, DFF, D])
    I["router_wT"] = din("router_wT", [1, NEXP, D])
    I["moe_w1"] = din("moe_w1", [1, NEXP, D, EFF])
    I["moe_w3"] = din("moe_w3", [1, NEXP, D, EFF])
    I["moe_w2"] = din("moe_w2", [1, NEXP, EFF, D])
    g.I = I
    out = nc.dram_tensor("out", [HALF, D], F32, kind="ExternalOutput").ap()
    g.out = out
    g.r_out = kb.res("out", persist=True)

    S = {}
    S["Z"] = dscr("sZ", [ZW, LT], BF16)
    for n in ("MP", "YL", "AT"):
        S[n] = dscr("s" + n, [D, LT], BF16)
    S["KN"] = dscr("sKN", [8, 128, LT], BF16)
    S["KRD"] = dscr("sKRD", [64, LT], BF16)
    S["V"] = dscr("sV", [LT, D], BF16)
    S["QN"] = dscr("sQN", [8, 128, LT], BF16)
    S["QR"] = dscr("sQR", [8, 64, LT], BF16)
    S["X1"] = dscr("sX1", [LT, D], F32)
    S["X2"] = dscr("sX2", [LT, D], F32)
    S["FA"] = dscr("sFA", [EFF, LT], BF16)
    S["H2T"] = dscr("sH2T", [D, LT], BF16)
    S["GT"] = dscr("sGT", [LT, NEXP], F32)
    S["YA0"] = dscr("sYA0", [LT, D], F32)
    S["YA1"] = dscr("sYA1", [LT, D], F32)
    S["CQl"] = dscr("sCQl", [384, HALF], BF16)
    S["Gl"] = dscr("sGl", [3072, HALF], BF16)
    S["MPl"] = dscr("sMPl", [D, HALF], BF16)
    S["YLl"] = dscr("sYLl", [D, HALF], BF16)
    S["ATl"] = dscr("sATl", [D, HALF], BF16)
    S["Xl"] = dscr("sXl", [HALF, D], F32)
    S["X1l"] = dscr("sX1l", [HALF, D], F32)
    S["QNl"] = dscr("sQNl", [8, 128, HALF], BF16)
    S["QRl"] = dscr("sQRl", [8, 64, HALF], BF16)
    g.S = S
    g.RS = {k: kb.res("s" + k, persist=True) for k in S}
    g.r_in = kb.res("inputs", persist=True)

    def dbg_out(name, src_ap, src_res, shape, dt):
        o = nc.dram_tensor("dbg_" + name, list(shape), dt, kind="ExternalOutput").ap()
        r = kb.res("dbg" + name, persist=True)
        kb.dma("sp", o, src_ap, reads=[src_res], writes=[r])
        g.dbg[name] = r

    def sb(name, shape, dt):
        return nc.alloc_sbuf_tensor(nm(name), list(shape), dt)

    g.ident = sb("ident", [128, 128], BF16)
    g.r_ident = kb.res("ident", persist=True)
    kb.dma("pool", g.ident[:], I["ident"], reads=[g.r_in], writes=[g.r_ident])
    g.ones = sb("ones", [128, 128], BF16)
    g.r_ones = kb.res("ones", persist=True)
    kb.op("dve", lambda e: e.memset(g.ones[:], 1.0), writes=[g.r_ones])
    g.mv = sb("mv", [128, 48, 2], F32)
    g.G1 = sb("G1", [128, 8, 2], F32)
    g.G2 = sb("G2", [128, 8, 2], F32)
    g.GR = sb("GR", [128, 4, D], F32)
    g.MR = sb("MR", [128, 2, D], F32)
    g.r_mod = kb.res("mod", persist=True)
    g.psum = [nc.alloc_psum_tensor("ps%d" % i, [128, 512], F32) for i in range(8)]
    g.rps = [kb.res("ps", persist=True) for i in range(8)]

    def dbgS(name, dt=BF16):
        if name in dbg:
            dbg_out(name, S[name], g.RS[name], list(S[name].shape), dt)

    for l in range(n_layers):
        dl = (l == g_dbg_layer(dbg))
        Xsrc, r_X = (I["xall"], g.r_in) if l == 0 else (S["X2"], g.RS["X2"])
        phase_mod(g, l)
        kb.end_phase()
        phase_A(g, l, Xsrc, r_X)
        kb.end_phase()
        if dl:
            dbgS("Z")
        if stop_after == "A":
            break
        phase_pool(g, l)
        kb.end_phase()
        if dl:
            dbgS("MP")
        if stop_after == "pool":
            break
        phase_lru(g, l)
        kb.end_phase()
        if dl:
            dbgS("YL")
        if stop_after == "lru":
            break
        phase_kv(g, l)
        kb.end_phase()
        loc = (l == n_layers - 1) and n_layers == 2
        if loc:
            phase_sel(g)
            kb.end_phase()
        phase_q(g, l, loc)
        kb.end_phase()
        if dl:
            dbgS("KN"); dbgS("KRD"); dbgS("V"); dbgS("QN"); dbgS("QR")
        if stop_after == "kvq":
            break
        phase_att(g, l, loc, att_heads=g_att_heads(dbg))
        kb.end_phase()
        if dl:
            dbgS("AT")
        if stop_after == "att":
            break
        phase_D1(g, l, Xsrc, r_X, loc)
        kb.end_phase()
        if dl:
            dbgS("X1", F32)
        if stop_after == "D1":
            break
        if l == 0:
            phase_ffn_up(g, I["ffn_w1"][0], I["ffn_w3"][0], DFF // 128, S["X1"], g.RS["X1"], None, TILES, l)
            kb.end_phase()
            phase_ffn_down(g, I["ffn_w2"][0], DFF // 128, TILES, l, first=True, last=True, e=0, dst=S["X2"], dst_res=g.RS["X2"], dst_off=0,
                           x1=S["X1"], r_x1=g.RS["X1"])
            kb.end_phase()
            if dl:
                dbgS("X2", F32)
        else:
            phase_router(g, l, S["X1l"], g.RS["X1l"])
            kb.end_phase()
            if dl:
                dbgS("GT", F32)
            for e in range(NEXP):
                phase_ffn_up(g, I["moe_w1"][0, e], I["moe_w3"][0, e], EFF // 128, None, None, S["H2T"], LTILES, l)
                kb.end_phase()
                phase_ffn_down(g, I["moe_w2"][0, e], EFF // 128, LTILES, l, first=(e == 0), last=(e == NEXP - 1), e=e,
                               dst=g.out, dst_res=g.r_out, dst_off=0, x1=S["X1l"], r_x1=g.RS["X1l"])
                kb.end_phase()

    fin = [g.r_out] + list(g.dbg.values())
    kb.wait_all("sp", fin)
    return g


def load_w(g, dst, src, r_dst, stg, r_stg, cnt):
    kb = g.kb
    n = dst.shape[-1]
    cw = stg.shape[-1]
    for c0 in range(0, n, cw):
        c1 = min(n, c0 + cw)
        b = cnt[0] % 2
        cnt[0] += 1
        kb.dma("sp", stg[:, b, :c1 - c0], src[:, c0:c1], reads=[g.r_in], writes=[r_stg[b]])
        if b == 0:
            kb.op("dve", lambda e, b=b, c0=c0, c1=c1: e.tensor_copy(out=dst[:, c0:c1], in_=stg[:, b, :c1 - c0]), reads=[r_stg[b]], writes=[r_dst])
        else:
            kb.op("act", lambda e, b=b, c0=c0, c1=c1: e.activation(out=dst[:, c0:c1], in_=stg[:, b, :c1 - c0], func=AF.Copy), reads=[r_stg[b]], writes=[r_dst])


def phase_mod(g, l):
    nc, kb, I = g.nc, g.kb, g.I
    with nc.sbuf_tensor(nm("MW"), [128, 8, 6 * D], BF16) as MW, \
            nc.sbuf_tensor(nm("cc"), [128, 8, 2], F32) as cc, \
            nc.sbuf_tensor(nm("sc"), [128, 8, 2], BF16) as sc, \
            nc.sbuf_tensor(nm("scb"), [128, 8, 2, 128], BF16) as scb, \
            nc.sbuf_tensor(nm("modb_col"), [128, 48], F32) as modb_col, \
            nc.sbuf_tensor(nm("gcol"), [128, 2, 8], F32) as gcol, \
            nc.sbuf_tensor(nm("modb_bc"), [128, 2, D], F32) as modb_bc, \
            nc.sbuf_tensor(nm("postg_bc"), [128, 2, D], F32) as postg_bc, \
            nc.sbuf_tensor(nm("modb_bc2"), [128, 2, D], F32) as modb_bc2, \
            nc.sbuf_tensor(nm("preg_bc"), [128, D], F32) as preg_bc, \
            nc.sbuf_tensor(nm("stg"), [128, 2, 2048], F32) as stg, \
            nc.sbuf_tensor(nm("mtmp"), [128, 512], F32) as mtmp:
        r_stg = [kb.res("stg") for _ in range(2)]
        cnt = [0]
        r_MW = [kb.res("MW") for _ in range(8)]
        r_c, r_sc, r_scb, r_mb, r_gc, r_bc, r_tmp, r_bc2 = (kb.res("m") for _ in range(8))
        mw = I["mod_w"][l].rearrange("(k p) n -> p k n", p=128)
        for k in range(8):
            load_w(g, MW[:, k, :], mw[:, k, :], r_MW[k], stg, r_stg, cnt)
        kb.dma("sp", cc[:], I["ccol"], reads=[g.r_in], writes=[r_c])
        kb.dma("sp", modb_col[:], I["mod_b_col"][l], reads=[g.r_in], writes=[r_mb])
        kb.dma("sp", gcol[:], I["vcol"][l, :, 0:2, :], reads=[g.r_in], writes=[r_gc])
        kb.dma("sp", modb_bc[:, 0, :], I["mod_b"][l, 2 * D:3 * D].partition_broadcast(128), reads=[g.r_in], writes=[r_bc])
        kb.dma("sp", modb_bc[:, 1, :], I["mod_b"][l, 5 * D:6 * D].partition_broadcast(128), reads=[g.r_in], writes=[r_bc])
        kb.dma("sp", postg_bc[:, 0, :], I["post_mix_g"][l].partition_broadcast(128), reads=[g.r_in], writes=[r_bc])
        kb.dma("sp", postg_bc[:, 1, :], I["post_ffn_g"][l].partition_broadcast(128), reads=[g.r_in], writes=[r_bc])
        kb.op("act", lambda e: e.activation(out=sc[:], in_=cc[:], func=AF.Silu), reads=[r_c], writes=[r_sc])
        for k in range(8):
            for s in range(2):
                kb.op("dve", lambda e, k=k, s=s: e.tensor_copy(out=scb[:, k, s, :], in_=sc[:, k, s:s + 1].to_broadcast([128, 128])),
                      reads=[r_sc], writes=[r_scb])
        for j in range(48):
            bank = 0 if j < 24 else 3
            ps = g.psum[bank]
            c0 = (j % 24) * 16
            for k in range(8):
                kb.op("pe", lambda e, j=j, k=k, ps=ps, c0=c0: e.matmul(ps[:, c0:c0 + 2], lhsT=MW[:, k, j * 128:(j + 1) * 128], rhs=sc[:, k, :],
                                                                      start=(k == 0), stop=(k == 7)),
                      reads=[r_MW[k], r_sc], writes=[g.rps[bank]], inc=(j % 24 == 23 and k == 7))
        for hb, bank in enumerate((0, 3)):
            ps = g.psum[bank]
            kb.op("dve", lambda e, ps=ps, hb=hb: e.tensor_tensor(
                out=g.mv[:, hb * 24:(hb + 1) * 24, :], in0=ps[:, 0:384].rearrange("p (j s) -> p j s", s=16)[:, :, 0:2],
                in1=modb_col[:, hb * 24:(hb + 1) * 24].unsqueeze(2).to_broadcast([128, 24, 2]), op=ALU.add),
                reads=[g.rps[bank], r_mb], writes=[g.r_mod])
        kb.op("dve", lambda e: e.scalar_tensor_tensor(out=g.G1[:], in0=g.mv[:, 8:16, :], scalar=1.0,
                                                      in1=gcol[:, 0, :].unsqueeze(2).to_broadcast([128, 8, 2]), op0=ALU.add, op1=ALU.mult),
              reads=[g.r_mod, r_gc], writes=[g.r_mod])
        kb.op("dve", lambda e: e.scalar_tensor_tensor(out=g.G2[:], in0=g.mv[:, 32:40, :], scalar=1.0,
                                                      in1=gcol[:, 1, :].unsqueeze(2).to_broadcast([128, 8, 2]), op0=ALU.add, op1=ALU.mult),
              reads=[g.r_mod, r_gc], writes=[g.r_mod])
        if l == 1:
            kb.dma("sp", modb_bc2[:, 0, :], I["mod_b"][l, 3 * D:4 * D].partition_broadcast(128), reads=[g.r_in], writes=[r_bc2])
            kb.dma("sp", modb_bc2[:, 1, :], I["mod_b"][l, 4 * D:5 * D].partition_broadcast(128), reads=[g.r_in], writes=[r_bc2])
            kb.dma("sp", preg_bc[:], I["pre_ffn_g"][l].partition_broadcast(128), reads=[g.r_in], writes=[r_bc2])
            for part in range(2):
                col0 = (3 + part) * D
                for cb in range(2):
                    bank = 1 + cb
                    pb = g.psum[bank]
                    for k in range(8):
                        kb.op("pe", lambda e, k=k, pb=pb, c0=col0 + cb * 512: e.matmul(
                            pb[:], lhsT=scb[:, k, 0, :], rhs=MW[:, k, c0:c0 + 512], start=(k == 0), stop=(k == 7)),
                            reads=[r_MW[k], r_scb], writes=[g.rps[bank]], inc=(k == 7))
                    cs = slice(cb * 512, (cb + 1) * 512)
                    if part == 0:
                        kb.op("dve", lambda e, pb=pb, cs=cs: e.tensor_tensor(out=g.MR[:, 1, cs], in0=pb[:], in1=modb_bc2[:, 0, cs], op=ALU.add),
                              reads=[g.rps[bank], r_bc2], writes=[g.r_mod])
                    else:
                        kb.op("dve", lambda e, pb=pb, cs=cs: e.tensor_tensor(out=mtmp[:], in0=pb[:], in1=modb_bc2[:, 1, cs], op=ALU.add),
                              reads=[g.rps[bank], r_bc2], writes=[r_tmp])
                        kb.op("dve", lambda e, cs=cs: e.scalar_tensor_tensor(out=g.MR[:, 0, cs], in0=mtmp[:], scalar=1.0, in1=preg_bc[:, cs],
                                                                            op0=ALU.add, op1=ALU.mult), reads=[r_tmp, r_bc2], writes=[g.r_mod])
        n = 0
        for part in range(2):
            col0 = (2 if part == 0 else 5) * D
            for s in range(2):
                for cb in range(2):
                    bank = 1 + (n % 2)
                    n += 1
                    pb = g.psum[bank]
                    for k in range(8):
                        kb.op("pe", lambda e, k=k, s=s, pb=pb, c0=col0 + cb * 512: e.matmul(
                            pb[:], lhsT=scb[:, k, s, :], rhs=MW[:, k, c0:c0 + 512], start=(k == 0), stop=(k == 7)),
                            reads=[r_MW[k], r_scb], writes=[g.rps[bank]], inc=(k == 7))
                    kb.op("dve", lambda e, pb=pb, part=part, cb=cb: e.tensor_tensor(
                        out=mtmp[:], in0=pb[:], in1=modb_bc[:, part, cb * 512:(cb + 1) * 512], op=ALU.add),
                        reads=[g.rps[bank], r_bc], writes=[r_tmp])
                    kb.op("dve", lambda e, part=part, s=s, cb=cb: e.tensor_tensor(
                        out=g.GR[:, part * 2 + s, cb * 512:(cb + 1) * 512], in0=mtmp[:],
                        in1=postg_bc[:, part, cb * 512:(cb + 1) * 512], op=ALU.mult),
                        reads=[r_tmp, r_bc], writes=[g.r_mod])


def rms_rows(g, xt, r_x, nsub, ss, rstd, r_ss, junk, width=D):
    kb = g.kb
    for s in range(nsub):
        kb.op("act", lambda e, s=s: e.activation(out=junk[:], in_=xt[:, s, :], func=AF.Square, accum_out=ss[:, s:s + 1]),
              reads=[r_x], writes=[r_ss])
    kb.op("act", lambda e: e.activation(out=rstd[:, :nsub], in_=ss[:, :nsub], func=AF.Sqrt, bias=EPS, scale=1.0 / width),
          reads=[r_ss], writes=[r_ss])
    kb.op("dve", lambda e: e.reciprocal(out=rstd[:, :nsub], in_=rstd[:, :nsub]), reads=[r_ss], writes=[r_ss])


def norm_transpose(g, xt, r_x, nsub, T, Gv, Sv, sel, hT, r_hT, xn, r_xn, ss, rstd, r_ss, junk, pt_banks):
    kb = g.kb
    rms_rows(g, xt, r_x, nsub, ss, rstd, r_ss, junk)
    for s in range(nsub):
        kb.op("dve", lambda e, s=s: e.tensor_scalar(out=xn[:, s, :], in0=xt[:, s, :], scalar1=rstd[:, s:s + 1], scalar2=None, op0=ALU.mult),
              reads=[r_x, r_ss], writes=[r_xn])
    for k in range(8):
        bank = pt_banks[k // 2]
        pv = g.psum[bank][:].bitcast(BF16)
        off = (k % 2) * 512
        for s in range(nsub):
            kb.op("pe", lambda e, k=k, s=s, pv=pv, off=off: e.transpose(pv[:, off + s * 128:off + (s + 1) * 128],
                                                                       xn[:, s, k * 128:(k + 1) * 128], g.ident[:]),
                  reads=[r_xn, g.r_ident], writes=[g.rps[bank]], inc=(s == nsub - 1))
        kb.op("act", lambda e, k=k, pv=pv, off=off: e.activation(out=hT[:, k, :T], in_=pv[:, off:off + T], func=AF.Identity,
                                                                 bias=Sv[:, k, sel:sel + 1], scale=Gv[:, k, sel:sel + 1]),
              reads=[g.rps[bank], g.r_mod], writes=[r_hT])


def phase_A(g, l, Xsrc, r_X):
    nc, kb, I, S = g.nc, g.kb, g.I, g.S
    with nc.sbuf_tensor(nm("WIN"), [128, 8, ZW], BF16) as WIN, \
            nc.sbuf_tensor(nm("XT"), [128, 1, 4, D], F32) as XT, \
            nc.sbuf_tensor(nm("xn"), [128, 4, D], BF16) as xn, \
            nc.sbuf_tensor(nm("junk"), [128, D], BF16) as junk, \
            nc.sbuf_tensor(nm("hT"), [128, 2, 8, 512], BF16) as hT, \
            nc.sbuf_tensor(nm("ss"), [128, 2, 4], F32) as ss, \
            nc.sbuf_tensor(nm("rstd"), [128, 2, 4], F32) as rstd, \
            nc.sbuf_tensor(nm("stg"), [128, 2, 1152], F32) as stg, \
            nc.sbuf_tensor(nm("zo"), [128, 12, 512], BF16) as zo:
        r_stg = [kb.res("stg") for _ in range(2)]
        cnt = [0]
        r_W = [kb.res("WIN") for _ in range(8)]
        r_XT = [kb.res("XT") for _ in range(1)]
        r_xn = kb.res("xn")
        r_hT = [kb.res("hT") for _ in range(2)]
        r_ss = [kb.res("ss") for _ in range(2)]
        r_zo = [kb.res("zo") for _ in range(12)]
        wv = I["w_in"][l].rearrange("(k p) n -> p k n", p=128)
        for k in range(8):
            load_w(g, WIN[:, k, :], wv[:, k, :], r_W[k], stg, r_stg, cnt)
        nz = 0
        for ti, (t0, T) in enumerate(TILES):
            sel = 1 if ti == 0 else 0
            nsub = T // 128
            b = ti % 2
            kb.dma("sp", XT[:, 0, :nsub, :], Xsrc[t0:t0 + T, :].rearrange("(s p) d -> p s d", p=128), reads=[r_X], writes=[r_XT[0]])
            norm_transpose(g, XT[:, 0], r_XT[0], nsub, T, g.G1, g.mv[:, 0:8, :], sel, hT[:, b], r_hT[b], xn, r_xn,
                           ss[:, b], rstd[:, b], r_ss[b], junk, [0, 1, 2, 3])
            for j in range(NCH):
                bank = 4 + (j % 4)
                pb = g.psum[bank]
                for k in range(8):
                    kb.op("pe", lambda e, j=j, k=k, pb=pb: e.matmul(pb[:, :T], lhsT=WIN[:, k, j * 128:(j + 1) * 128], rhs=hT[:, b, k, :T],
                                                                    start=(k == 0), stop=(k == 7)),
                          reads=[r_W[k], r_hT[b]], writes=[g.rps[bank]], inc=(k == 7))
                zi = nz % 12
                nz += 1
                if j >= 30:
                    kb.op("act", lambda e, pb=pb, zi=zi: e.activation(out=zo[:, zi, :T], in_=pb[:, :T], func=AF.Sigmoid),
                          reads=[g.rps[bank]], writes=[r_zo[zi]])
                else:
                    kb.op("dve", lambda e, pb=pb, zi=zi: e.tensor_copy(out=zo[:, zi, :T], in_=pb[:, :T]),
                          reads=[g.rps[bank]], writes=[r_zo[zi]])
                kb.dma("pool", S["Z"][j * 128:(j + 1) * 128, t0:t0 + T], zo[:, zi, :T], reads=[r_zo[zi]], writes=[g.RS["Z"]])


def phase_pool(g, l):
    nc, kb, I, S = g.nc, g.kb, g.I, g.S
    LPM = SEQ + 32
    with nc.sbuf_tensor(nm("ub"), [128, 2, LPM], BF16) as ub, \
            nc.sbuf_tensor(nm("T1"), [128, LPM], F32) as T1, \
            nc.sbuf_tensor(nm("T2"), [128, LPM], F32) as T2, \
            nc.sbuf_tensor(nm("M"), [128, 2, SEQ], BF16) as M, \
            nc.sbuf_tensor(nm("RC"), [128, SEQ], F32) as RC, \
            nc.sbuf_tensor(nm("PW"), [128, 4, 2, 256], BF16) as PW, \
            nc.sbuf_tensor(nm("pscol"), [128, 8], F32) as pscol, \
            nc.sbuf_tensor(nm("po"), [128, 8, 512], BF16) as po:
        r_ub = [kb.res("ub") for _ in range(2)]
        r_T1, r_T2, r_RC, r_PW, r_ps = (kb.res("p") for _ in range(5))
        r_M = [kb.res("M") for _ in range(2)]
        r_po = [kb.res("po") for _ in range(8)]
        kb.dma("pool", PW[:], I["pool_w"][l].rearrange("g (ic p) j -> p g ic j", p=128), reads=[g.r_in], writes=[r_PW])
        kb.dma("sp", pscol[:], I["vcol"][l, :, VI["pool_scale"], :], reads=[g.r_in], writes=[r_ps])
        npo = 0
        nb = 0
        for gi in range(4):
            w = 2 << gi
            hw = w // 2
            for (off, L) in ((0, CTX), (CTX, SEQ)):
                LP = L + 32
                kb.op("dve", lambda e: e.memset(RC[:, :L], 1.0 / w), writes=[r_RC])
                for t in range(hw):
                    kb.op("dve", lambda e, t=t: e.memset(RC[:, t:t + 1], 1.0 / (t + hw)), writes=[r_RC])
                for t in range(L - hw + 1, L):
                    kb.op("dve", lambda e, t=t: e.memset(RC[:, t:t + 1], 1.0 / (L - t + hw)), writes=[r_RC])
                for ch in range(2):
                    c = 2 * gi + ch
                    u = ub[:, ch, :]
                    kb.op("dve", lambda e, u=u: e.memset(u[:, 0:16], 0.0), writes=[r_ub[ch]])
                    kb.op("dve", lambda e, u=u: e.memset(u[:, 16 + L:32 + L], 0.0), writes=[r_ub[ch]])
                    kb.dma("sp", u[:, 16:16 + L], S["Z"][c * 128:(c + 1) * 128, off:off + L], reads=[g.RS["Z"]], writes=[r_ub[ch]])
                    kb.op("dve", lambda e, u=u: e.tensor_tensor(out=T1[:, 1:LP], in0=u[:, 0:LP - 1], in1=u[:, 1:LP], op=ALU.add),
                          reads=[r_ub[ch]], writes=[r_T1])
                    Sb, rS, Ob, rO = T1, r_T1, T2, r_T2
                    if w >= 4:
                        kb.op("dve", lambda e: e.tensor_tensor(out=T2[:, 2:LP - 1], in0=T1[:, 1:LP - 2], in1=T1[:, 3:LP], op=ALU.add),
                              reads=[r_T1], writes=[r_T2])
                        Sb, rS, Ob, rO = T2, r_T2, T1, r_T1
                    if w >= 8:
                        kb.op("dve", lambda e: e.tensor_tensor(out=T1[:, 4:LP - 3], in0=T2[:, 2:LP - 5], in1=T2[:, 6:LP - 1], op=ALU.add),
                              reads=[r_T2], writes=[r_T1])
                        Sb, rS, Ob, rO = T1, r_T1, T2, r_T2
                    if w >= 16:
                        kb.op("dve", lambda e: e.tensor_tensor(out=T2[:, 8:LP - 7], in0=T1[:, 4:LP - 11], in1=T1[:, 12:LP - 3], op=ALU.add),
                              reads=[r_T1], writes=[r_T2])
                        Sb, rS, Ob, rO = T2, r_T2, T1, r_T1
                    kb.op("dve", lambda e, Sb=Sb, Ob=Ob: e.tensor_tensor(out=Ob[:, 16:16 + L], in0=Sb[:, 16:16 + L], in1=RC[:, :L], op=ALU.mult),
                          reads=[rS, r_RC], writes=[rO])
                    kb.op("dve", lambda e, Ob=Ob, u=u, ch=ch: e.tensor_tensor(out=M[:, ch, :L], in0=Ob[:, 16:16 + L], in1=u[:, 16:16 + L], op=ALU.subtract),
                          reads=[rO, r_ub[ch]], writes=[r_M[ch]])
                for t0 in range(0, L, 512):
                    T = min(512, L - t0)
                    for jc in range(2):
                        bank = nb % 8
                        nb += 1
                        pb = g.psum[bank]
                        for ic in range(2):
                            kb.op("pe", lambda e, ic=ic, jc=jc, pb=pb, t0=t0, T=T: e.matmul(
                                pb[:, :T], lhsT=PW[:, gi, ic, jc * 128:(jc + 1) * 128], rhs=M[:, ic, t0:t0 + T], start=(ic == 0), stop=(ic == 1)),
                                reads=[r_PW, r_M[ic]], writes=[g.rps[bank]], inc=(ic == 1))
                        pi = npo % 8
                        npo += 1
                        c = 2 * gi + jc
                        kb.op("act", lambda e, pb=pb, pi=pi, T=T, c=c: e.activation(out=po[:, pi, :T], in_=pb[:, :T], func=AF.Identity,
                                                                                   scale=pscol[:, c:c + 1]),
                              reads=[g.rps[bank], r_ps], writes=[r_po[pi]])
                        kb.dma("pool", S["MP"][c * 128:(c + 1) * 128, off + t0:off + t0 + T], po[:, pi, :T], reads=[r_po[pi]], writes=[g.RS["MP"]])


def phase_lru(g, l):
    nc, kb, I, S = g.nc, g.kb, g.I, g.S
    with nc.sbuf_tensor(nm("LXG"), [128, LT], BF16) as LXG, \
            nc.sbuf_tensor(nm("UB"), [128, LT], BF16) as UB, \
            nc.sbuf_tensor(nm("T1"), [128, LT], F32) as T1, \
            nc.sbuf_tensor(nm("T2"), [128, LT], F32) as T2, \
            nc.sbuf_tensor(nm("T3"), [128, LT], F32) as T3, \
            nc.sbuf_tensor(nm("T4"), [128, LT], F32) as T4, \
            nc.sbuf_tensor(nm("GA"), [128, 2, 8, 128], BF16) as GA, \
            nc.sbuf_tensor(nm("GX"), [128, 2, 8, 128], BF16) as GX, \
            nc.sbuf_tensor(nm("vc"), [128, NV, 8], F32) as vc, \
            nc.sbuf_tensor(nm("cn"), [128, 2, 2, 8], F32) as cn:
        r_LXG, r_UB, r_T1, r_T2, r_T3, r_T4, r_GA, r_vc, r_cn = (kb.res("l") for _ in range(9))
        kb.dma("pool", GA[:], I["gate_a_w"][l].rearrange("d c j k -> j d c k"), reads=[g.r_in], writes=[r_GA])
        kb.dma("pool", GX[:], I["gate_x_w"][l].rearrange("d c j k -> j d c k"), reads=[g.r_in], writes=[r_GA])
        kb.dma("sp", vc[:], I["vcol"][l], reads=[g.r_in], writes=[r_vc])
        for d in range(2):
            lam = vc[:, VI["lru_lambda%d" % d], :]
            kb.op("act", lambda e, d=d, lam=lam: e.activation(out=cn[:, 0, d, :], in_=lam, func=AF.Exp, scale=-1.0), reads=[r_vc], writes=[r_cn])
            kb.op("act", lambda e, d=d: e.activation(out=cn[:, 0, d, :], in_=cn[:, 0, d, :], func=AF.Ln, bias=1.0), reads=[r_cn], writes=[r_cn])
            kb.op("dve", lambda e, d=d: e.tensor_scalar(out=cn[:, 1, d, :], in0=cn[:, 0, d, :], scalar1=-16.0, scalar2=None, op0=ALU.mult),
                  reads=[r_cn], writes=[r_cn])
            kb.op("dve", lambda e, d=d: e.tensor_scalar(out=cn[:, 0, d, :], in0=cn[:, 0, d, :], scalar1=-8.0, scalar2=None, op0=ALU.mult),
                  reads=[r_cn], writes=[r_cn])
        segs = ((0, CTX), (CTX, LT))
        nb = 0
        for c in range(8):
            def col(name):
                return vc[:, VI[name], c:c + 1]
            kb.dma("sp", LXG[:], S["Z"][(8 + c) * 128:(9 + c) * 128, :], reads=[g.RS["Z"]], writes=[r_LXG])
            for (s0, s1) in segs:
                kb.op("dve", lambda e, s0=s0, s1=s1: e.tensor_scalar(out=T1[:, s0:s1], in0=LXG[:, s0:s1], scalar1=col("conv_w2"), scalar2=col("conv_b"),
                                                                    op0=ALU.mult, op1=ALU.add), reads=[r_LXG, r_vc], writes=[r_T1])
                for k, o in ((0, -2), (1, -1), (3, 1)):
                    a, b = max(s0, s0 - o), min(s1, s1 - o)
                    kb.op("dve", lambda e, a=a, b=b, o=o, k=k: e.scalar_tensor_tensor(out=T1[:, a:b], in0=LXG[:, a + o:b + o], scalar=col("conv_w%d" % k),
                                                                                    in1=T1[:, a:b], op0=ALU.mult, op1=ALU.add),
                          reads=[r_LXG, r_vc, r_T1], writes=[r_T1])
            kb.op("act", lambda e: e.activation(out=UB[:], in_=T1[:], func=AF.Copy), reads=[r_T1], writes=[r_UB])
            kb.dma("sp", LXG[:], S["Z"][(16 + c) * 128:(17 + c) * 128, :], reads=[g.RS["Z"]], writes=[r_LXG])
            for d in range(2):
                Rb, rR = T1, r_T1
                Ib, rI = (T2, r_T2) if d == 0 else (T4, r_T4)
                Ab, rA = T3, r_T3
                for (t0, T) in TILES:
                    b0, b1 = nb % 8, (nb + 1) % 8
                    nb += 2
                    kb.op("pe", lambda e, t0=t0, T=T, b0=b0: e.matmul(g.psum[b0][:, :T], lhsT=GA[:, d, c, :], rhs=UB[:, t0:t0 + T], start=True, stop=True),
                          reads=[r_GA, r_UB], writes=[g.rps[b0]])
                    kb.op("pe", lambda e, t0=t0, T=T, b1=b1: e.matmul(g.psum[b1][:, :T], lhsT=GX[:, d, c, :], rhs=UB[:, t0:t0 + T], start=True, stop=True),
                          reads=[r_GA, r_UB], writes=[g.rps[b1]])
                    kb.op("act", lambda e, t0=t0, T=T, b0=b0, Rb=Rb: e.activation(out=Rb[:, t0:t0 + T], in_=g.psum[b0][:, :T], func=AF.Sigmoid,
                                                                                 bias=col("gate_a_b%d" % d)), reads=[g.rps[b0], r_vc], writes=[rR])
                    kb.op("act", lambda e, t0=t0, T=T, b1=b1, Ib=Ib: e.activation(out=Ib[:, t0:t0 + T], in_=g.psum[b1][:, :T], func=AF.Sigmoid,
                                                                                 bias=col("gate_x_b%d" % d)), reads=[g.rps[b1], r_vc], writes=[rI])
                kb.op("act", lambda e, Ab=Ab, Rb=Rb: e.activation(out=Ab[:], in_=Rb[:], func=AF.Exp, scale=cn[:, 0, d, c:c + 1]), reads=[rR, r_cn], writes=[rA])
                kb.op("act", lambda e, Rb=Rb: e.activation(out=Rb[:], in_=Rb[:], func=AF.Exp, scale=cn[:, 1, d, c:c + 1]), reads=[rR, r_cn], writes=[rR])
                kb.op("act", lambda e, Rb=Rb: e.activation(out=Rb[:], in_=Rb[:], func=AF.Sqrt, bias=1.0, scale=-1.0), reads=[rR], writes=[rR])
                kb.op("dve", lambda e, Rb=Rb, Ib=Ib: e.tensor_tensor(out=Rb[:], in0=Rb[:], in1=Ib[:], op=ALU.mult), reads=[rR, rI], writes=[rR])
                kb.op("dve", lambda e, Rb=Rb: e.tensor_tensor(out=Rb[:], in0=Rb[:], in1=UB[:], op=ALU.mult), reads=[rR, r_UB], writes=[rR])
                if d == 0:
                    kb.op("dve", lambda e, Ab=Ab, Rb=Rb, Ib=Ib: e.tensor_tensor_scan(out=Ib[:, 0:CTX], data0=Ab[:, 0:CTX], data1=Rb[:, 0:CTX], initial=0.0,
                                                                                   op0=ALU.mult, op1=ALU.add), reads=[rA, rR], writes=[rI])
                    kb.op("dve", lambda e, Ab=Ab, Rb=Rb, Ib=Ib: e.tensor_tensor_scan(out=Ib[:, CTX:LT], data0=Ab[:, CTX:LT], data1=Rb[:, CTX:LT],
                                                                                   initial=Ib[:, CTX - 1:CTX], op0=ALU.mult, op1=ALU.add),
                          reads=[rA, rR, rI], writes=[rI])
                else:
                    kb.op("dve", lambda e, Ab=Ab, Rb=Rb, Ib=Ib: e.tensor_tensor_scan(out=Ib[:, CTX - 1::-1], data0=Ab[:, CTX - 1::-1], data1=Rb[:, CTX - 1::-1],
                                                                                   initial=0.0, op0=ALU.mult, op1=ALU.add), reads=[rA, rR], writes=[rI])
                    kb.op("dve", lambda e, Ab=Ab, Rb=Rb, Ib=Ib: e.tensor_tensor_scan(out=Ib[:, LT - 1:CTX - 1:-1], data0=Ab[:, LT - 1:CTX - 1:-1],
                                                                                   data1=Rb[:, LT - 1:CTX - 1:-1], initial=Ib[:, 0:1],
                                                                                   op0=ALU.mult, op1=ALU.add), reads=[rA, rR, rI], writes=[rI])
            kb.op("dve", lambda e: e.tensor_tensor(out=T2[:], in0=T2[:], in1=T4[:], op=ALU.add), reads=[r_T2, r_T4], writes=[r_T2])
            kb.op("dve", lambda e: e.tensor_tensor(out=T1[:], in0=LXG[:], in1=LXG[:], op=ALU.mult), reads=[r_LXG], writes=[r_T1])
            kb.op("dve", lambda e: e.tensor_scalar(out=T1[:], in0=T1[:], scalar1=0.044715, scalar2=1.0, op0=ALU.mult, op1=ALU.add), reads=[r_T1], writes=[r_T1])
            kb.op("dve", lambda e: e.tensor_tensor(out=T1[:], in0=T1[:], in1=LXG[:], op=ALU.mult), reads=[r_T1, r_LXG], writes=[r_T1])
            kb.op("act", lambda e: e.activation(out=T1[:], in_=T1[:], func=AF.Sigmoid, scale=1.5957691216057308), reads=[r_T1], writes=[r_T1])
            kb.op("dve", lambda e: e.tensor_tensor(out=T1[:], in0=T1[:], in1=LXG[:], op=ALU.mult), reads=[r_T1, r_LXG], writes=[r_T1])
            kb.op("dve", lambda e: e.tensor_tensor(out=UB[:], in0=T1[:], in1=T2[:], op=ALU.mult), reads=[r_T1, r_T2], writes=[r_UB])
            kb.dma("pool", S["YL"][c * 128:(c + 1) * 128, :], UB[:], reads=[r_UB], writes=[g.RS["YL"]])


def phase_sel(g):
    nc, kb, I, S = g.nc, g.kb, g.I, g.S
    with nc.sbuf_tensor(nm("hm"), [128, 2], F32) as hm, \
            nc.sbuf_tensor(nm("sa"), [128, 2, HALF], BF16) as sa, \
            nc.sbuf_tensor(nm("sbb"), [128, 2, HALF], BF16) as sbb, \
            nc.sbuf_tensor(nm("so"), [128, 2, HALF], BF16) as so, \
            nc.sbuf_tensor(nm("xa"), [128, 2, 4, D], F32) as xa, \
            nc.sbuf_tensor(nm("xb"), [128, 2, 4, D], F32) as xb, \
            nc.sbuf_tensor(nm("xo"), [128, 2, 4, D], F32) as xo:
        r_hm = kb.res("hm")
        r_sa = [kb.res("sa") for _ in range(2)]
        r_sb = [kb.res("sb") for _ in range(2)]
        r_so = [kb.res("so") for _ in range(2)]
        kb.dma("sp", hm[:], I["hm"], reads=[g.r_in], writes=[r_hm])
        n = 0
        for src, r_src, dst, nrows in ((S["Z"][24 * 128:27 * 128, :], g.RS["Z"], "CQl", 384), (S["Z"][30 * 128:54 * 128, :], g.RS["Z"], "Gl", 3072),
                                      (S["MP"], g.RS["MP"], "MPl", D), (S["YL"], g.RS["YL"], "YLl", D)):
            for rc in range(nrows // 128):
                b = n % 2
                n += 1
                rows = slice(rc * 128, (rc + 1) * 128)
                kb.dma("sp", sa[:, b, :], src[rows, CTX:CTX + HALF], reads=[r_src], writes=[r_sa[b]])
                kb.dma("sp", sbb[:, b, :], src[rows, CTX + HALF:LT], reads=[r_src], writes=[r_sb[b]])
                kb.op("dve", lambda e, b=b: e.tensor_scalar(out=so[:, b, :], in0=sa[:, b, :], scalar1=hm[:, 0:1], scalar2=None, op0=ALU.mult),
                      reads=[r_sa[b], r_hm], writes=[r_so[b]])
                kb.op("dve", lambda e, b=b: e.scalar_tensor_tensor(out=so[:, b, :], in0=sbb[:, b, :], scalar=hm[:, 1:2], in1=so[:, b, :],
                                                                  op0=ALU.mult, op1=ALU.add), reads=[r_sb[b], r_hm, r_so[b]], writes=[r_so[b]])
                kb.dma("pool", S[dst][rows, :], so[:, b, :], reads=[r_so[b]], writes=[g.RS[dst]])
        for i, (t0, T) in enumerate(LTILES):
            b = i % 2
            kb.dma("sp", xa[:, b], S["X2"][CTX + t0:CTX + t0 + T, :].rearrange("(s p) d -> p s d", p=128), reads=[g.RS["X2"]], writes=[r_sa[b]])
            kb.dma("sp", xb[:, b], S["X2"][CTX + HALF + t0:CTX + HALF + t0 + T, :].rearrange("(s p) d -> p s d", p=128), reads=[g.RS["X2"]], writes=[r_sb[b]])
            kb.op("dve", lambda e, b=b: e.tensor_scalar(out=xo[:, b], in0=xa[:, b], scalar1=hm[:, 0:1], scalar2=None, op0=ALU.mult),
                  reads=[r_sa[b], r_hm], writes=[r_so[b]])
            kb.op("dve", lambda e, b=b: e.scalar_tensor_tensor(out=xo[:, b], in0=xb[:, b], scalar=hm[:, 1:2], in1=xo[:, b],
                                                              op0=ALU.mult, op1=ALU.add), reads=[r_sb[b], r_hm, r_so[b]], writes=[r_so[b]])
            kb.dma("pool", S["Xl"][t0:t0 + T, :].rearrange("(s p) d -> p s d", p=128), xo[:, b], reads=[r_so[b]], writes=[g.RS["Xl"]])


def rms_feat(g, src, nk, t0, T, width, gcol, r_g, sq, r_sq, rb, r_rb, cnm, r_cn, r_src, bank):
    kb = g.kb
    for k in range(nk):
        kb.op("dve", lambda e, k=k: e.tensor_tensor(out=sq[:, k, :T], in0=src[:, k, t0:t0 + T], in1=src[:, k, t0:t0 + T], op=ALU.mult),
              reads=[r_src], writes=[r_sq])
    pb = g.psum[bank]
    for k in range(nk):
        kb.op("pe", lambda e, k=k: e.matmul(pb[:, :T], lhsT=g.ones[:], rhs=sq[:, k, :T], start=(k == 0), stop=(k == nk - 1)),
              reads=[g.r_ones, r_sq], writes=[g.rps[bank]], inc=(k == nk - 1))
    kb.op("act", lambda e: e.activation(out=rb[:, :T], in_=pb[:, :T], func=AF.Sqrt, bias=EPS, scale=1.0 / width), reads=[g.rps[bank]], writes=[r_rb])
    kb.op("dve", lambda e: e.reciprocal(out=rb[:, :T], in_=rb[:, :T]), reads=[r_rb], writes=[r_rb])
    for k in range(nk):
        kb.op("dve", lambda e, k=k: e.scalar_tensor_tensor(out=cnm[:, k, :T], in0=src[:, k, t0:t0 + T], scalar=gcol[:, k:k + 1], in1=rb[:, :T],
                                                          op0=ALU.mult, op1=ALU.mult), reads=[r_src, r_rb, r_g], writes=[r_cn])


def phase_kv(g, l):
    nc, kb, I, S = g.nc, g.kb, g.I, g.S
    with nc.sbuf_tensor(nm("CKV"), [128, 2, LT], BF16) as CKV, \
            nc.sbuf_tensor(nm("KRa"), [64, LT], BF16) as KRa, \
            nc.sbuf_tensor(nm("KRb"), [64, LT], BF16) as KRb, \
            nc.sbuf_tensor(nm("COS"), [64, SEQ], F32) as COS, \
            nc.sbuf_tensor(nm("SIN"), [64, SEQ], F32) as SIN, \
            nc.sbuf_tensor(nm("WUKV"), [128, 2, 2048], BF16) as WUKV, \
            nc.sbuf_tensor(nm("kvg"), [128, 2], F32) as kvg, \
            nc.sbuf_tensor(nm("sq"), [128, 2, 512], BF16) as sq, \
            nc.sbuf_tensor(nm("rb"), [128, 512], F32) as rb, \
            nc.sbuf_tensor(nm("cnm"), [128, 2, 512], BF16) as cnm, \
            nc.sbuf_tensor(nm("ko"), [128, 8, 512], BF16) as ko, \
            nc.sbuf_tensor(nm("vo"), [128, 2, D], BF16) as vo, \
            nc.sbuf_tensor(nm("r1"), [64, 512], F32) as r1, \
            nc.sbuf_tensor(nm("r2"), [64, 512], F32) as r2, \
            nc.sbuf_tensor(nm("kro"), [64, 2, 512], BF16) as kro:
        r_CKV, r_KR, r_tab, r_W, r_g, r_sq, r_rb, r_cn, r_r1, r_r2 = (kb.res("k") for _ in range(10))
        r_ko = [kb.res("ko") for _ in range(8)]
        r_vo = [kb.res("vo") for _ in range(2)]
        r_kro = [kb.res("kro") for _ in range(2)]
        kb.dma("sp", CKV[:], S["Z"][27 * 128:29 * 128, :].rearrange("(k p) t -> p k t", p=128), reads=[g.RS["Z"]], writes=[r_CKV])
        kb.dma("sp", KRa[:], S["Z"][29 * 128:29 * 128 + 64, :], reads=[g.RS["Z"]], writes=[r_KR])
        kb.dma("sp", KRb[:], S["Z"][29 * 128 + 64:30 * 128, :], reads=[g.RS["Z"]], writes=[r_KR])
        kb.dma("sp", COS[:], I["cosT"], reads=[g.r_in], writes=[r_tab])
        kb.dma("sp", SIN[:], I["sinT"], reads=[g.r_in], writes=[r_tab])
        kb.dma("pool", WUKV[:], I["w_ukv"][l].rearrange("(k p) n -> p k n", p=128), reads=[g.r_in], writes=[r_W])
        kb.dma("sp", kvg[:], I["kvg_col"][l], reads=[g.r_in], writes=[r_g])
        nko = nvo = 0
        for ti, (t0, T) in enumerate(TILES):
            rms_feat(g, CKV, 2, t0, T, 256.0, kvg, r_g, sq, r_sq, rb, r_rb, cnm, r_cn, r_CKV, 0)
            for h in range(8):
                bank = 1 + (h % 3)
                pb = g.psum[bank]
                for k in range(2):
                    kb.op("pe", lambda e, k=k, h=h, pb=pb: e.matmul(pb[:, :T], lhsT=WUKV[:, k, h * 128:(h + 1) * 128], rhs=cnm[:, k, :T],
                                                                    start=(k == 0), stop=(k == 1)), reads=[r_W, r_cn], writes=[g.rps[bank]], inc=(k == 1))
                ki = nko % 8
                nko += 1
                if h % 2 == 0:
                    kb.op("act", lambda e, pb=pb, ki=ki: e.activation(out=ko[:, ki, :T], in_=pb[:, :T], func=AF.Copy), reads=[g.rps[bank]], writes=[r_ko[ki]])
                else:
                    kb.op("dve", lambda e, pb=pb, ki=ki: e.tensor_copy(out=ko[:, ki, :T], in_=pb[:, :T]), reads=[g.rps[bank]], writes=[r_ko[ki]])
                kb.dma("pool", S["KN"][h, :, t0:t0 + T], ko[:, ki, :T], reads=[r_ko[ki]], writes=[g.RS["KN"]])
            for sub in range(T // 128):
                vi = nvo % 2
                nvo += 1
                for half in range(2):
                    bank = 4 + half
                    pb = g.psum[bank]
                    for k in range(2):
                        kb.op("pe", lambda e, k=k, half=half, pb=pb, sub=sub: e.matmul(
                            pb[:], lhsT=cnm[:, k, sub * 128:(sub + 1) * 128], rhs=WUKV[:, k, 1024 + half * 512:1536 + half * 512],
                            start=(k == 0), stop=(k == 1)), reads=[r_W, r_cn], writes=[g.rps[bank]], inc=(k == 1))
                    if half == 0:
                        kb.op("act", lambda e, pb=pb, vi=vi: e.activation(out=vo[:, vi, 0:512], in_=pb[:], func=AF.Copy), reads=[g.rps[bank]], writes=[r_vo[vi]])
                    else:
                        kb.op("dve", lambda e, pb=pb, vi=vi: e.tensor_copy(out=vo[:, vi, 512:1024], in_=pb[:]), reads=[g.rps[bank]], writes=[r_vo[vi]])
                kb.dma("pool", S["V"][t0 + sub * 128:t0 + (sub + 1) * 128, :], vo[:, vi, :], reads=[r_vo[vi]], writes=[g.RS["V"]])
            oi = ti % 2
            if ti == 0:
                kb.op("dve", lambda e, oi=oi: e.tensor_copy(out=kro[:, oi, :T], in_=KRa[:, t0:t0 + T]), reads=[r_KR], writes=[r_kro[oi]])
            else:
                q0 = t0 - CTX
                kb.op("dve", lambda e: e.tensor_tensor(out=r1[:, :T], in0=KRa[:, t0:t0 + T], in1=COS[:, q0:q0 + T], op=ALU.mult), reads=[r_KR, r_tab], writes=[r_r1])
                kb.op("dve", lambda e: e.tensor_tensor(out=r2[:, :T], in0=KRb[:, t0:t0 + T], in1=SIN[:, q0:q0 + T], op=ALU.mult), reads=[r_KR, r_tab], writes=[r_r2])
                kb.op("dve", lambda e, oi=oi: e.tensor_tensor(out=kro[:, oi, :T], in0=r1[:, :T], in1=r2[:, :T], op=ALU.add), reads=[r_r1, r_r2], writes=[r_kro[oi]])
            kb.dma("pool", S["KRD"][:, t0:t0 + T], kro[:, oi, :T], reads=[r_kro[oi]], writes=[g.RS["KRD"]])


def phase_q(g, l, loc=False):
    nc, kb, I, S = g.nc, g.kb, g.I, g.S
    NT = HALF if loc else LT
    NR = HALF if loc else SEQ
    tiles = [(t0, T, False) for (t0, T) in LTILES] if loc else [(t0, T, t0 == 0) for (t0, T) in TILES]
    qoff = 0 if loc else CTX
    cq_src, r_cq = (S["CQl"], g.RS["CQl"]) if loc else (S["Z"][24 * 128:27 * 128, :], g.RS["Z"])
    cos_src, sin_src = (I["cosQ"], I["sinQ"]) if loc else (I["cosT"], I["sinT"])
    QN_dst, QR_dst = ("QNl", "QRl") if loc else ("QN", "QR")
    with nc.sbuf_tensor(nm("CQ"), [128, 3, NT], BF16) as CQ, \
            nc.sbuf_tensor(nm("COS"), [64, NR], F32) as COS, \
            nc.sbuf_tensor(nm("SIN"), [64, NR], F32) as SIN, \
            nc.sbuf_tensor(nm("WUQ"), [128, 3, 2048], BF16) as WUQ, \
            nc.sbuf_tensor(nm("qg"), [128, 3], F32) as qg, \
            nc.sbuf_tensor(nm("sq"), [128, 3, 512], BF16) as sq, \
            nc.sbuf_tensor(nm("rb"), [128, 512], F32) as rb, \
            nc.sbuf_tensor(nm("cnm"), [128, 3, 512], BF16) as cnm, \
            nc.sbuf_tensor(nm("qo"), [128, 8, 512], BF16) as qo, \
            nc.sbuf_tensor(nm("r1"), [64, 512], F32) as r1, \
            nc.sbuf_tensor(nm("r2"), [64, 512], F32) as r2, \
            nc.sbuf_tensor(nm("qro"), [64, 8, 512], BF16) as qro:
        r_CQ, r_tab, r_W, r_g, r_sq, r_rb, r_cn, r_r1, r_r2 = (kb.res("q") for _ in range(9))
        r_qo = [kb.res("qo") for _ in range(8)]
        r_qro = [kb.res("qro") for _ in range(8)]
        kb.dma("sp", CQ[:], cq_src.rearrange("(k p) t -> p k t", p=128), reads=[r_cq], writes=[r_CQ])
        kb.dma("sp", COS[:], cos_src, reads=[g.r_in], writes=[r_tab])
        kb.dma("sp", SIN[:], sin_src, reads=[g.r_in], writes=[r_tab])
        kb.dma("pool", WUQ[:], I["w_uq"][l].rearrange("(k p) n -> p k n", p=128), reads=[g.r_in], writes=[r_W])
        kb.dma("sp", qg[:], I["qg_col"][l], reads=[g.r_in], writes=[r_g])
        nq = 0
        for ti, (t0, T, isctx) in enumerate(tiles):
            rms_feat(g, CQ, 3, t0, T, 384.0, qg, r_g, sq, r_sq, rb, r_rb, cnm, r_cn, r_CQ, 0)
            for h in range(8):
                qi = nq % 8
                nq += 1
                bn, br, bp = 1 + 3 * (h % 2), 2 + 3 * (h % 2), 3 + 3 * (h % 2)
                for k in range(3):
                    kb.op("pe", lambda e, k=k, h=h: e.matmul(g.psum[bn][:, :T], lhsT=WUQ[:, k, h * 256:h * 256 + 128], rhs=cnm[:, k, :T],
                                                             start=(k == 0), stop=(k == 2)), reads=[r_W, r_cn], writes=[g.rps[bn]], inc=(k == 2))
                for k in range(3):
                    kb.op("pe", lambda e, k=k, h=h: e.matmul(g.psum[br][0:64, :T], lhsT=WUQ[:, k, h * 256 + 128:h * 256 + 192], rhs=cnm[:, k, :T],
                                                             start=(k == 0), stop=(k == 2)), reads=[r_W, r_cn], writes=[g.rps[br]], inc=(k == 2))
                kb.op("act", lambda e, qi=qi: e.activation(out=qo[:, qi, :T], in_=g.psum[bn][:, :T], func=AF.Copy, scale=MLA_SCALE),
                      reads=[g.rps[bn]], writes=[r_qo[qi]])
                kb.dma("pool", S[QN_dst][h, :, t0:t0 + T], qo[:, qi, :T], reads=[r_qo[qi]], writes=[g.RS[QN_dst]])
                if isctx:
                    kb.op("act", lambda e, qi=qi: e.activation(out=qro[:, qi, :T], in_=g.psum[br][0:64, :T], func=AF.Copy, scale=MLA_SCALE),
                          reads=[g.rps[br]], writes=[r_qro[qi]])
                else:
                    q0 = t0 - qoff
                    for k in range(3):
                        kb.op("pe", lambda e, k=k, h=h: e.matmul(g.psum[bp][0:64, :T], lhsT=WUQ[:, k, h * 256 + 192:h * 256 + 256], rhs=cnm[:, k, :T],
                                                                 start=(k == 0), stop=(k == 2)), reads=[r_W, r_cn], writes=[g.rps[bp]], inc=(k == 2))
                    kb.op("dve", lambda e: e.tensor_tensor(out=r1[:, :T], in0=g.psum[br][0:64, :T], in1=COS[:, q0:q0 + T], op=ALU.mult),
                          reads=[g.rps[br], r_tab], writes=[r_r1])
                    kb.op("dve", lambda e: e.scalar_tensor_tensor(out=r2[:, :T], in0=g.psum[bp][0:64, :T], scalar=MLA_SCALE, in1=SIN[:, q0:q0 + T],
                                                                 op0=ALU.mult, op1=ALU.mult), reads=[g.rps[bp], r_tab], writes=[r_r2])
                    kb.op("dve", lambda e, qi=qi: e.scalar_tensor_tensor(out=qro[:, qi, :T], in0=r1[:, :T], scalar=MLA_SCALE, in1=r2[:, :T],
                                                                        op0=ALU.mult, op1=ALU.add), reads=[r_r1, r_r2], writes=[r_qro[qi]])
                kb.dma("pool", S[QR_dst][h, :, t0:t0 + T], qro[:, qi, :T], reads=[r_qro[qi]], writes=[g.RS[QR_dst]])


def phase_att(g, l, loc=False, att_heads=8):
    nc, kb, I, S = g.nc, g.kb, g.I, g.S
    NKT = LT // 128
    tiles = [(t0, T, False) for (t0, T) in LTILES] if loc else [(t0, T, t0 == 0) for (t0, T) in TILES]
    QN_src, QR_src, AT_dst = ("QNl", "QRl", "ATl") if loc else ("QN", "QR", "AT")
    with nc.sbuf_tensor(nm("KNh"), [128, LT], BF16) as KNh, \
            nc.sbuf_tensor(nm("KRD"), [128, LT], BF16) as KRD, \
            nc.sbuf_tensor(nm("Vh"), [128, NKT, 128], BF16) as Vh, \
            nc.sbuf_tensor(nm("QNb"), [128, 2, 512], BF16) as QNb, \
            nc.sbuf_tensor(nm("QRb"), [128, 2, 512], BF16) as QRb, \
            nc.sbuf_tensor(nm("PT"), [128, 8, 512], BF16) as PT, \
            nc.sbuf_tensor(nm("rl"), [128, 512], F32) as rl, \
            nc.sbuf_tensor(nm("ahl"), [128, 2, 512], BF16) as ahl, \
            nc.sbuf_tensor(nm("atmp"), [128, 512], F32) as atmp, \
            nc.sbuf_tensor(nm("ob"), [128, 2, 512], BF16) as ob:
        r_KN, r_KR, r_V, r_rl, r_ahl, r_atmp = (kb.res("a") for _ in range(6))
        r_Q = [kb.res("Q") for _ in range(2)]
        r_PT = [kb.res("PT") for _ in range(8)]
        r_ob = [kb.res("ob") for _ in range(2)]
        kb.dma("sp", KRD[0:64, :], S["KRD"], reads=[g.RS["KRD"]], writes=[r_KR])
        kb.dma("sp", KRD[64:128, :], S["KRD"], reads=[g.RS["KRD"]], writes=[r_KR])
        bLp, bLr = 6, 7
        nq = 0
        for h in range(att_heads):
            kb.dma("sp", KNh[:], S["KN"][h], reads=[g.RS["KN"]], writes=[r_KN])
            kb.dma("sp", Vh[:], S["V"][:, h * 128:(h + 1) * 128].rearrange("(kt p) d -> p kt d", p=128), reads=[g.RS["V"]], writes=[r_V])
            for ti, (t0, T, isctx) in enumerate(tiles):
                nk = 2 if isctx else NKT
                npair = nk // 2
                ngrp = (nk + 3) // 4
                qi = nq % 2
                nq += 1
                bO = 4 + qi
                kb.dma("sp", QNb[:, qi, :T], S[QN_src][h, :, t0:t0 + T], reads=[g.RS[QN_src]], writes=[r_Q[qi]])
                kb.dma("sp", QRb[0:64, qi, :T], S[QR_src][h, :, t0:t0 + T], reads=[g.RS[QR_src]], writes=[r_Q[qi]])
                kb.dma("sp", QRb[64:128, qi, :T], S[QR_src][h, :, t0:t0 + T], reads=[g.RS[QR_src]], writes=[r_Q[qi]])

                def emit_S_pair(p):
                    k0, k1 = 2 * p, 2 * p + 1
                    b0, b1 = k0 % 4, k1 % 4
                    kb.op("pe", lambda e: e.matmul(g.psum[b0][:, :T], lhsT=KNh[:, k0 * 128:(k0 + 1) * 128], rhs=QNb[:, qi, :T], start=True, stop=False),
                          reads=[r_KN, r_Q[qi]], writes=[g.rps[b0]], inc=False)
                    kb.op("pe", lambda e: e.matmul(g.psum[b1][:, :T], lhsT=KNh[:, k1 * 128:(k1 + 1) * 128], rhs=QNb[:, qi, :T], start=True, stop=False),
                          reads=[r_KN, r_Q[qi]], writes=[g.rps[b1]], inc=False)
                    kb.op("pe", lambda e: e.matmul(g.psum[b0][:, :T], lhsT=KRD[0:64, k0 * 128:(k0 + 1) * 128], rhs=QRb[0:64, qi, :T], start=False, stop=True,
                                                   tile_position=(0, 0)), reads=[r_KR, r_Q[qi]], writes=[g.rps[b0]], inc=False)
                    kb.op("pe", lambda e: e.matmul(g.psum[b1][:, :T], lhsT=KRD[64:128, k1 * 128:(k1 + 1) * 128], rhs=QRb[64:128, qi, :T], start=False, stop=True,
                                                   tile_position=(64, 0)), reads=[r_KR, r_Q[qi]], writes=[g.rps[b1], g.rps[b0]])

                emit_S_pair(0)
                for p in range(npair):
                    for kt in (2 * p, 2 * p + 1):
                        kb.op("act", lambda e, kt=kt: e.activation(out=PT[:, kt % 8, :T], in_=g.psum[kt % 4][:, :T], func=AF.Exp),
                              reads=[g.rps[kt % 4]], writes=[r_PT[kt % 8]])
                    if p + 1 < npair:
                        emit_S_pair(p + 1)
                    for kt in (2 * p, 2 * p + 1):
                        kb.op("pe", lambda e, kt=kt: e.matmul(g.psum[bO][:, :T], lhsT=Vh[:, kt, :], rhs=PT[:, kt % 8, :T], start=(kt == 0), stop=(kt == nk - 1)),
                              reads=[r_V, r_PT[kt % 8]], writes=[g.rps[bO]], inc=(kt % 2 == 1))
                    if p % 2 == 1 or p == npair - 1:
                        gi = p // 2
                        kts = [kt for kt in range(4 * gi, 4 * gi + 4) if kt < nk]
                        for kt in kts:
                            j = kt % 4
                            lastg = max(gg for gg in range(ngrp) if 4 * gg + j < nk)
                            kb.op("pe", lambda e, kt=kt, j=j, lastg=lastg: e.matmul(
                                g.psum[bLp][32 * j:32 * j + 32, :T], lhsT=g.ones[:, 0:32], rhs=PT[:, kt % 8, :T], start=(gi == 0), stop=(gi == lastg),
                                tile_position=(0, 32 * j)), reads=[g.r_ones, r_PT[kt % 8]], writes=[g.rps[bLp]], inc=(kt == kts[-1]))
                KR_ = 128 if nk >= 4 else 32 * nk
                kb.op("dve", lambda e: e.tensor_copy(out=ahl[:KR_, 0, :T], in_=g.psum[bLp][:KR_, :T]), reads=[g.rps[bLp]], writes=[r_ahl])
                kb.op("dve", lambda e: e.tensor_tensor(out=atmp[:KR_, :T], in0=g.psum[bLp][:KR_, :T], in1=ahl[:KR_, 0, :T], op=ALU.subtract),
                      reads=[g.rps[bLp], r_ahl], writes=[r_atmp])
                kb.op("dve", lambda e: e.tensor_copy(out=ahl[:KR_, 1, :T], in_=atmp[:KR_, :T]), reads=[r_atmp], writes=[r_ahl])
                kb.op("pe", lambda e: e.matmul(g.psum[bLr][:, :T], lhsT=g.ones[:KR_, :], rhs=ahl[:KR_, 0, :T], start=True, stop=False),
                      reads=[g.r_ones, r_ahl], writes=[g.rps[bLr]], inc=False)
                kb.op("pe", lambda e: e.matmul(g.psum[bLr][:, :T], lhsT=g.ones[:KR_, :], rhs=ahl[:KR_, 1, :T], start=False, stop=True),
                      reads=[g.r_ones, r_ahl], writes=[g.rps[bLr]])
                kb.op("dve", lambda e: e.reciprocal(out=rl[:, :T], in_=g.psum[bLr][:, :T]), reads=[g.rps[bLr]], writes=[r_rl])
                kb.op("dve", lambda e: e.scalar_tensor_tensor(out=ob[:, qi, :T], in0=g.psum[bO][:, :T], scalar=32.0, in1=rl[:, :T], op0=ALU.mult, op1=ALU.mult),
                      reads=[g.rps[bO], r_rl], writes=[r_ob[qi]])
                kb.dma("pool", S[AT_dst][h * 128:(h + 1) * 128, t0:t0 + T], ob[:, qi, :T], reads=[r_ob[qi]], writes=[g.RS[AT_dst]])


def resid_epilogue(g, srcs, r_srcs, xsub, r_x, gr_idx, y1, r_y1, ss2, r_ss, junk, dst_ap, dst_res):
    kb = g.kb
    for cb in range(2):
        kb.op("act", lambda e, cb=cb: e.activation(out=junk[:, 0:512], in_=srcs[cb], func=AF.Square, accum_out=ss2[:, cb:cb + 1]),
              reads=[r_srcs[cb]], writes=[r_ss])
    kb.op("dve", lambda e: e.tensor_tensor(out=ss2[:, 2:3], in0=ss2[:, 0:1], in1=ss2[:, 1:2], op=ALU.add), reads=[r_ss], writes=[r_ss])
    kb.op("act", lambda e: e.activation(out=ss2[:, 3:4], in_=ss2[:, 2:3], func=AF.Sqrt, bias=EPS, scale=1.0 / D), reads=[r_ss], writes=[r_ss])
    kb.op("dve", lambda e: e.reciprocal(out=ss2[:, 3:4], in_=ss2[:, 3:4]), reads=[r_ss], writes=[r_ss])
    for cb in range(2):
        kb.op("dve", lambda e, cb=cb: e.scalar_tensor_tensor(out=y1[:, cb * 512:(cb + 1) * 512], in0=srcs[cb], scalar=ss2[:, 3:4],
                                                            in1=g.GR[:, gr_idx, cb * 512:(cb + 1) * 512], op0=ALU.mult, op1=ALU.mult),
              reads=[r_srcs[cb], r_ss, g.r_mod], writes=[r_y1])
    kb.op("dve", lambda e: e.tensor_tensor(out=y1[:], in0=y1[:], in1=xsub, op=ALU.add), reads=[r_y1, r_x], writes=[r_y1])
    kb.dma("pool", dst_ap, y1[:], reads=[r_y1], writes=[dst_res])


def phase_D1(g, l, Xsrc, r_X, loc=False):
    nc, kb, I, S = g.nc, g.kb, g.I, g.S
    tiles = [(t0, T, False) for (t0, T) in LTILES] if loc else [(t0, T, t0 == 0) for (t0, T) in TILES]
    srcs = ("MPl", "YLl", "ATl") if loc else ("MP", "YL", "AT")
    g_src, r_gsrc = (S["Gl"], g.RS["Gl"]) if loc else (S["Z"][30 * 128:54 * 128, :], g.RS["Z"])
    if loc:
        Xsrc, r_X = S["Xl"], g.RS["Xl"]
    X1_dst = "X1l" if loc else "X1"
    with nc.sbuf_tensor(nm("WP"), [128, 4, 8, D], BF16) as WP, \
            nc.sbuf_tensor(nm("act3"), [128, 2, 3, 8, 512], BF16) as act3, \
            nc.sbuf_tensor(nm("gts"), [128, 24, 512], BF16) as gts, \
            nc.sbuf_tensor(nm("xt"), [128, 4, D], F32) as xt, \
            nc.sbuf_tensor(nm("mgT"), [128, 8, 512], BF16) as mgT, \
            nc.sbuf_tensor(nm("ta"), [128, 512], F32) as ta, \
            nc.sbuf_tensor(nm("tb"), [128, 512], F32) as tb, \
            nc.sbuf_tensor(nm("y1"), [128, 2, D], F32) as y1, \
            nc.sbuf_tensor(nm("ss2"), [128, 2, 4], F32) as ss2, \
            nc.sbuf_tensor(nm("stg"), [128, 2, 1024], F32) as stg, \
            nc.sbuf_tensor(nm("junk"), [128, 512], BF16) as junk:
        r_stg = [kb.res("stg") for _ in range(2)]
        cnt = [0]
        r_WP = [kb.res("WP") for _ in range(4)]
        r_act = [kb.res("act3") for _ in range(2)]
        r_gts, r_xt, r_mg, r_ta, r_tb = (kb.res("d") for _ in range(5))
        r_y1 = [kb.res("y1") for _ in range(2)]
        r_ss = [kb.res("ss") for _ in range(2)]
        for wi, n in enumerate(("pool_proj", "lru_proj", "mla_proj", "w_out")):
            wsrc = I[n][l].rearrange("(k p) n -> p k n", p=128)
            for k in range(8):
                load_w(g, WP[:, wi, k, :], wsrc[:, k, :], r_WP[wi], stg, r_stg, cnt)
        ny = 0
        for ti, (t0, T, isctx) in enumerate(tiles):
            sel = 1 if isctx else 0
            nsub = T // 128
            ab = ti % 2
            for si, n in enumerate(srcs):
                kb.dma("sp", act3[:, ab, si, :, :T], S[n][:, t0:t0 + T].rearrange("(k p) t -> p k t", p=128), reads=[g.RS[n]], writes=[r_act[ab]])
            kb.dma("sp", gts[:, :, :T], g_src[:, t0:t0 + T].rearrange("(j p) t -> p j t", p=128), reads=[r_gsrc], writes=[r_gts])
            kb.dma("sp", xt[:, :nsub, :], Xsrc[t0:t0 + T, :].rearrange("(s p) d -> p s d", p=128), reads=[r_X], writes=[r_xt])
            for oc in range(8):
                banks = [(oc % 2) * 3 + i for i in range(3)]
                for si in range(3):
                    for k in range(8):
                        kb.op("pe", lambda e, si=si, k=k: e.matmul(g.psum[banks[si]][:, :T], lhsT=WP[:, si, k, oc * 128:(oc + 1) * 128],
                                                                  rhs=act3[:, ab, si, k, :T], start=(k == 0), stop=(k == 7)),
                              reads=[r_WP[si], r_act[ab]], writes=[g.rps[banks[si]]], inc=(k == 7))
                kb.op("dve", lambda e: e.tensor_tensor(out=ta[:, :T], in0=g.psum[banks[0]][:, :T], in1=gts[:, oc, :T], op=ALU.mult),
                      reads=[g.rps[banks[0]], r_gts], writes=[r_ta])
                kb.op("dve", lambda e: e.tensor_tensor(out=tb[:, :T], in0=g.psum[banks[1]][:, :T], in1=gts[:, 8 + oc, :T], op=ALU.mult),
                      reads=[g.rps[banks[1]], r_gts], writes=[r_tb])
                kb.op("dve", lambda e: e.tensor_tensor(out=ta[:, :T], in0=ta[:, :T], in1=tb[:, :T], op=ALU.add), reads=[r_ta, r_tb], writes=[r_ta])
                kb.op("dve", lambda e: e.tensor_tensor(out=tb[:, :T], in0=g.psum[banks[2]][:, :T], in1=gts[:, 16 + oc, :T], op=ALU.mult),
                      reads=[g.rps[banks[2]], r_gts], writes=[r_tb])
                kb.op("dve", lambda e: e.tensor_tensor(out=mgT[:, oc, :T], in0=ta[:, :T], in1=tb[:, :T], op=ALU.add), reads=[r_ta, r_tb], writes=[r_mg])
            for sub in range(nsub):
                pair = ((6, 7), (0, 1), (2, 3), (4, 5))[sub % 4]
                for cb in range(2):
                    bank = pair[cb]
                    for k in range(8):
                        kb.op("pe", lambda e, k=k, cb=cb, bank=bank: e.matmul(g.psum[bank][:], lhsT=mgT[:, k, sub * 128:(sub + 1) * 128],
                                                                             rhs=WP[:, 3, k, cb * 512:(cb + 1) * 512], start=(k == 0), stop=(k == 7)),
                              reads=[r_WP[3], r_mg], writes=[g.rps[bank]], inc=(k == 7))
                yi = ny % 2
                ny += 1
                resid_epilogue(g, [g.psum[pair[0]][:], g.psum[pair[1]][:]], [g.rps[pair[0]], g.rps[pair[1]]], xt[:, sub, :], r_xt, 0 + sel, y1[:, yi], r_y1[yi],
                               ss2[:, yi], r_ss[yi], junk, S[X1_dst][t0 + sub * 128:t0 + (sub + 1) * 128, :], g.RS[X1_dst])


def phase_ffn_up(g, w1, w3, nf, Xsrc, r_X, HT, tiles, l):
    nc, kb, I, S = g.nc, g.kb, g.I, g.S
    dense = HT is None
    with nc.sbuf_tensor(nm("W1"), [128, 8, nf * 128], BF16) as W1, \
            nc.sbuf_tensor(nm("W3"), [128, 8, nf * 128], BF16) as W3, \
            nc.sbuf_tensor(nm("stg"), [128, 2, nf * 128], F32) as stg, \
            nc.sbuf_tensor(nm("XT"), [128, 4, D if dense else 8], F32) as XT, \
            nc.sbuf_tensor(nm("xn"), [128, 4, D if dense else 8], BF16) as xn, \
            nc.sbuf_tensor(nm("junk"), [128, D if dense else 8], BF16) as junk, \
            nc.sbuf_tensor(nm("hT"), [128, 2, 8, 512], BF16) as hT, \
            nc.sbuf_tensor(nm("ss"), [128, 4], F32) as ss, \
            nc.sbuf_tensor(nm("rstd"), [128, 4], F32) as rstd, \
            nc.sbuf_tensor(nm("sg"), [128, 2, 512], F32) as sg, \
            nc.sbuf_tensor(nm("ao"), [128, 8, 512], BF16) as ao:
        r_W1 = [kb.res("W1") for _ in range(8)]
        r_W3 = [kb.res("W3") for _ in range(8)]
        r_XT, r_xn, r_ss = (kb.res("f") for _ in range(3))
        r_hT = [kb.res("hT") for _ in range(2)]
        r_sg = [kb.res("sg") for _ in range(2)]
        r_ao = [kb.res("ao") for _ in range(8)]
        w1v = w1.rearrange("(k p) n -> p k n", p=128)
        w3v = w3.rearrange("(k p) n -> p k n", p=128)
        r_stg = [kb.res("stg") for _ in range(2)]
        cnt = [0]
        for k in range(8):
            load_w(g, W1[:, k, :], w1v[:, k, :], r_W1[k], stg, r_stg, cnt)
        for k in range(8):
            load_w(g, W3[:, k, :], w3v[:, k, :], r_W3[k], stg, r_stg, cnt)
        na = 0
        for ti, (t0, T) in enumerate(tiles):
            sel = 1 if t0 == 0 else 0
            nsub = T // 128
            b = ti % 2
            if HT is None:
                kb.dma("sp", XT[:, :nsub, :], Xsrc[t0:t0 + T, :].rearrange("(s p) d -> p s d", p=128), reads=[r_X], writes=[r_XT])
                norm_transpose(g, XT, r_XT, nsub, T, g.G2, g.mv[:, 24:32, :], sel, hT[:, b], r_hT[b], xn, r_xn, ss, rstd, r_ss, junk, [0, 1, 2, 3])
            else:
                kb.dma("sp", hT[:, b, :, :T], HT[:, t0:t0 + T].rearrange("(k p) t -> p k t", p=128), reads=[g.RS["H2T"]], writes=[r_hT[b]])
            for f in range(nf):
                b1, b3 = 4 + (f % 2) * 2, 5 + (f % 2) * 2
                for k in range(8):
                    kb.op("pe", lambda e, k=k: e.matmul(g.psum[b1][:, :T], lhsT=W1[:, k, f * 128:(f + 1) * 128], rhs=hT[:, b, k, :T],
                                                        start=(k == 0), stop=(k == 7)), reads=[r_W1[k], r_hT[b]], writes=[g.rps[b1]], inc=(k == 7))
                for k in range(8):
                    kb.op("pe", lambda e, k=k: e.matmul(g.psum[b3][:, :T], lhsT=W3[:, k, f * 128:(f + 1) * 128], rhs=hT[:, b, k, :T],
                                                        start=(k == 0), stop=(k == 7)), reads=[r_W3[k], r_hT[b]], writes=[g.rps[b3]], inc=(k == 7))
                si = f % 2
                ai = na % 8
                na += 1
                kb.op("act", lambda e: e.activation(out=sg[:, si, :T], in_=g.psum[b1][:, :T], func=AF.Silu), reads=[g.rps[b1]], writes=[r_sg[si]])
                kb.op("dve", lambda e: e.tensor_tensor(out=ao[:, ai, :T], in0=g.psum[b3][:, :T], in1=sg[:, si, :T], op=ALU.mult),
                      reads=[g.rps[b3], r_sg[si]], writes=[r_ao[ai]])
                kb.dma("pool", S["FA"][f * 128:(f + 1) * 128, t0:t0 + T], ao[:, ai, :T], reads=[r_ao[ai]], writes=[g.RS["FA"]])


def phase_ffn_down(g, w2, nf, tiles, l, first, last, e, dst, dst_res, dst_off, x1, r_x1):
    nc, kb, I, S = g.nc, g.kb, g.I, g.S
    moe = not (first and last)
    ya_in, ya_out = ("YA0", "YA1") if e % 2 == 1 else ("YA1", "YA0")
    with nc.sbuf_tensor(nm("W2"), [128, nf, D], BF16) as W2, \
            nc.sbuf_tensor(nm("aT"), [128, 2, nf, 512], BF16) as aT, \
            nc.sbuf_tensor(nm("xt"), [128, 4, D], F32) as xt, \
            nc.sbuf_tensor(nm("ya"), [128, 4, D], F32) as ya, \
            nc.sbuf_tensor(nm("gt"), [128, 4, NEXP], F32) as gt, \
            nc.sbuf_tensor(nm("y1"), [128, 2, D], F32) as y1, \
            nc.sbuf_tensor(nm("ss2"), [128, 2, 4], F32) as ss2, \
            nc.sbuf_tensor(nm("stg"), [128, 2, 1024], F32) as stg, \
            nc.sbuf_tensor(nm("junk"), [128, 512], BF16) as junk:
        r_stg = [kb.res("stg") for _ in range(2)]
        cnt = [0]
        r_W2, r_xt, r_ya, r_gt = (kb.res("w") for _ in range(4))
        r_aT = [kb.res("aT") for _ in range(2)]
        r_y1 = [kb.res("y1") for _ in range(2)]
        r_ss = [kb.res("ss") for _ in range(2)]
        w2v = w2.rearrange("(f p) n -> p f n", p=128)
        for f in range(nf):
            load_w(g, W2[:, f, :], w2v[:, f, :], r_W2, stg, r_stg, cnt)
        ny = 0
        for ti, (t0, T) in enumerate(tiles):
            sel = 1 if t0 == 0 else 0
            nsub = T // 128
            ab = ti % 2
            kb.dma("sp", aT[:, ab, :, :T], S["FA"][0:nf * 128, t0:t0 + T].rearrange("(f p) t -> p f t", p=128), reads=[g.RS["FA"]], writes=[r_aT[ab]])
            if last:
                kb.dma("sp", xt[:, :nsub, :], x1[t0:t0 + T, :].rearrange("(s p) d -> p s d", p=128), reads=[r_x1], writes=[r_xt])
            if moe:
                kb.dma("sp", gt[:, :nsub, :], S["GT"][t0:t0 + T, :].rearrange("(s p) e -> p s e", p=128), reads=[g.RS["GT"]], writes=[r_gt])
                if not first:
                    kb.dma("sp", ya[:, :nsub, :], S[ya_in][t0:t0 + T, :].rearrange("(s p) d -> p s d", p=128), reads=[g.RS[ya_in]], writes=[r_ya])
            for sub in range(nsub):
                bb = 4 * (sub % 2)
                for cb in range(2):
                    bank = bb + cb
                    for f in range(nf):
                        kb.op("pe", lambda e_, f=f, cb=cb, bank=bank: e_.matmul(g.psum[bank][:], lhsT=aT[:, ab, f, sub * 128:(sub + 1) * 128],
                                                                               rhs=W2[:, f, cb * 512:(cb + 1) * 512], start=(f == 0), stop=(f == nf - 1)),
                              reads=[r_W2, r_aT[ab]], writes=[g.rps[bank]], inc=(f == nf - 1))
                yi = ny % 2
                ny += 1
                dst_ap = dst[t0 - dst_off + sub * 128:t0 - dst_off + (sub + 1) * 128, :]
                if not moe:
                    resid_epilogue(g, [g.psum[bb][:], g.psum[bb + 1][:]], [g.rps[bb], g.rps[bb + 1]], xt[:, sub, :], r_xt, 2 + sel, y1[:, yi], r_y1[yi],
                                   ss2[:, yi], r_ss[yi], junk, dst_ap, dst_res)
                else:
                    for cb in range(2):
                        yv = ya[:, sub, cb * 512:(cb + 1) * 512]
                        if first:
                            kb.op("dve", lambda e_, cb=cb, yv=yv: e_.tensor_scalar(out=yv, in0=g.psum[bb + cb][:], scalar1=gt[:, sub, e:e + 1], scalar2=None,
                                                                                  op0=ALU.mult), reads=[g.rps[bb + cb], r_gt], writes=[r_ya])
                        else:
                            kb.op("dve", lambda e_, cb=cb, yv=yv: e_.scalar_tensor_tensor(out=yv, in0=g.psum[bb + cb][:], scalar=gt[:, sub, e:e + 1], in1=yv,
                                                                                         op0=ALU.mult, op1=ALU.add), reads=[g.rps[bb + cb], r_gt, r_ya], writes=[r_ya])
                    if last:
                        resid_epilogue(g, [ya[:, sub, 0:512], ya[:, sub, 512:1024]], [r_ya, r_ya], xt[:, sub, :], r_xt, 2, y1[:, yi], r_y1[yi],
                                       ss2[:, yi], r_ss[yi], junk, dst_ap, dst_res)
            if moe and not last:
                kb.dma("pool", S[ya_out][t0:t0 + T, :].rearrange("(s p) d -> p s d", p=128), ya[:, :nsub, :], reads=[r_ya], writes=[g.RS[ya_out]])


def phase_router(g, l, x1, r_x1):
    nc, kb, I, S = g.nc, g.kb, g.I, g.S
    with nc.sbuf_tensor(nm("RW"), [128, NEXP, D], F32) as RW, \
            nc.sbuf_tensor(nm("XT"), [128, 2, 4, D], F32) as XT, \
            nc.sbuf_tensor(nm("xn"), [128, 4, D], BF16) as xn, \
            nc.sbuf_tensor(nm("h2"), [128, D], F32) as h2, \
            nc.sbuf_tensor(nm("junkf"), [128, D], F32) as junkf, \
            nc.sbuf_tensor(nm("junk"), [128, D], BF16) as junk, \
            nc.sbuf_tensor(nm("hT"), [128, 2, 8, 512], BF16) as hT, \
            nc.sbuf_tensor(nm("ss"), [128, 2, 4], F32) as ss, \
            nc.sbuf_tensor(nm("rstd"), [128, 2, 4], F32) as rstd, \
            nc.sbuf_tensor(nm("lg"), [128, 4, NEXP], F32) as lg, \
            nc.sbuf_tensor(nm("mx"), [128, 4, 8], F32) as mx, \
            nc.sbuf_tensor(nm("sm"), [128, 4, 4], F32) as sm, \
            nc.sbuf_tensor(nm("ge"), [128, 4, NEXP], F32) as ge, \
            nc.sbuf_tensor(nm("mk"), [128, 4, NEXP], F32) as mk, \
            nc.sbuf_tensor(nm("go"), [128, 2, 4, NEXP], F32) as go:
        r_RW, r_xn, r_h2, r_lg, r_mx, r_sm, r_ge, r_mk = (kb.res("r") for _ in range(8))
        r_XT = [kb.res("XT") for _ in range(2)]
        r_hT = [kb.res("hT") for _ in range(2)]
        r_ss = [kb.res("ss") for _ in range(2)]
        r_go = [kb.res("go") for _ in range(2)]
        for e in range(NEXP):
            kb.dma("sp", RW[:, e, :], I["router_wT"][0, e].partition_broadcast(128), reads=[g.r_in], writes=[r_RW])
        for ti, (t0, T) in enumerate(LTILES):
            nsub = T // 128
            b = ti % 2
            kb.dma("sp", XT[:, b, :nsub, :], x1[t0:t0 + T, :].rearrange("(s p) d -> p s d", p=128), reads=[r_x1], writes=[r_XT[b]])
            norm_transpose(g, XT[:, b], r_XT[b], nsub, T, g.G2, g.mv[:, 24:32, :], 0, hT[:, b], r_hT[b], xn, r_xn,
                           ss[:, b], rstd[:, b], r_ss[b], junk, [0, 1, 2, 3])
            kb.dma("pool", S["H2T"][:, t0:t0 + T].rearrange("(k p) t -> p k t", p=128), hT[:, b, :, :T], reads=[r_hT[b]], writes=[g.RS["H2T"]])
            for s in range(nsub):
                kb.op("dve", lambda e_, s=s: e_.scalar_tensor_tensor(out=h2[:], in0=XT[:, b, s, :], scalar=rstd[:, b, s:s + 1], in1=g.MR[:, 0, :],
                                                                    op0=ALU.mult, op1=ALU.mult), reads=[r_XT[b], r_ss[b], g.r_mod], writes=[r_h2])
                kb.op("dve", lambda e_: e_.tensor_tensor(out=h2[:], in0=h2[:], in1=g.MR[:, 1, :], op=ALU.add), reads=[r_h2, g.r_mod], writes=[r_h2])
                for e in range(NEXP):
                    kb.op("dve", lambda e_, e=e, s=s: e_.scalar_tensor_tensor(out=junkf[:], in0=h2[:], scalar=1.0, in1=RW[:, e, :], op0=ALU.mult, op1=ALU.mult,
                                                                             accum_out=lg[:, s, e:e + 1]), reads=[r_h2, r_RW], writes=[r_lg])
                kb.op("dve", lambda e_, s=s: e_.max(out=mx[:, s, :], in_=lg[:, s, :]), reads=[r_lg], writes=[r_mx])
                kb.op("dve", lambda e_, s=s: e_.tensor_scalar(out=sm[:, s, 0:1], in0=mx[:, s, 0:1], scalar1=-1.0, scalar2=None, op0=ALU.mult),
                      reads=[r_mx], writes=[r_sm])
                kb.op("act", lambda e_, s=s: e_.activation(out=ge[:, s, :], in_=lg[:, s, :], func=AF.Exp, bias=sm[:, s, 0:1]), reads=[r_lg, r_sm], writes=[r_ge])
                kb.op("dve", lambda e_, s=s: e_.tensor_scalar(out=mk[:, s, :], in0=lg[:, s, :], scalar1=mx[:, s, 1:2], scalar2=None, op0=ALU.is_ge),
                      reads=[r_lg, r_mx], writes=[r_mk])
                kb.op("dve", lambda e_, s=s: e_.scalar_tensor_tensor(out=ge[:, s, :], in0=ge[:, s, :], scalar=1.0, in1=mk[:, s, :], op0=ALU.mult, op1=ALU.mult,
                                                                    accum_out=sm[:, s, 1:2]), reads=[r_ge, r_mk], writes=[r_ge, r_sm])
                kb.op("dve", lambda e_, s=s: e_.reciprocal(out=sm[:, s, 2:3], in_=sm[:, s, 1:2]), reads=[r_sm], writes=[r_sm])
                kb.op("dve", lambda e_, s=s: e_.tensor_scalar(out=go[:, b, s, :], in0=ge[:, s, :], scalar1=sm[:, s, 2:3], scalar2=None, op0=ALU.mult),
                      reads=[r_ge, r_sm], writes=[r_go[b]])
            kb.dma("pool", S["GT"][t0:t0 + T, :].rearrange("(s p) e -> p s e", p=128), go[:, b, :nsub, :], reads=[r_go[b]], writes=[g.RS["GT"]])


def rope_perm():
    p = np.arange(64)
    half = (p % 32) // 16
    return np.where(half == 0, p + 16, p - 16)


def rope_tables():
    rows = SEQ // 64
    t = np.arange(SEQ)
    row = (t // 64).astype(np.float32)
    col = (t % 64).astype(np.float32)
    inv = (np.float32(10000.0) ** (-np.arange(16, dtype=np.float32) / np.float32(16))).astype(np.float32)
    cosT = np.zeros((64, SEQ), np.float32)
    sinT = np.zeros((64, SEQ), np.float32)
    for p in range(64):
        axis, half, f = p // 32, (p % 32) // 16, p % 16
        ang = (row if axis == 0 else col) * inv[f]
        cosT[p] = np.cos(ang)
        sinT[p] = np.sin(ang) * (-1.0 if half == 0 else 1.0)
    return cosT, sinT


def host_inputs(inp):
    perm = rope_perm()
    w_in = inp["w_in"]
    w_in_aug = np.concatenate([w_in[:, :, :3776], w_in[:, :, 3712:3776][:, :, perm], w_in[:, :, 3776:]], axis=2)
    w_uq = inp["w_uq"].reshape(2, 384, 8, 192)
    w_uq_aug = np.concatenate([w_uq, w_uq[:, :, :, 128:][:, :, :, perm]], axis=3).reshape(2, 384, 2048)
    w_ukv = inp["w_ukv"].reshape(2, 256, 8, 256)
    w_ukv_aug = np.concatenate([w_ukv[:, :, :, :128].reshape(2, 256, 1024), w_ukv[:, :, :, 128:].reshape(2, 256, 1024)], axis=2)
    cosT, sinT = rope_tables()
    shared = {k: np.ascontiguousarray(v) for k, v in inp.items() if k not in ("x", "c", "ctx", "c_ctx", "w_in", "w_uq", "w_ukv")}
    shared["w_in"] = np.ascontiguousarray(w_in_aug)
    shared["w_uq"] = np.ascontiguousarray(w_uq_aug)
    shared["w_ukv"] = np.ascontiguousarray(w_ukv_aug)
    shared["ident"] = np.eye(128, dtype=np.float32)
    del shared["router_w"]
    shared["router_wT"] = np.ascontiguousarray(inp["router_w"].transpose(0, 2, 1))
    shared["mod_b_col"] = np.ascontiguousarray(inp["mod_b"].reshape(2, 48, 128).transpose(0, 2, 1))
    vecs = {"pre_mix_g": inp["pre_mix_g"], "pre_ffn_g": inp["pre_ffn_g"], "pool_scale": inp["pool_scale"], "conv_b": inp["conv_b"]}
    for k in range(4):
        vecs["conv_w%d" % k] = inp["conv_w"][:, k]
    for d in range(2):
        vecs["gate_a_b%d" % d] = inp["gate_a_b"][:, d]
        vecs["gate_x_b%d" % d] = inp["gate_x_b"][:, d]
        vecs["lru_lambda%d" % d] = inp["lru_lambda"][:, d]
    vc = np.stack([vecs[n] for n in VNAMES], axis=1)
    shared["vcol"] = np.ascontiguousarray(vc.reshape(2, NV, 8, 128).transpose(0, 3, 1, 2))
    shared["qg_col"] = np.ascontiguousarray(inp["q_norm_g"].reshape(2, 3, 128).transpose(0, 2, 1))
    shared["kvg_col"] = np.ascontiguousarray(inp["kv_norm_g"].reshape(2, 2, 128).transpose(0, 2, 1))
    shared["cosT"] = cosT
    shared["sinT"] = sinT
    maps = []
    for c in range(8):
        b, half = c % 4, c // 4
        m = dict(shared)
        m["cosQ"] = np.ascontiguousarray(cosT[:, half * HALF:(half + 1) * HALF])
        m["sinQ"] = np.ascontiguousarray(sinT[:, half * HALF:(half + 1) * HALF])
        hmv = np.zeros((128, 2), np.float32)
        hmv[:, half] = 1.0
        m["hm"] = hmv
        m["xall"] = np.ascontiguousarray(np.concatenate([inp["ctx"][b], inp["x"][b]], axis=0))
        cc = np.stack([inp["c"][b], inp["c_ctx"]], axis=1)
        m["ccol"] = np.ascontiguousarray(cc.reshape(8, 128, 2).transpose(1, 0, 2))
        maps.append(m)
    return maps


def kernel(**inputs):
    inp = {k: np.asarray(v) for k, v in inputs.items()}
    maps = host_inputs(inp)
    nc = bass.Bass("TRN2", target_bir_lowering=False)
    build(nc)
    res = run_bass_kernel_spmd(nc, maps, core_ids=list(range(8)))
    return np.stack([np.concatenate([res.results[b]["out"], res.results[b + 4]["out"]], axis=0) for b in range(4)], axis=0).astype(np.float32)
```
